# Optimizing a Trainium2 kernel written in Bass

```python
import math
import jax, jax.numpy as jnp
from jax import lax
import numpy as np

D_MODEL = 1024
BATCH = 32
SEQ = 2048
DEPTH = 1

GRID_W = 64
CTX_LEN = 256
EPS = 1e-6
N_HEADS = 8
HEAD_DIM = D_MODEL // 16
QK_W = N_HEADS * 2 * HEAD_DIM
V_W = N_HEADS * 2 * HEAD_DIM
ATTN_SCALE = HEAD_DIM ** -0.5
ROT_FREQS = HEAD_DIM // 4
ROPE_BASE = 10000.0
Q_BLOCK = 128
LAMBDA_STD = 0.1
F_GROUPS = 4
F_GROUP_DIM = D_MODEL // 8
F_W = F_GROUPS * F_GROUP_DIM
Q_OFF = 0
K_OFF = Q_OFF + QK_W
V_OFF = K_OFF + QK_W
F_OFF = V_OFF + V_W
GA_OFF = F_OFF + F_W
GF_OFF = GA_OFF + D_MODEL
PROJ_W = GF_OFF + D_MODEL
N_GROUPS = 4
EXPERTS_PER_GROUP = 4
N_EXPERTS = N_GROUPS * EXPERTS_PER_GROUP
TOP_K_IN_GROUP = 2
EXPERT_HIDDEN = D_MODEL // 2

kernel_name = 'hybrid_diffattn_fnet_hmoe_dit'


def rmsnorm(x, g):
    x32 = x.astype(jnp.float32)
    y = x32 * lax.rsqrt(jnp.mean(x32 * x32, axis=-1, keepdims=True) + EPS)
    return (y * g.astype(jnp.float32)).astype(x.dtype)


def modulate(h, shift, scale):
    return h * (1 + scale) + shift


def axial_angles(rows):
    inv = 1.0 / (ROPE_BASE ** (jnp.arange(ROT_FREQS, dtype=jnp.float32) / ROT_FREQS))
    row = jnp.broadcast_to(jnp.arange(rows, dtype=jnp.float32)[:, None], (rows, GRID_W)).reshape(-1)
    col = jnp.broadcast_to(jnp.arange(GRID_W, dtype=jnp.float32)[None, :], (rows, GRID_W)).reshape(-1)
    return row[:, None] * inv[None, :], col[:, None] * inv[None, :]


def rotate(x, ang):
    cos = jnp.cos(ang)[:, None, None, :].astype(x.dtype)
    sin = jnp.sin(ang)[:, None, None, :].astype(x.dtype)
    x1, x2 = x[..., :ROT_FREQS], x[..., ROT_FREQS:]
    return jnp.concatenate([x1 * cos - x2 * sin, x2 * cos + x1 * sin], axis=-1)


def axial_rope(x, ang_row, ang_col):
    half = 2 * ROT_FREQS
    return jnp.concatenate([rotate(x[..., :half], ang_row), rotate(x[..., half:], ang_col)], axis=-1)


def split_proj(p):
    b, n = p.shape[:2]
    q = p[..., Q_OFF:K_OFF].reshape(b, n, N_HEADS, 2, HEAD_DIM)
    k = p[..., K_OFF:V_OFF].reshape(b, n, N_HEADS, 2, HEAD_DIM)
    v = p[..., V_OFF:F_OFF].reshape(b, n, N_HEADS, 2 * HEAD_DIM)
    f = p[..., F_OFF:GA_OFF]
    ga = p[..., GA_OFF:GF_OFF]
    gf = p[..., GF_OFF:PROJ_W]
    return q, k, v, f, ga, gf


def diff_attend(q, k, v, lam):
    s = jnp.einsum('bqhid,bkhid->bhiqk', q, k, preferred_element_type=jnp.float32) * ATTN_SCALE
    p = jax.nn.softmax(s, axis=-1)
    a = p[:, :, 0] - lam * p[:, :, 1]
    return jnp.einsum('bhqk,bkhe->bqhe', a.astype(v.dtype), v)


def blocked_latent_attention(q, k_all, v_all, lam):
    b, n = q.shape[:2]
    qb = q.reshape(b, n // Q_BLOCK, Q_BLOCK, N_HEADS, 2, HEAD_DIM).transpose(1, 0, 2, 3, 4, 5)
    out = lax.map(lambda qblk: diff_attend(qblk, k_all, v_all, lam), qb)
    return out.transpose(1, 0, 2, 3, 4).reshape(b, n, N_HEADS, 2 * HEAD_DIM)


def merge_branches(heads, f, ga, gf, subln_g, lam_init, w_ao, w_fo, w_o):
    b, n = heads.shape[:2]
    attn = (rmsnorm(heads, subln_g) * (1.0 - lam_init)).reshape(b, n, V_W)
    fg = f.reshape(b, n, F_GROUPS, F_GROUP_DIM).astype(jnp.float32)
    four = jnp.fft.fftn(fg, axes=(1, 3), norm='ortho').real.astype(f.dtype).reshape(b, n, F_W)
    y = jax.nn.sigmoid(ga) * (attn @ w_ao) + jax.nn.sigmoid(gf) * (four @ w_fo)
    return y @ w_o


def hier_moe(h, w_rg, b_rg, w_re, b_re, w1, w3, w2):
    b, n, d = h.shape
    t = h.reshape(-1, d)
    lg = (t @ w_rg + b_rg).astype(jnp.float32)
    g_sel = jnp.argmax(lg, axis=-1)
    w_grp = jnp.take_along_axis(jax.nn.softmax(lg, axis=-1), g_sel[:, None], axis=-1)
    le = (t @ w_re + b_re).astype(jnp.float32).reshape(-1, N_GROUPS, EXPERTS_PER_GROUP)
    le_g = jnp.take_along_axis(le, g_sel[:, None, None], axis=1)[:, 0]
    top_v, top_i = lax.top_k(le_g, TOP_K_IN_GROUP)
    w_top = jax.nn.softmax(top_v, axis=-1) * w_grp
    eid = g_sel[:, None] * EXPERTS_PER_GROUP + top_i
    gates = jnp.einsum('tk,tke->et', w_top, jax.nn.one_hot(eid, N_EXPERTS, dtype=jnp.float32)).astype(t.dtype)

    def expert_step(acc, xs):
        w1e, w3e, w2e, ge = xs
        y = (jax.nn.silu(t @ w1e) * (t @ w3e)) @ w2e
        return acc + ge[:, None] * y, None

    acc, _ = lax.scan(expert_step, jnp.zeros_like(t), (w1, w3, w2, gates))
    return acc.reshape(b, n, d)


def setup_inputs(seed: int = 0) -> dict:
    key = jax.random.key(seed)
    ks = jax.random.split(key, 32)
    f32 = jnp.float32
    d = D_MODEL

    def nrm(k, shape, scale):
        return jax.random.normal(k, shape, f32) * scale

    return {
        'x': nrm(ks[0], (BATCH, SEQ, d), 1.0),
        'c': nrm(ks[1], (BATCH, d), 1.0),
        'ctx': nrm(ks[2], (BATCH, CTX_LEN, d), 1.0),
        'c_ctx': nrm(ks[3], (d,), 1.0),
        'w_mod': nrm(ks[4], (DEPTH, d, 6 * d), 0.5 * d ** -0.5),
        'b_mod': nrm(ks[5], (DEPTH, 6 * d), 0.02),
        'norm1_g': 1.0 + nrm(ks[6], (DEPTH, d), 0.02),
        'norm2_g': 1.0 + nrm(ks[7], (DEPTH, d), 0.02),
        'w_in': nrm(ks[8], (DEPTH, d, PROJ_W), d ** -0.5),
        'lam_q1': nrm(ks[9], (DEPTH, HEAD_DIM), LAMBDA_STD),
        'lam_k1': nrm(ks[10], (DEPTH, HEAD_DIM), LAMBDA_STD),
        'lam_q2': nrm(ks[11], (DEPTH, HEAD_DIM), LAMBDA_STD),
        'lam_k2': nrm(ks[12], (DEPTH, HEAD_DIM), LAMBDA_STD),
        'subln_g': 1.0 + nrm(ks[13], (DEPTH, 2 * HEAD_DIM), 0.02),
        'w_attn_out': nrm(ks[14], (DEPTH, V_W, d), V_W ** -0.5),
        'w_four_out': nrm(ks[15], (DEPTH, F_W, d), F_W ** -0.5),
        'w_out': nrm(ks[16], (DEPTH, d, d), d ** -0.5),
        'w_router_group': nrm(ks[17], (DEPTH, d, N_GROUPS), d ** -0.5),
        'b_router_group': nrm(ks[18], (DEPTH, N_GROUPS), 0.01),
        'w_router_expert': nrm(ks[19], (DEPTH, d, N_EXPERTS), d ** -0.5),
        'b_router_expert': nrm(ks[20], (DEPTH, N_EXPERTS), 0.01),
        'w_exp_gate': nrm(ks[21], (DEPTH, N_EXPERTS, d, EXPERT_HIDDEN), d ** -0.5),
        'w_exp_up': nrm(ks[22], (DEPTH, N_EXPERTS, d, EXPERT_HIDDEN), d ** -0.5),
        'w_exp_down': nrm(ks[23], (DEPTH, N_EXPERTS, EXPERT_HIDDEN, d), EXPERT_HIDDEN ** -0.5),
        'final_g': 1.0 + nrm(ks[24], (d,), 0.02),
    }


def reference(x, c, ctx, c_ctx, w_mod, b_mod, norm1_g, norm2_g, w_in, lam_q1, lam_k1, lam_q2, lam_k2,
              subln_g, w_attn_out, w_four_out, w_out, w_router_group, b_router_group, w_router_expert,
              b_router_expert, w_exp_gate, w_exp_up, w_exp_down, final_g):
    n = x.shape[1]
    rows = n // GRID_W
    ang_row, ang_col = axial_angles(rows)
    x_lat, x_ctx = x, ctx
    for l in range(DEPTH):
        update_ctx = l < DEPTH - 1
        lam_init = 0.8 - 0.6 * math.exp(-0.3 * l)
        lam = (jnp.exp(jnp.sum(lam_q1[l].astype(jnp.float32) * lam_k1[l].astype(jnp.float32)))
               - jnp.exp(jnp.sum(lam_q2[l].astype(jnp.float32) * lam_k2[l].astype(jnp.float32)))
               + lam_init)
        sh1, sc1, g1, sh2, sc2, g2 = [m[:, None, :] for m in jnp.split(jax.nn.silu(c) @ w_mod[l] + b_mod[l], 6, axis=-1)]
        csh1, csc1, cg1, csh2, csc2, cg2 = jnp.split(jax.nn.silu(c_ctx) @ w_mod[l] + b_mod[l], 6, axis=-1)

        h_c = modulate(rmsnorm(x_ctx, norm1_g[l]), csh1, csc1)
        if update_ctx:
            q_c, k_c, v_c, f_c, ga_c, gf_c = split_proj(h_c @ w_in[l])
        else:
            kv_c = h_c @ w_in[l][:, K_OFF:F_OFF]
            k_c = kv_c[..., :QK_W].reshape(x_ctx.shape[0], x_ctx.shape[1], N_HEADS, 2, HEAD_DIM)
            v_c = kv_c[..., QK_W:].reshape(x_ctx.shape[0], x_ctx.shape[1], N_HEADS, 2 * HEAD_DIM)
        h_l = modulate(rmsnorm(x_lat, norm1_g[l]), sh1, sc1)
        q_l, k_l, v_l, f_l, ga_l, gf_l = split_proj(h_l @ w_in[l])
        q_l = axial_rope(q_l, ang_row, ang_col)
        k_l = axial_rope(k_l, ang_row, ang_col)
        k_all = jnp.concatenate([k_c, k_l], axis=1)
        v_all = jnp.concatenate([v_c, v_l], axis=1)
        heads_l = blocked_latent_attention(q_l, k_all, v_all, lam)
        mix_l = merge_branches(heads_l, f_l, ga_l, gf_l, subln_g[l], lam_init, w_attn_out[l], w_four_out[l], w_out[l])
        x_lat = x_lat + g1 * mix_l
        if update_ctx:
            heads_c = diff_attend(q_c, k_c, v_c, lam)
            mix_c = merge_branches(heads_c, f_c, ga_c, gf_c, subln_g[l], lam_init, w_attn_out[l], w_four_out[l], w_out[l])
            x_ctx = x_ctx + cg1 * mix_c

        h2_l = modulate(rmsnorm(x_lat, norm2_g[l]), sh2, sc2)
        x_lat = x_lat + g2 * hier_moe(h2_l, w_router_group[l], b_router_group[l], w_router_expert[l],
                                      b_router_expert[l], w_exp_gate[l], w_exp_up[l], w_exp_down[l])
        if update_ctx:
            h2_c = modulate(rmsnorm(x_ctx, norm2_g[l]), csh2, csc2)
            x_ctx = x_ctx + cg2 * hier_moe(h2_c, w_router_group[l], b_router_group[l], w_router_expert[l],
                                          b_router_expert[l], w_exp_gate[l], w_exp_up[l], w_exp_down[l])
    return rmsnorm(x_lat, final_g)
```

```python
import contextlib
import math
import os
import numpy as np
import ml_dtypes
import concourse.bass as bass
import concourse.mybir as mybir
from concourse.bass_utils import run_bass_kernel_spmd

F32 = mybir.dt.float32
BF16 = mybir.dt.bfloat16
AF = mybir.ActivationFunctionType
ALU = mybir.AluOpType
AX = mybir.AxisListType

D = 1024
DC = 8
N = 2048
NT = 16
CTX = 256
NK = N + CTX
KC = NK // 128
NH = 8
PROJ_W = 5632
Q_OFF, K_OFF, V_OFF, F_OFF, GA_OFF, GF_OFF = 0, 1024, 2048, 3072, 3584, 4608
NE = 16
EH = 512
EPS = 1e-6
LAM_INIT = 0.8 - 0.6 * math.exp(-0.3 * 0)
ATTN_SCALE = 64 ** -0.5
N_CORES = 8


class Buf:
    __slots__ = ("name", "last_w", "readers", "dsem", "dcount")

    def __init__(self, name):
        self.name = name
        self.last_w = {}
        self.readers = {}
        self.dsem = None
        self.dcount = 0


class Fw:
    ENG = ("pe", "act", "dve", "pool", "sp")

    def __init__(self, nc, stack):
        self.nc = nc
        self.stack = stack
        self.sems = []
        self.q = {e: [] for e in self.ENG}
        self.esem = {e: self.newsem("e_" + e) for e in ("pe", "act", "dve", "pool")}
        self.ecnt = {e: 0 for e in ("pe", "act", "dve", "pool")}
        self.waited = {e: {} for e in self.ENG}
        self.dma_events = {}
        self.dsem_by_name = {}

    def newsem(self, name):
        s = self.stack.enter_context(self.nc.semaphore(name))
        self.sems.append(s)
        return len(self.sems) - 1

    def _deps(self, reads, writes, acc=False):
        evs = {}

        def add(s, v):
            if evs.get(s, 0) < v:
                evs[s] = v

        for b in reads:
            for s, v in b.last_w.items():
                add(s, v)
        for b in writes:
            if not acc:
                for s, v in b.last_w.items():
                    add(s, v)
            for s, v in b.readers.items():
                add(s, v)
        return evs

    def _waits(self, eng, evs):
        w = self.waited[eng]
        own = self.esem.get(eng) if eng == "pe" else None
        for s, v in evs.items():
            if s == own:
                continue
            if w.get(s, 0) < v:
                w[s] = v
                self.q[eng].append(("w", s, v))

    def _record(self, ev, reads, writes, acc=False):
        for b in writes:
            if acc:
                if b.last_w.get(ev[0], 0) < ev[1]:
                    b.last_w[ev[0]] = ev[1]
            else:
                b.last_w = {ev[0]: ev[1]}
                b.readers = {}
        for b in reads:
            if b not in writes:
                if b.readers.get(ev[0], 0) < ev[1]:
                    b.readers[ev[0]] = ev[1]

    def op(self, eng, fn, reads=(), writes=(), acc=False):
        self._waits(eng, self._deps(reads, writes, acc))
        self.ecnt[eng] += 1
        ev = (self.esem[eng], self.ecnt[eng])
        self.q[eng].append(("i", fn, self.esem[eng]))
        self._record(ev, reads, writes, acc)
        return ev

    def dma(self, q, out_ap, in_ap, reads, writes, sembuf, acc=False):
        self._waits(q, self._deps(reads, writes, acc))
        if sembuf.name not in self.dsem_by_name:
            self.dsem_by_name[sembuf.name] = [self.newsem("d_" + sembuf.name), 0]
        ent = self.dsem_by_name[sembuf.name]
        ent[1] += 16
        sembuf.dsem, sembuf.dcount = ent[0], ent[1]
        ev = (sembuf.dsem, sembuf.dcount)
        self.dma_events[sembuf.dsem] = sembuf.dcount
        self.q[q].append(("d", out_ap, in_ap, sembuf.dsem))
        self._record(ev, reads, writes, acc)
        return ev

    def dma_fn(self, q, fn, reads, writes, sembuf, acc=False):
        self._waits(q, self._deps(reads, writes, acc))
        if sembuf.name not in self.dsem_by_name:
            self.dsem_by_name[sembuf.name] = [self.newsem("d_" + sembuf.name), 0]
        ent = self.dsem_by_name[sembuf.name]
        ent[1] += 16
        sembuf.dsem, sembuf.dcount = ent[0], ent[1]
        ev = (sembuf.dsem, sembuf.dcount)
        self.dma_events[sembuf.dsem] = sembuf.dcount
        self.q[q].append(("f", fn, sembuf.dsem))
        self._record(ev, reads, writes, acc)
        return ev

    def wait_bufs(self, eng, bufs):
        evs = {}
        for b in bufs:
            for s, v in list(b.last_w.items()) + list(b.readers.items()):
                if evs.get(s, 0) < v:
                    evs[s] = v
        self._waits(eng, evs)

    def emit(self):
        nc = self.nc
        sems = self.sems

        def run(e, items):
            for it in items:
                if it[0] == "w":
                    e.wait_ge(sems[it[1]], it[2])
                elif it[0] == "i":
                    ins = it[1](e)
                    ins.then_inc(sems[it[2]], 1)
                elif it[0] == "f":
                    ins = it[1](e)
                    ins.then_inc(sems[it[2]], 16)
                else:
                    e.dma_start(out=it[1], in_=it[2]).then_inc(sems[it[3]], 16)

        with nc.Block() as block:

            @block.tensor
            def _(t):
                run(t, self.q["pe"])

            @block.scalar
            def _(t):
                run(t, self.q["act"])

            @block.vector
            def _(t):
                run(t, self.q["dve"])

            @block.gpsimd
            def _(t):
                run(t, self.q["pool"])

            @block.sync
            def _(t):
                run(t, self.q["sp"])


def _bf(a):
    return np.ascontiguousarray(a.astype(ml_dtypes.bfloat16))


def make_consts():
    c = {}
    c["ident"] = np.eye(128, dtype=np.float32)
    R = np.zeros((128, 128), np.float32)
    for i in range(128):
        if (i % 32) < 16:
            R[i, i + 16] = -1.0
        else:
            R[i, i - 16] = 1.0
    c["rt"] = _bf(R.T)
    inv = (1.0 / (np.float32(10000.0) ** (np.arange(16, dtype=np.float32) / np.float32(16)))).astype(np.float32)
    tok = np.arange(N)
    row = (tok // 64).astype(np.float32)
    col = (tok % 64).astype(np.float32)
    cs = np.zeros((128, 2, N), np.float32)
    for p in range(128):
        hh = (p % 64) // 32
        f = p % 16
        ang = ((row if hh == 0 else col) * inv[f]).astype(np.float32)
        cs[p, 0] = np.cos(ang)
        cs[p, 1] = np.sin(ang)
    c["cossin"] = _bf(cs)
    k = np.arange(128, dtype=np.float64)
    a = 2 * np.pi * np.outer(k, k) / 128.0
    c["cs_c"] = _bf(np.concatenate([np.cos(a), np.sin(a)], axis=1) / np.sqrt(128.0))
    n = np.arange(N, dtype=np.int64)
    prod = np.outer(n, n) % N
    ang = 2 * np.pi * prod.astype(np.float64) / N
    tabs = np.stack([np.cos(ang), -np.sin(ang)], 0) / np.sqrt(float(N))
    t = tabs.reshape(2, 4, 4, 128, 4, 512)
    t = t.transpose(0, 4, 1, 3, 2, 5)
    c["dft"] = _bf(t)
    kk = np.arange(128)
    c["ustrict"] = (kk[:, None] < kk[None, :]).astype(np.float32)
    c["thr16"] = np.ascontiguousarray(np.broadcast_to((512.0 * np.arange(16, dtype=np.float32))[None, :], (128, 16)))
    c["thr48"] = np.ascontiguousarray(np.broadcast_to((512.0 * np.arange(48, dtype=np.float32))[None, :], (128, 48)))
    ee = np.arange(16)
    lt = (ee[None, :] < ee[:, None]).astype(np.float32)
    c["ltmask"] = np.ascontiguousarray(np.broadcast_to(lt[None], (128, 16, 16)))
    c["pidx"] = np.arange(128, dtype=np.float32).reshape(128, 1)
    return c


_CONSTS = None


def get_consts():
    global _CONSTS
    if _CONSTS is None:
        _CONSTS = make_consts()
    return _CONSTS


_DTSZ = {F32: 4, BF16: 2, mybir.dt.int32: 4}


class Region:
    def __init__(self, tensor, nbytes):
        self.t = tensor
        self.n = nbytes
        self.off = 0
        self.live = []
        self.hist = {}

    def newbuf(self, name):
        b = Buf(name)
        b.readers = dict(self.hist)
        self.live.append(b)
        return b

    def alloc(self, name, shape, dt):
        nbytes = int(np.prod(shape[1:])) * _DTSZ[dt]
        nbytes_al = (nbytes + 31) // 32 * 32
        assert self.off + nbytes_al <= self.n, (name, self.off, nbytes_al, self.n)
        v = self.t[:, self.off:self.off + nbytes].bitcast(dt)
        if len(shape) == 3:
            v = v.rearrange("p (a b) -> p a b", b=shape[2])
        self.off += nbytes_al
        return v, self.newbuf(name)

    def mark(self):
        return (self.off, len(self.live))

    def release(self, m):
        for b in self.live[m[1]:]:
            for ev in list(b.last_w.items()) + list(b.readers.items()):
                if self.hist.get(ev[0], 0) < ev[1]:
                    self.hist[ev[0]] = ev[1]
        del self.live[m[1]:]
        self.off = m[0]


def mm_group(out_ap, pairs):
    def fn(e):
        ins = None
        n = len(pairs)
        for i, (l, r) in enumerate(pairs):
            ins = e.matmul(out_ap, lhsT=l, rhs=r, start=(i == 0), stop=(i == n - 1))
        return ins
    return fn


def build(nb=4, stage=99, dbg=False):
    nc = bass.Bass("TRN2", target_bir_lowering=False)
    U8 = mybir.dt.uint8

    def inp(name, shape, dt=F32):
        return nc.dram_tensor(name, list(shape), dt, kind="ExternalInput").ap()

    x_d = inp("x", [nb, N, D])
    ctx_d = inp("ctx", [nb, CTX, D])
    cT_d = inp("cT", [128, DC, 8])
    wmod_d = inp("w_mod", [D, 6 * D])
    bmodT_d = inp("bmodT", [128, 48])
    bmg_d = inp("bmg", [128, 4 * D])
    n2gbc_d = inp("n2gbc", [128, D])
    ustrict_d = inp("ustrict", [128, 128])
    thr16_d = inp("thr16", [128, 16])
    thr48_d = inp("thr48", [128, 48])
    ltmask_d = inp("ltmask", [128, 16, 16])
    pidx_d = inp("pidx", [128, 1])
    n1g_d = inp("n1g", [128, DC])
    n2g_d = inp("n2g", [128, DC])
    fing_d = inp("fing", [128, D])
    lamv_d = inp("lamv", [128, 256])
    subg_d = inp("subg", [128, 128])
    wr_d = inp("wr", [128, DC, 20])
    br_d = inp("br", [128, 20])
    win_d = inp("w_in", [D, PROJ_W])
    wao_d = inp("w_ao", [D, D])
    wfo_d = inp("w_fo", [512, D])
    wo_d = inp("w_o", [D, D])
    w1_d = inp("w1", [NE, D, EH])
    w3_d = inp("w3", [NE, D, EH])
    w2_d = inp("w2", [NE, EH, D])
    ident_d = inp("ident", [128, 128])
    rt_d = inp("rt", [128, 128], BF16)
    cossin_d = inp("cossin", [128, 2, N], BF16)
    csc_d = inp("cs_c", [128, 256], BF16)
    dft_d = inp("dft", [2, 4, 4, 128, 4, 512], BF16)
    out_d = nc.dram_tensor("out", [nb, N, D], F32, kind="ExternalOutput").ap()

    def scr(name, shape, dt=BF16):
        return nc.dram_tensor(name, list(shape), dt).ap()

    wqkv_s = scr("wqkv_s", [NH, 128, DC, 384])
    wf_s = scr("wf_s", [128, DC, 512])
    wg_s = scr("wg_s", [4, 128, DC, 512])
    wao_s = scr("wao_s", [2, 128, DC, 512])
    wfo_s = scr("wfo_s", [128, 4, D])
    wo_s = scr("wo_s", [2, 128, DC, 512])
    w1_s = scr("w1_s", [NE, 128, DC, EH])
    w3_s = scr("w3_s", [NE, 128, DC, EH])
    w2_s = scr("w2_s", [NE, 128, 4, D])
    gsc_s = scr("gsc_s", [nb, 4, 128, D], F32)
    J = nb * NT
    NST = nb * 8 + 16
    x1_s = scr("x1_s", [nb * N, D], F32)
    h2_s = scr("h2_s", [nb * N, D], BF16)
    hs_s = scr("hs_s", [NST * 512, D], BF16)
    ys_s = scr("ys_s", [NST * 512, D], BF16)

    with contextlib.ExitStack() as st:
        fw = Fw(nc, st)

        def sb(name, shape, dt):
            return st.enter_context(nc.sbuf_tensor(name, list(shape), dt))

        regA_t = sb("regA", [128, DC * NK * 2], U8)
        regB_t = sb("regB", [128, 65536], U8)
        regC_t = sb("regC", [128, 32768], U8)
        regD_t = sb("regD", [128, 32768], U8)
        ring_t = sb("ring", [128, 32768], U8)
        ident = sb("ident_sb", [128, 128], F32)
        rt = sb("rt_sb", [128, 128], BF16)
        csc = sb("csc_sb", [128, 256], BF16)
        smallt = sb("small", [128, 16, 16], F32)
        modA1 = sb("modA1", [128, DC, 8], F32)
        modB1 = sb("modB1", [128, DC, 8], F32)
        modA2 = sb("modA2", [128, DC, 8], F32)
        modB2 = sb("modB2", [128, DC, 8], F32)
        subg = sb("subg_sb", [128, 128], F32)
        lamt = sb("lamt", [128, 8], F32)
        epst = sb("epst", [128, 8], F32)
        Gall = sb("Gall", [128, nb * NT, 16], F32)

        HT = regA_t[:, :].bitcast(BF16).rearrange("p (a b) -> p a b", b=NK)
        RB = Region(regB_t, 65536)
        RC = Region(regC_t, 32768)
        RD = Region(regD_t, 32768)

        psbig = st.enter_context(nc.psum_tensor("psbig", [128, 4096], F32))
        ps = [psbig[:, i * 512:(i + 1) * 512] for i in range(8)]
        PS = [Buf(f"ps{i}") for i in range(8)]

        B_const = Buf("const")
        B_HT = [Buf(f"HT{i}") for i in range(5)]
        B_mod = Buf("mod")
        B_lam = Buf("lam")
        B_scr = {k: Buf(k) for k in ("wqkv", "wf", "wg", "wao", "wfo", "wo", "gsc")}
        B_w1 = [Buf(f"w1s{e}") for e in range(NE)]
        B_out = Buf("outd")
        B_G = Buf("Gall")
        B_x1s = Buf("x1s")
        B_h2s = Buf("h2s")
        B_small = [Buf(f"small{i}") for i in range(16)]
        small_ctr = [0]

        def small_alloc():
            i = small_ctr[0] % 16
            small_ctr[0] += 1
            return smallt[:, i, :], B_small[i]

        ring_slots = [(ring_t[:, i * 8192:(i + 1) * 8192].bitcast(BF16), Buf(f"ring{i}")) for i in range(4)]

        def chunk_view(ap, shape):
            n = int(np.prod(shape[1:]))
            return ap[:, 0:n].rearrange("p (a b) -> p a b", b=shape[2])

        class Ring:
            def __init__(self, slots, la):
                self.slots = slots
                self.la = la
                self.sched = []
                self.issued = 0
                self.taken = 0
                self.views = {}

            def plan(self, src_ap, shape, srcbuf):
                self.sched.append((src_ap, shape, srcbuf))

            def _issue(self, i):
                src_ap, shape, srcbuf = self.sched[i]
                ap, buf = self.slots[i % len(self.slots)]
                v = chunk_view(ap, shape)
                fw.dma("sp", v, src_ap, [srcbuf], [buf], buf)
                return v, buf

            def take(self):
                i = self.taken
                self.taken += 1
                while self.issued < min(len(self.sched), i + 1 + self.la):
                    self.views[self.issued] = self._issue(self.issued)
                    self.issued += 1
                return self.views.pop(i)

        fw.dma("sp", ident[:], ident_d, [], [B_const], B_const)
        fw.dma("sp", rt[:], rt_d, [], [B_const], B_const)
        fw.dma("sp", csc[:], csc_d, [], [B_const], B_const)
        B_subg = Buf("subg")
        fw.dma("sp", subg[:], subg_d, [], [B_subg], B_subg)
        B_eps = Buf("eps")
        fw.op("pool", lambda e: e.memset(epst[:], EPS), [], [B_eps])

        def wview(w, c0, cw):
            return w[:, c0:c0 + cw].rearrange("(kc p) j -> p kc j", p=128)

        for h in range(NH):
            for sec, off in enumerate((Q_OFF, K_OFF, V_OFF)):
                fw.dma("pool", wqkv_s[h, :, :, sec * 128:(sec + 1) * 128],
                       wview(win_d, off + h * 128, 128), [], [B_scr["wqkv"]], B_scr["wqkv"])
        fw.dma("pool", wf_s, wview(win_d, F_OFF, 512), [], [B_scr["wf"]], B_scr["wf"])
        for i, off in enumerate((GA_OFF, GA_OFF + 512, GF_OFF, GF_OFF + 512)):
            fw.dma("pool", wg_s[i], wview(win_d, off, 512), [], [B_scr["wg"]], B_scr["wg"])
        for i in range(2):
            fw.dma("pool", wao_s[i], wview(wao_d, i * 512, 512), [], [B_scr["wao"]], B_scr["wao"])
            fw.dma("pool", wo_s[i], wview(wo_d, i * 512, 512), [], [B_scr["wo"]], B_scr["wo"])
        fw.dma("pool", wfo_s, wview(wfo_d, 0, D), [], [B_scr["wfo"]], B_scr["wfo"])
        for e in range(NE):
            fw.dma("pool", w1_s[e], wview(w1_d[e], 0, EH), [], [B_w1[e]], B_w1[e])
            fw.dma("pool", w3_s[e], wview(w3_d[e], 0, EH), [], [B_w1[e]], B_w1[e])
            fw.dma("pool", w2_s[e], wview(w2_d[e], 0, D), [], [B_w1[e]], B_w1[e])

        mD = RD.mark()
        mC = RC.mark()
        wm, B_wm = RD.alloc("wm", [128, DC, D], F32)
        rep, B_rep = RC.alloc("rep", [128, 4 * DC, 128], F32)
        sc, B_sc = RC.alloc("sc", [128, DC, 8], F32)
        modT, B_modT = RC.alloc("modT", [128, 6 * DC, 8], F32)
        bmodT, B_p0c = RC.alloc("bmodT", [128, 48, 1], F32)
        n1g, _ = RC.alloc("n1g", [128, DC, 1], F32)
        n2g, _ = RC.alloc("n2g", [128, DC, 1], F32)
        mB0 = RB.mark()
        wm2, B_wm2 = RB.alloc("wm2", [128, DC, D], F32)
        dgt = [RB.alloc(f"dg{i}", [128, 128], F32) for i in range(4)]
        gtmp0, B_gt0 = RC.alloc("gtmp0", [128, 512], F32)
        gtmp1, B_gt1 = RC.alloc("gtmp1", [128, 512], F32)
        gtmp = [gtmp0, gtmp1]
        B_gtmp = [B_gt0, B_gt1]
        ones_t, B_ones = RC.alloc("ones", [128, 128], F32)
        lamv, B_lamv = RC.alloc("lamv", [128, 256], F32)
        lamp, B_lamp = RC.alloc("lamp", [128, 2, 64], F32)

        fw.dma("sp", sc, cT_d, [], [B_sc], B_sc)
        fw.dma("sp", bmodT[:, :, 0], bmodT_d, [], [B_p0c], B_p0c)
        fw.dma("sp", n1g[:, :, 0], n1g_d, [], [B_p0c], B_p0c)
        fw.dma("sp", n2g[:, :, 0], n2g_d, [], [B_p0c], B_p0c)
        fw.dma("sp", lamv, lamv_d, [], [B_lamv], B_lamv)
        fw.op("dve", lambda e: e.tensor_tensor(out=lamp[:, 0, :], in0=lamv[:, 0:64], in1=lamv[:, 64:128],
                                               op=ALU.mult), [B_lamv], [B_lamp])
        fw.op("dve", lambda e: e.tensor_tensor(out=lamp[:, 1, :], in0=lamv[:, 128:192], in1=lamv[:, 192:256],
                                               op=ALU.mult), [B_lamv, B_lamp], [B_lamp])
        fw.op("dve", lambda e: e.tensor_reduce(out=lamt[:, 0:2], in_=lamp, axis=AX.X, op=ALU.add),
              [B_lamp], [B_lam])
        fw.op("act", lambda e: e.activation(out=lamt[:, 0:2], in_=lamt[:, 0:2], func=AF.Exp), [B_lam], [B_lam])
        fw.op("dve", lambda e: e.scalar_tensor_tensor(out=lamt[:, 2:3], in0=lamt[:, 1:2], scalar=-LAM_INIT,
                                                      in1=lamt[:, 0:1], op0=ALU.add, op1=ALU.subtract),
              [B_lam], [B_lam])
        fw.op("dve", lambda e: e.tensor_scalar(out=subg[:], in0=subg[:], scalar1=1.0 - LAM_INIT, scalar2=None,
                                               op0=ALU.mult), [B_subg], [B_subg])
        fw.op("act", lambda e: e.activation(out=sc, in_=sc, func=AF.Silu), [B_sc], [B_sc])
        fw.op("dve", lambda e: e.memset(ones_t, 1.0), [], [B_ones])
        for j in range(6):
            wmj, B_wmj = (wm, B_wm) if j % 2 == 0 else (wm2, B_wm2)
            fw.dma("sp", wmj, wmod_d[:, j * D:(j + 1) * D].rearrange("(kc p) f -> p kc f", p=128),
                   [], [B_wmj], B_wmj)

            def mm_feat(e, wmj=wmj):
                ins = None
                for fc in range(DC):
                    for kc in range(DC):
                        ins = e.matmul(ps[0][:, fc * 8:(fc + 1) * 8], lhsT=wmj[:, kc, fc * 128:(fc + 1) * 128],
                                       rhs=sc[:, kc, :], start=(kc == 0), stop=(kc == DC - 1))
                return ins
            fw.op("pe", mm_feat, [B_wmj, B_sc], [PS[0]])
            fw.op("dve", lambda e, j=j: e.tensor_tensor(
                out=modT[:, j * DC:(j + 1) * DC, :],
                in0=ps[0][:, 0:64].rearrange("p (a b) -> p a b", b=8),
                in1=bmodT[:, j * DC:(j + 1) * DC, :].broadcast_to([128, DC, 8]), op=ALU.add),
                [PS[0], B_p0c], [B_modT], acc=True)
        fw.op("dve", lambda e: e.scalar_tensor_tensor(
            out=modA1[:], in0=modT[:, 1 * DC:2 * DC, :], scalar=1.0, in1=n1g.broadcast_to([128, DC, 8]),
            op0=ALU.add, op1=ALU.mult), [B_modT, B_p0c], [B_mod], acc=True)
        fw.op("dve", lambda e: e.tensor_copy(out=modB1[:], in_=modT[:, 0:DC, :]), [B_modT], [B_mod], acc=True)
        fw.op("dve", lambda e: e.scalar_tensor_tensor(
            out=modA2[:], in0=modT[:, 4 * DC:5 * DC, :], scalar=1.0, in1=n2g.broadcast_to([128, DC, 8]),
            op0=ALU.add, op1=ALU.mult), [B_modT, B_p0c], [B_mod], acc=True)
        fw.op("dve", lambda e: e.tensor_copy(out=modB2[:], in_=modT[:, 3 * DC:4 * DC, :]), [B_modT], [B_mod], acc=True)
        row_src = (lambda fc, b: modT[:, 2 * DC + fc, b:b + 1], lambda fc, b: modT[:, 5 * DC + fc, b:b + 1],
                   lambda fc, b: modB2[:, fc, b:b + 1], lambda fc, b: modA2[:, fc, b:b + 1])
        dgc = 0
        rbk = 0
        for which in range(4):
            for b in range(nb):
                for half in range(2):
                    bank = 1 + (rbk % 4)
                    rbk += 1
                    for q in range(4):
                        fc = half * 4 + q
                        dg, B_dg = dgt[dgc % 4]
                        dgc += 1
                        fw.op("dve", lambda e, dg=dg, v=row_src[which](fc, b): e.tensor_scalar(
                            out=dg, in0=ident[:], scalar1=v, scalar2=None, op0=ALU.mult),
                            [B_const, B_modT, B_mod], [B_dg])
                        fw.op("pe", lambda e, dg=dg, bank=bank, q=q: e.matmul(
                            ps[bank][:, q * 128:(q + 1) * 128], lhsT=ones_t, rhs=dg, start=True, stop=True),
                            [B_dg, B_ones], [PS[bank]])
                    if bank % 2 == 0:
                        fw.op("dve", lambda e, half=half, bank=bank: e.tensor_copy(out=gtmp[half], in_=ps[bank]),
                              [PS[bank]], [B_gtmp[half]])
                    else:
                        fw.op("act", lambda e, half=half, bank=bank: e.activation(out=gtmp[half], in_=ps[bank],
                                                                                  func=AF.Identity),
                              [PS[bank]], [B_gtmp[half]])
                    fw.dma("sp", gsc_s[b, which, :, half * 512:(half + 1) * 512], gtmp[half],
                           [B_gtmp[half]], [B_scr["gsc"]], B_gtmp[half], acc=True)

        dbgq = []

        def dump(name, ap, shape, dt, bufs):
            d = nc.dram_tensor("dbg_" + name, list(shape), dt, kind="ExternalOutput").ap()
            bb = Buf("dbg_" + name)
            fw.dma("sp", d, ap, bufs, [], bb)
            dbgq.append(bb)

        if dbg and stage == 0:
            for nm, t in (("modA1", modA1), ("modB1", modB1), ("modA2", modA2), ("modB2", modB2)):
                dump(nm, t[:], [128, DC, 8], F32, [B_mod])
            fw.wait_bufs("sp", B_gtmp)
            dump("gsc", gsc_s, [nb, 4, 128, D], F32, [B_scr["gsc"]])
            dump("w1s", w1_s[3], [128, DC, EH], BF16, [B_w1[3]])
            dump("lam", lamt[:], [128, 8], F32, [B_lam])
        RD.release(mD)
        RC.release(mC)
        RB.release(mB0)

        def rms_stats(src_ap, src_bufs, junk, B_junk, n):
            sm, B_sm = small_alloc()
            ss = sm[:, 0:1]
            rstd = sm[:, 1:2]
            fw.op("act", lambda e: e.activation(out=junk, in_=src_ap, func=AF.Square, accum_out=ss),
                  src_bufs, [B_junk, B_sm])
            fw.op("act", lambda e: e.activation(out=rstd, in_=ss, func=AF.Ln, scale=1.0 / n, bias=epst[:, 0:1]),
                  [B_sm, B_eps], [B_sm])
            fw.op("act", lambda e: e.activation(out=rstd, in_=rstd, func=AF.Exp, scale=-0.5), [B_sm], [B_sm])
            return rstd, B_sm, sm

        def evac_affine(eng, out_ap, in_ap, scale_ap, bias_ap, reads, writes):
            if eng == "dve":
                fw.op("dve", lambda e: e.tensor_scalar(out=out_ap, in0=in_ap, scalar1=scale_ap, scalar2=bias_ap,
                                                       op0=ALU.mult, op1=ALU.add), reads, writes, acc=True)
            else:
                fw.op("act", lambda e: e.activation(out=out_ap, in_=in_ap, func=AF.Identity, scale=scale_ap,
                                                    bias=bias_ap), reads, writes, acc=True)

        def evac_copy(eng, out_ap, in_ap, reads, writes):
            if eng == "dve":
                fw.op("dve", lambda e: e.tensor_copy(out=out_ap, in_=in_ap), reads, writes, acc=True)
            else:
                fw.op("act", lambda e: e.activation(out=out_ap, in_=in_ap, func=AF.Identity), reads, writes, acc=True)

        def beng(bank):
            return "dve" if bank % 2 == 0 else "act"

        for b in range(nb if stage > 0 else 0):
            ringA = Ring(ring_slots, 2)
            ringA.plan(wf_s, [128, DC, 512], B_scr["wf"])
            for h in range(NH):
                ringA.plan(wqkv_s[h], [128, DC, 384], B_scr["wqkv"])
            for cb in range(2):
                ringA.plan(wfo_s, [128, 4, D], B_scr["wfo"])
                ringA.plan(wg_s[2 + cb], [128, DC, 512], B_scr["wg"])
                ringA.plan(wg_s[cb], [128, DC, 512], B_scr["wg"])
                ringA.plan(wao_s[cb], [128, DC, 512], B_scr["wao"])
            ringA.plan(wo_s[0], [128, DC, 512], B_scr["wo"])
            ringA.plan(wo_s[1], [128, DC, 512], B_scr["wo"])

            mC = RC.mark()
            xt = []
            xn = []
            for i in range(2):
                xt.append(RC.alloc(f"xt{i}", [128, D], F32))
                xn.append(RC.alloc(f"xn{i}", [128, D], F32))
            tcount = 0

            def norm_tile(src_ap, A, Bm, bcol, HTbuf, tok0):
                nonlocal tcount
                s = tcount % 2
                tcount += 1
                xts, B_xts = xt[s]
                xns, B_xns = xn[s]
                fw.dma("sp", xts, src_ap, [], [B_xts], B_xts)
                rstd, B_sm, _ = rms_stats(xts, [B_xts], xns, B_xns, D)
                fw.op("act", lambda e: e.activation(out=xns, in_=xts, func=AF.Identity, scale=rstd),
                      [B_xts, B_sm], [B_xns])
                return lambda: norm_tile_b(s, xns, B_xns, A, Bm, bcol, HTbuf, tok0)

            def norm_tile_b(s, xns, B_xns, A, Bm, bcol, HTbuf, tok0):
                for half in range(2):
                    bank = 4 + 2 * s + half

                    def tr(e, half=half, bank=bank):
                        ins = None
                        for j in range(4):
                            dc = half * 4 + j
                            ins = e.transpose(ps[bank][:, j * 128:(j + 1) * 128], xns[:, dc * 128:(dc + 1) * 128],
                                              ident[:])
                        return ins
                    fw.op("pe", tr, [B_xns, B_const], [PS[bank]])
                    for j in range(4):
                        dc = half * 4 + j
                        evac_affine(beng(bank), HT[:, dc, tok0:tok0 + 128], ps[bank][:, j * 128:(j + 1) * 128],
                                    A[:, dc, bcol:bcol + 1], Bm[:, dc, bcol:bcol + 1], [PS[bank], B_mod], [HTbuf])

            p1args = [(ctx_d[b, t * 128:(t + 1) * 128, :], modA1, modB1, 4, B_HT[0], t * 128) for t in range(2)]
            p1args += [(x_d[b, t * 128:(t + 1) * 128, :], modA1, modB1, b, B_HT[1 + t // 4], CTX + t * 128)
                       for t in range(NT)]
            pend = norm_tile(*p1args[0])
            for t in range(len(p1args)):
                nxt_b = norm_tile(*p1args[t + 1]) if t + 1 < len(p1args) else None
                pend()
                pend = nxt_b
            if stage == 1:
                if dbg:
                    dump("HT", HT, [128, DC, NK], BF16, B_HT)
                break

            wf, B_wf = ringA.take()
            FT = [RC.alloc(f"FT{i}", [128, 4, 512], BF16) for i in range(2)]
            mB = RB.mark()
            ZT, B_ZT = RB.alloc("ZT", [128, 4, N], BF16)
            mB2 = RB.mark()
            AB, _ = RB.alloc("AB", [128, NT, 1024], BF16)
            B_AB = [RB.newbuf(f"AB{i}") for i in range(4)]
            mD = RD.mark()
            dring = [RD.alloc(f"dft{i}", [128, 4, 512], BF16) for i in range(4)]
            cdft = 0
            for tb in range(4):
                ft, B_ft = FT[tb % 2]
                for g in range(4):
                    fw.op("pe", mm_group(ps[g][:, :], [
                        (wf[:, kc, g * 128:(g + 1) * 128], HT[:, kc, CTX + tb * 512:CTX + (tb + 1) * 512])
                        for kc in range(DC)]), [B_wf, B_HT[1 + tb]], [PS[g]])
                    evac_copy(beng(g), ft[:, g, :], ps[g][:, :], [PS[g]], [B_ft])
                for tt in range(4):
                    tile = tb * 4 + tt
                    for gp in range(2):
                        bank = 4 + (cdft % 2)
                        cdft += 1

                        def cdf(e, gp=gp, tt=tt, bank=bank, ft=ft):
                            ins = None
                            for k in range(2):
                                ins = e.matmul(ps[bank][:, k * 256:(k + 1) * 256],
                                               lhsT=ft[:, 2 * gp + k, tt * 128:(tt + 1) * 128], rhs=csc[:, :],
                                               start=True, stop=True)
                            return ins
                        fw.op("pe", cdf, [B_ft, B_const], [PS[bank]])
                        evac_copy(beng(bank), AB[:, tile, gp * 512:(gp + 1) * 512], ps[bank][:, :], [PS[bank]], [B_AB[tb]])
            for mb in range(4):
                for piece in range(8):
                    kind, nq = piece // 4, piece % 4
                    dv, B_dv = dring[(mb * 8 + piece) % 4]
                    fw.dma("sp", dv, dft_d[kind, mb, nq], [], [B_dv], B_dv)

                    def sdf(e, piece=piece, kind=kind, nq=nq, dv=dv):
                        ins = None
                        for g in range(4):
                            for n_ in range(4):
                                ins = e.matmul(ps[g][:, :],
                                               lhsT=AB[:, nq * 4 + n_, g * 256 + kind * 128:g * 256 + (kind + 1) * 128],
                                               rhs=dv[:, n_, :], start=(piece == 0 and n_ == 0),
                                               stop=(piece == 7 and n_ == 3))
                        return ins
                    fw.op("pe", sdf, [B_dv] + B_AB, PS[0:4])
                for g in range(4):
                    evac_copy(beng(g), ZT[:, g, mb * 512:(mb + 1) * 512], ps[g][:, :], [PS[g]], [B_ZT])
            RD.release(mD)
            RB.release(mB2)
            RC.release(mC)
            if stage == 2:
                if dbg:
                    dump("ZT", ZT, [128, 4, N], BF16, [B_ZT])
                break

            mB2 = RB.mark()
            mC = RC.mark()
            mD = RD.mark()
            attnT, _ = RC.alloc("attnT", [128, DC, N], BF16)
            B_attnT = [RC.newbuf(f"attnT{i}") for i in range(4)]
            qkv = []
            for i in range(2):
                QT_, B_QT_ = RD.alloc(f"QT{i}", [128, N], BF16)
                KT_, B_KT_ = RD.alloc(f"KT{i}", [128, NK], BF16)
                VA_, B_VA_ = RD.alloc(f"VA{i}", [128, KC, 132], BF16)
                qkv.append((QT_, B_QT_, KT_, B_KT_, VA_, B_VA_))
                fw.op("pool", lambda e, VA_=VA_: e.memset(VA_[:, :, 128:129], 1.0), [], [B_VA_], acc=True)
            cossin, B_cs = RB.alloc("cossin", [128, 2, N], BF16)
            fw.dma("sp", cossin, cossin_d, [], [B_cs], B_cs)
            PT = [RB.alloc(f"PT{i}", [128, 1024], BF16) for i in range(2)]
            kraw = [RB.alloc(f"kraw{i}", [128, 512], BF16) for i in range(2)]
            t1 = [RB.alloc(f"t1_{i}", [128, 512], F32) for i in range(2)]
            t2 = [RB.alloc(f"t2_{i}", [128, 512], F32) for i in range(2)]
            ocp = [RB.alloc(f"ocp{i}", [128, 9, 160], F32) for i in range(2)]
            hd0 = [RB.alloc(f"hd0_{i}", [128, 4, 128], F32) for i in range(2)]
            hd1 = [RB.alloc(f"hd1_{i}", [128, 4, 128], F32) for i in range(2)]
            rope_ctr = 0
            pj_ctr = 0

            def inproj_units(h, wq, B_wq, standalone):
                QT, B_QT, KT, B_KT, VA, B_VA = qkv[h % 2]
                units = []

                def pbank():
                    nonlocal pj_ctr
                    pj_ctr += 1
                    if not standalone:
                        return 7
                    return pj_ctr % 4

                def kctx():
                    pb = pbank()
                    fw.op("pe", mm_group(ps[pb][:, 0:CTX], [(wq[:, kc, 128:256], HT[:, kc, 0:CTX]) for kc in range(DC)]),
                          [B_wq, B_HT[0]], [PS[pb]])
                    fw.op("dve", lambda e: e.tensor_copy(out=KT[:, 0:CTX], in_=ps[pb][:, 0:CTX]), [PS[pb]], [B_KT],
                          acc=True)
                units.append(kctx)

                def rope_pair(c0, dst_ap, dst_buf, tb):
                    st_ = {}

                    def ua():
                        nonlocal rope_ctr
                        r = rope_ctr % 2
                        rope_ctr += 1
                        st_["r"] = r
                        kr, B_kr = kraw[r]
                        pb = pbank()
                        fw.op("pe", mm_group(ps[pb], [
                            (wq[:, kc, c0:c0 + 128], HT[:, kc, CTX + tb * 512:CTX + (tb + 1) * 512])
                            for kc in range(DC)]), [B_wq, B_HT[1 + tb]], [PS[pb]])
                        fw.op("dve", lambda e: e.tensor_copy(out=kr, in_=ps[pb]), [PS[pb]], [B_kr])

                    def ub():
                        r = st_["r"]
                        kr, B_kr = kraw[r]
                        a1, B_a1 = t1[r]
                        a2, B_a2 = t2[r]
                        rb = pbank()
                        fw.op("pe", lambda e: e.matmul(ps[rb], lhsT=rt[:], rhs=kr, start=True, stop=True),
                              [B_kr, B_const], [PS[rb]])
                        fw.op("pool", lambda e: e.tensor_tensor(out=a1, in0=kr, in1=cossin[:, 0, tb * 512:(tb + 1) * 512],
                                                                op=ALU.mult), [B_kr, B_cs], [B_a1])
                        fw.op("dve", lambda e: e.tensor_tensor(out=a2, in0=ps[rb],
                                                               in1=cossin[:, 1, tb * 512:(tb + 1) * 512], op=ALU.mult),
                              [PS[rb], B_cs], [B_a2])
                        fw.op("pool", lambda e: e.tensor_tensor(out=dst_ap, in0=a1, in1=a2, op=ALU.add),
                              [B_a1, B_a2], [dst_buf], acc=True)
                    units.append(ua)
                    units.append(ub)

                for tb in range(4):
                    rope_pair(128, KT[:, CTX + tb * 512:CTX + (tb + 1) * 512], B_KT, tb)
                for tb in range(4):
                    rope_pair(0, QT[:, tb * 512:(tb + 1) * 512], B_QT, tb)

                def vunit(kc):
                    def u():
                        hb = B_HT[0] if kc < 2 else B_HT[1 + (kc - 2) // 4]
                        pb = pbank()
                        fw.op("pe", mm_group(ps[pb][:, 0:128], [
                            (HT[:, dc, kc * 128:(kc + 1) * 128], wq[:, dc, 256:384]) for dc in range(DC)]),
                            [B_wq, hb], [PS[pb]])
                        fw.op("dve", lambda e: e.tensor_copy(out=VA[:, kc, 0:128], in_=ps[pb][:, 0:128]),
                              [PS[pb]], [B_VA], acc=True)
                    return u
                for kc in range(KC):
                    units.append(vunit(kc))
                return units

            def ogrp(g):
                return 4 + g // 3, (g % 3) * 160

            NHR = int(os.environ.get('DBG_NH', NH))
            pending_fin = []
            wq0, B_wq0 = ringA.take()
            for u in inproj_units(0, wq0, B_wq0, True):
                u()
            for h in range(NHR):
                QT, B_QT, KT, B_KT, VA, B_VA = qkv[h % 2]
                nxt = []
                if h + 1 < NHR:
                    wqn, B_wqn = ringA.take()
                    nxt = inproj_units(h + 1, wqn, B_wqn, False)
                step = 0
                for qb in range(4):
                    q0 = qb * 512

                    def s_op(kc, q0=q0, QT=QT, KT=KT, B_QT=B_QT, B_KT=B_KT):
                        sb0 = 2 * (kc % 2)

                        def fn(e):
                            e.matmul(ps[sb0], lhsT=KT[0:64, kc * 128:(kc + 1) * 128],
                                     rhs=QT[0:64, q0:q0 + 512], start=True, stop=True)
                            return e.matmul(ps[sb0 + 1], lhsT=KT[64:128, kc * 128:(kc + 1) * 128],
                                            rhs=QT[64:128, q0:q0 + 512], start=True, stop=True)
                        fw.op("pe", fn, [B_KT, B_QT], [PS[sb0], PS[sb0 + 1]])

                    def av_op(kc, VA=VA, B_VA=B_VA):
                        pt, B_pt = PT[kc % 2]

                        def av(e):
                            ins = None
                            for g in range(8):
                                bank, c0 = ogrp(g)
                                i, qt = g // 4, g % 4
                                ins = e.matmul(ps[bank][:, c0:c0 + 129],
                                               lhsT=pt[:, i * 512 + qt * 128:i * 512 + (qt + 1) * 128],
                                               rhs=VA[:, kc, 0:129], start=(kc == 0 and g % 3 == 0),
                                               stop=(kc == KC - 1), skip_group_check=True)
                            return ins
                        fw.op("pe", av, [B_pt, B_VA], PS[4:7])

                    s_op(0)
                    for kc in range(KC):
                        if kc + 1 < KC:
                            s_op(kc + 1)
                        sb0 = 2 * (kc % 2)
                        pt, B_pt = PT[kc % 2]
                        fw.op("act", lambda e, pt=pt, sb0=sb0: e.activation(
                            out=pt, in_=psbig[:, sb0 * 512:sb0 * 512 + 1024], func=AF.Exp, scale=ATTN_SCALE),
                            [PS[sb0], PS[sb0 + 1]], [B_pt])
                        if nxt and (step % 2 == 1):
                            nxt.pop(0)()
                        step += 1
                        av_op(kc)
                        if kc == 8 and pending_fin:
                            if nxt and getattr(nxt[0], "second_half", False):
                                nxt.pop(0)()
                            pending_fin.pop(0)()
                    def make_fin(qb=qb, q0=q0, h=h):
                        oc, B_oc = ocp[qb % 2]
                        h0, B_h0 = hd0[qb % 2]
                        h1, B_h1 = hd1[qb % 2]
                        sm, B_sm = small_alloc()

                        def fa():
                            for k in range(3):
                                ng = 3 if k < 2 else 2
                                fw.op("dve", lambda e, k=k, ng=ng: e.tensor_copy(
                                    out=oc[:, 3 * k:3 * k + ng, :],
                                    in_=ps[4 + k][:, 0:ng * 160].rearrange("p (a b) -> p a b", b=160)),
                                    [PS[4 + k]], [B_oc], acc=True)
                            fw.op("dve", lambda e: e.reciprocal(out=sm[:, 0:8].unsqueeze(2), in_=oc[:, 0:8, 128:129]),
                                  [B_oc], [B_sm])
                            fw.op("dve", lambda e: e.tensor_scalar(out=sm[:, 8:12], in0=sm[:, 4:8], scalar1=lamt[:, 2:3],
                                                                   scalar2=None, op0=ALU.mult), [B_sm, B_lam], [B_sm])
                            fw.op("dve", lambda e: e.tensor_tensor(
                                out=h0, in0=oc[:, 0:4, 0:128], in1=sm[:, 0:4].unsqueeze(2).broadcast_to([128, 4, 128]),
                                op=ALU.mult), [B_oc, B_sm], [B_h0])
                            fw.op("dve", lambda e: e.tensor_tensor(
                                out=h1, in0=oc[:, 4:8, 0:128], in1=sm[:, 8:12].unsqueeze(2).broadcast_to([128, 4, 128]),
                                op=ALU.mult), [B_oc, B_sm], [B_h1])
                            fw.op("pool", lambda e: e.tensor_tensor(out=h1, in0=h0, in1=h1, op=ALU.add),
                                  [B_h0, B_h1], [B_h1])
                            fw.op("dve", lambda e: e.tensor_tensor(out=h0, in0=h1, in1=h1, op=ALU.mult),
                                  [B_h1], [B_h0])
                            fw.op("dve", lambda e: e.tensor_reduce(out=sm[:, 12:16], in_=h0, axis=AX.X, op=ALU.add),
                                  [B_h0, B_sm], [B_sm])

                        def fbc():
                            fw.op("act", lambda e: e.activation(out=sm[:, 12:16], in_=sm[:, 12:16], func=AF.Ln,
                                                                scale=1.0 / 128, bias=epst[:, 0:1]),
                                  [B_sm, B_eps], [B_sm])
                            fw.op("act", lambda e: e.activation(out=sm[:, 12:16], in_=sm[:, 12:16], func=AF.Exp,
                                                                scale=-0.5), [B_sm], [B_sm])
                            fw.op("dve", lambda e: e.tensor_tensor(
                                out=h0, in0=h1, in1=sm[:, 12:16].unsqueeze(2).broadcast_to([128, 4, 128]), op=ALU.mult),
                                [B_h1, B_sm], [B_h0])
                            fw.op("pool", lambda e: e.tensor_tensor(
                                out=h1, in0=h0, in1=subg[:].unsqueeze(1).broadcast_to([128, 4, 128]), op=ALU.mult),
                                [B_h0, B_subg], [B_h1])

                            def trf(e):
                                ins = None
                                for qt in range(4):
                                    ins = e.transpose(ps[7][:, qt * 128:(qt + 1) * 128], h1[:, qt, :], ident[:])
                                return ins
                            fw.op("pe", trf, [B_h1, B_const], [PS[7]])
                            fw.op("dve", lambda e: e.tensor_copy(out=attnT[:, h, q0:q0 + 512], in_=ps[7]),
                                  [PS[7]], [B_attnT[qb]])
                        return fa, fbc
                    fa_, fbc_ = make_fin()
                    fa_()
                    pending_fin.append(fbc_)
                while nxt:
                    nxt.pop(0)()
            while pending_fin:
                pending_fin.pop(0)()
            RD.release(mD)
            RB.release(mB2)
            if stage == 3:
                if dbg:
                    dump("attnT", attnT, [128, DC, N], BF16, B_attnT)
                break

            mB2 = RB.mark()
            mD = RD.mark()
            YT, _ = RD.alloc("YT", [128, DC, N], BF16)
            B_YT = [RD.newbuf(f"YT{i}") for i in range(4)]
            sgt = [RB.alloc(f"sg{i}", [128, 512], BF16) for i in range(2)]
            tt_ = [RB.alloc(f"tt{i}", [128, 512], BF16) for i in range(2)]
            cnt4 = 0
            for cb in range(2):
                wfo, B_wfo = ringA.take()
                wgf, B_wgf = ringA.take()
                for dcl in range(4):
                    dc = cb * 4 + dcl
                    for tb in range(4):
                        bg = 2 * (cnt4 % 2)
                        bf_ = bg + 1
                        sg, B_sg = sgt[cnt4 % 2]
                        cnt4 += 1
                        fw.op("pe", mm_group(ps[bg][:, :], [
                            (wgf[:, kc, dcl * 128:(dcl + 1) * 128], HT[:, kc, CTX + tb * 512:CTX + (tb + 1) * 512])
                            for kc in range(DC)]), [B_wgf, B_HT[1 + tb]], [PS[bg]])
                        fw.op("pe", mm_group(ps[bf_][:, :], [
                            (wfo[:, g, dc * 128:(dc + 1) * 128], ZT[:, g, tb * 512:(tb + 1) * 512])
                            for g in range(4)]), [B_wfo, B_ZT], [PS[bf_]])
                        fw.op("act", lambda e, sg=sg, bg=bg: e.activation(out=sg, in_=ps[bg][:, :], func=AF.Sigmoid),
                              [PS[bg]], [B_sg])
                        fw.op("dve", lambda e, sg=sg, bf_=bf_, dc=dc, tb=tb: e.tensor_tensor(
                            out=YT[:, dc, tb * 512:(tb + 1) * 512], in0=ps[bf_][:, :], in1=sg, op=ALU.mult),
                            [PS[bf_], B_sg], [B_YT[tb]], acc=True)
                wga, B_wga = ringA.take()
                wao, B_wao = ringA.take()
                for dcl in range(4):
                    dc = cb * 4 + dcl
                    for tb in range(4):
                        bg = 4 + 2 * (cnt4 % 2)
                        ba = bg + 1
                        sg, B_sg = sgt[cnt4 % 2]
                        tq, B_tq = tt_[cnt4 % 2]
                        cnt4 += 1
                        fw.op("pe", mm_group(ps[bg][:, :], [
                            (wga[:, kc, dcl * 128:(dcl + 1) * 128], HT[:, kc, CTX + tb * 512:CTX + (tb + 1) * 512])
                            for kc in range(DC)]), [B_wga, B_HT[1 + tb]], [PS[bg]])
                        fw.op("pe", mm_group(ps[ba][:, :], [
                            (wao[:, kc, dcl * 128:(dcl + 1) * 128], attnT[:, kc, tb * 512:(tb + 1) * 512])
                            for kc in range(DC)]), [B_wao, B_attnT[tb]], [PS[ba]])
                        fw.op("act", lambda e, sg=sg, bg=bg: e.activation(out=sg, in_=ps[bg][:, :], func=AF.Sigmoid),
                              [PS[bg]], [B_sg])
                        fw.op("dve", lambda e, sg=sg, ba=ba, tq=tq: e.tensor_tensor(
                            out=tq, in0=ps[ba][:, :], in1=sg, op=ALU.mult), [PS[ba], B_sg], [B_tq])
                        fw.op("pool", lambda e, tq=tq, dc=dc, tb=tb: e.tensor_tensor(
                            out=YT[:, dc, tb * 512:(tb + 1) * 512], in0=YT[:, dc, tb * 512:(tb + 1) * 512], in1=tq,
                            op=ALU.add), [B_tq, B_YT[tb]], [B_YT[tb]])
            RB.release(mB2)
            RB.release(mB)
            RC.release(mC)
            if stage == 4:
                if dbg:
                    dump("YT", YT, [128, DC, N], BF16, B_YT)
                break

            mB = RB.mark()
            mC = RC.mark()
            x1r = [RB.alloc(f"x1r{i}", [128, D], F32) for i in range(2)]
            h2r = [RB.alloc(f"h2r{i}", [128, D], BF16) for i in range(2)]
            h2tmp, B_h2tmp = RB.alloc("h2tmp", [128, D], F32)
            A2bc, B_a2bc = RB.alloc("A2bc", [128, D], F32)
            B2bc, _ = RB.alloc("B2bc", [128, D], F32)
            fw.dma("sp", B2bc, gsc_s[b, 2], [B_scr["gsc"]], [B_a2bc], B_a2bc)
            fw.dma("sp", A2bc, gsc_s[b, 3], [B_scr["gsc"]], [B_a2bc], B_a2bc)
            wo0, B_wo0 = ringA.take()
            wo1, B_wo1 = ringA.take()
            wo = [wo0, wo1]
            mC1 = RC.mark()
            g1bc, B_g1 = RC.alloc("g1bc", [128, D], F32)
            xt2 = [RC.alloc(f"xt2_{i}", [128, D], F32) for i in range(2)]
            tmp2, B_tmp2 = RC.alloc("tmp2", [128, D], F32)
            xn2 = [RC.alloc(f"xn2_{i}", [128, D], F32) for i in range(2)]
            h2f, B_h2f = RC.alloc("h2f", [128, DC, 128], F32)
            wr, B_wr = RC.alloc("wr", [128, DC, 20], F32)
            brt, _ = RC.alloc("br", [128, 20], F32)
            LG, B_LG = RB.alloc("LG", [128, NT, 20], F32)
            RV, B_RW = RB.alloc("RV", [128, 7, NT], F32)
            RW3, _ = RB.alloc("RW3", [128, 7 * NT, 4], F32)
            RW3 = RW3.rearrange("p (i t) e -> p i t e", t=NT)
            RW4, _ = RB.alloc("RW4", [128, NT * 4, 4], F32)
            RW4 = RW4.rearrange("p (t g) e -> p t g e", g=4)
            fw.dma("sp", g1bc, gsc_s[b, 0], [B_scr["gsc"]], [B_g1], B_g1)
            fw.dma("sp", wr, wr_d, [], [B_wr], B_wr)
            fw.dma("sp", brt, br_d, [], [B_wr], B_wr)
            def stageA(tt, b=b):
                s = tt % 2
                xts, B_xts = xt2[s]
                xns, B_xns = xn2[s]
                fw.dma("sp", xts, x_d[b, tt * 128:(tt + 1) * 128, :], [], [B_xts], B_xts)
                for nb_ in range(2):
                    bank = 2 * s + nb_
                    fw.op("pe", mm_group(ps[bank][:, :], [
                        (YT[:, kc, tt * 128:(tt + 1) * 128], wo[nb_][:, kc, :]) for kc in range(DC)]),
                        [B_YT[tt // 4], B_wo0, B_wo1], [PS[bank]])
                    fw.op("dve", lambda e, bank=bank, nb_=nb_: e.tensor_tensor(
                        out=tmp2[:, nb_ * 512:(nb_ + 1) * 512], in0=ps[bank][:, :],
                        in1=g1bc[:, nb_ * 512:(nb_ + 1) * 512], op=ALU.mult), [PS[bank], B_g1], [B_tmp2], acc=True)
                x1t, B_x1t = x1r[s]
                h2t, B_h2t = h2r[s]
                fw.op("pool", lambda e, x1t=x1t, xts=xts: e.tensor_tensor(out=x1t, in0=tmp2, in1=xts, op=ALU.add),
                      [B_tmp2, B_xts], [B_x1t])
                fw.dma("sp", x1_s[b * N + tt * 128:b * N + (tt + 1) * 128, :], x1t, [B_x1t], [B_x1s], B_x1t, acc=True)
                rstd, B_sm, _ = rms_stats(x1t, [B_x1t], xns, B_xns, D)
                fw.op("act", lambda e, x1t=x1t, xns=xns, rstd=rstd: e.activation(
                    out=xns, in_=x1t, func=AF.Identity, scale=rstd), [B_x1t, B_sm], [B_xns])
                fw.op("pool", lambda e, xns=xns: e.tensor_tensor(out=h2tmp, in0=xns, in1=A2bc, op=ALU.mult),
                      [B_xns, B_a2bc], [B_h2tmp])
                fw.op("pool", lambda e, h2t=h2t: e.tensor_tensor(out=h2t, in0=h2tmp, in1=B2bc, op=ALU.add),
                      [B_h2tmp, B_a2bc], [B_h2t])
                fw.dma("sp", h2_s[b * N + tt * 128:b * N + (tt + 1) * 128, :], h2t, [B_h2t], [B_h2s], B_h2t, acc=True)

            def stageB(tt, b=b):
                s = tt % 2
                xns, B_xns = xn2[s]
                for half in range(2):
                    bank = 4 + half

                    def tr(e, half=half, bank=bank, xns=xns):
                        ins = None
                        for j in range(4):
                            dc = half * 4 + j
                            ins = e.transpose(ps[bank][:, j * 128:(j + 1) * 128], xns[:, dc * 128:(dc + 1) * 128],
                                              ident[:])
                        return ins
                    fw.op("pe", tr, [B_xns, B_const], [PS[bank]])
                    for j in range(4):
                        dc = half * 4 + j
                        fw.op("dve", lambda e, dc=dc, j=j, bank=bank, b=b: e.tensor_scalar(
                            out=h2f[:, dc, :], in0=ps[bank][:, j * 128:(j + 1) * 128],
                            scalar1=modA2[:, dc, b:b + 1], scalar2=modB2[:, dc, b:b + 1],
                            op0=ALU.mult, op1=ALU.add), [PS[bank], B_mod], [B_h2f], acc=True)
                fw.op("pe", mm_group(ps[6][:, 0:20], [(h2f[:, kc, :], wr[:, kc, :]) for kc in range(DC)]),
                      [B_h2f, B_wr], [PS[6]])
                fw.op("dve", lambda e, tt=tt: e.tensor_tensor(out=LG[:, tt, :], in0=ps[6][:, 0:20], in1=brt,
                                                              op=ALU.add), [PS[6], B_wr], [B_LG], acc=True)

            stageA(0)
            for tt in range(NT):
                if tt + 1 < NT:
                    stageA(tt + 1)
                stageB(tt)
            lg = LG[:, :, 0:4]
            le4 = LG[:, :, 4:20].rearrange("p t (g e) -> p t g e", e=4)
            T3 = [128, NT, 4]
            T4 = [128, NT, 4, 4]

            def RR(fn):
                fw.op("dve", fn, [B_LG, B_RW], [B_RW])

            def bc3(v):
                return v.unsqueeze(2).broadcast_to(T3)
            gmax, wgrp, m1, m2, dd, p1, p2 = (RV[:, i, :] for i in range(7))
            ohg, dlg, leg, oh1, leg2, oh2, gi = (RW3[:, i, :, :] for i in range(7))
            RR(lambda e: e.tensor_reduce(out=gmax, in_=lg, axis=AX.X, op=ALU.max))
            RR(lambda e: e.tensor_tensor(out=ohg, in0=lg, in1=bc3(gmax), op=ALU.is_equal))
            RR(lambda e: e.tensor_tensor(out=dlg, in0=lg, in1=bc3(gmax), op=ALU.subtract))
            fw.op("act", lambda e: e.activation(out=dlg, in_=dlg, func=AF.Exp), [B_RW], [B_RW])
            RR(lambda e: e.tensor_reduce(out=wgrp, in_=dlg, axis=AX.X, op=ALU.add))
            RR(lambda e: e.reciprocal(out=wgrp, in_=wgrp))
            RR(lambda e: e.tensor_tensor(out=RW4, in0=le4, in1=ohg.unsqueeze(3).broadcast_to(T4), op=ALU.mult))
            RR(lambda e: e.tensor_reduce(out=leg, in_=RW4.rearrange("p t g e -> p t e g"), axis=AX.X, op=ALU.add))
            RR(lambda e: e.tensor_reduce(out=m1, in_=leg, axis=AX.X, op=ALU.max))
            RR(lambda e: e.tensor_tensor(out=oh1, in0=leg, in1=bc3(m1), op=ALU.is_equal))
            RR(lambda e: e.scalar_tensor_tensor(out=leg2, in0=oh1, scalar=-1e30, in1=leg, op0=ALU.mult, op1=ALU.add))
            RR(lambda e: e.tensor_reduce(out=m2, in_=leg2, axis=AX.X, op=ALU.max))
            RR(lambda e: e.tensor_tensor(out=oh2, in0=leg2, in1=bc3(m2), op=ALU.is_equal))
            RR(lambda e: e.tensor_tensor(out=dd, in0=m2, in1=m1, op=ALU.subtract))
            fw.op("act", lambda e: e.activation(out=dd, in_=dd, func=AF.Exp), [B_RW], [B_RW])
            RR(lambda e: e.tensor_scalar(out=p1, in0=dd, scalar1=1.0, scalar2=None, op0=ALU.add))
            RR(lambda e: e.reciprocal(out=p1, in_=p1))
            RR(lambda e: e.tensor_tensor(out=p2, in0=dd, in1=p1, op=ALU.mult))
            RR(lambda e: e.tensor_tensor(out=p1, in0=p1, in1=wgrp, op=ALU.mult))
            RR(lambda e: e.tensor_tensor(out=p2, in0=p2, in1=wgrp, op=ALU.mult))
            RR(lambda e: e.tensor_tensor(out=gi, in0=oh1, in1=bc3(p1), op=ALU.mult))
            RR(lambda e: e.tensor_tensor(out=oh2, in0=oh2, in1=bc3(p2), op=ALU.mult))
            RR(lambda e: e.tensor_tensor(out=gi, in0=gi, in1=oh2, op=ALU.add))
            fw.op("dve", lambda e, b=b: e.tensor_tensor(
                out=Gall[:, b * NT:(b + 1) * NT, :].rearrange("p t (g e) -> p t g e", e=4),
                in0=gi.unsqueeze(2).broadcast_to(T4), in1=ohg.unsqueeze(3).broadcast_to(T4), op=ALU.mult),
                [B_RW], [B_G], acc=True)
            RD.release(mD)
            RC.release(mC1)
            RC.release(mC)
            RB.release(mB)

        if stage >= 5:
            JJ = J * 16
            mC = RC.mark()
            mB = RB.mark()
            mD = RD.mark()
            ustr, B_sc0 = RC.alloc("ustr", [128, 128], F32)
            onesm, _ = RC.alloc("onesm", [128, 128], F32)
            thr16, _ = RC.alloc("thr16", [128, 16], F32)
            thr48, _ = RC.alloc("thr48", [128, 48], F32)
            ltm, _ = RC.alloc("ltm", [128, 16, 16], F32)
            fw.dma("sp", ustr, ustrict_d, [], [B_sc0], B_sc0)
            fw.dma("sp", thr16, thr16_d, [], [B_sc0], B_sc0)
            fw.dma("sp", thr48, thr48_d, [], [B_sc0], B_sc0)
            fw.dma("sp", ltm, ltmask_d, [], [B_sc0], B_sc0)
            B_on = Buf("onesm")
            fw.op("dve", lambda e: e.memset(onesm, 1.0), [], [B_on])
            Mt, B_M = RB.alloc("Mt", [128, J, 16], F32)
            rank, B_rank = RB.alloc("rank", [128, J, 16], F32)
            tot, B_tot = RB.alloc("tot", [128, J, 16], F32)
            cum, B_cum = RB.alloc("cum", [128, J, 16], F32)
            smt, B_smt = RB.alloc("smt", [128, J, 16], F32)
            s48, B_s48 = RB.alloc("s48", [128, 48, 16], F32)
            s16, B_s16 = RB.alloc("s16", [128, 16, 16], F32)
            vec, B_vec = RC.alloc("vec", [128, 8, 16], F32)
            slots, B_slots = RC.alloc("slots", [128, 6, J], F32)
            sloti, B_sloti = RC.alloc("sloti", [128, 2, J], mybir.dt.int32)
            texpf, B_texp = RC.alloc("texpf", [128, 48], F32)
            texpi, _ = RC.alloc("texpi", [128, 48], mybir.dt.int32)
            pidx, _ = RC.alloc("pidx", [128, 1], F32)
            fw.dma("sp", pidx, pidx_d, [], [B_sc0], B_sc0)
            Mf = Mt.rearrange("p j e -> p (j e)")
            fw.op("dve", lambda e: e.tensor_single_scalar(out=Mt, in_=Gall[:], scalar=0.0, op=ALU.is_gt),
                  [B_G], [B_M])
            for cbk in range((JJ + 511) // 512):
                c0, c1 = cbk * 512, min(JJ, (cbk + 1) * 512)
                fw.op("pe", lambda e, c0=c0, c1=c1: e.matmul(ps[0][:, 0:c1 - c0], lhsT=ustr, rhs=Mf[:, c0:c1],
                                                             start=True, stop=True), [B_M, B_sc0], [PS[0]])
                fw.op("pe", lambda e, c0=c0, c1=c1: e.matmul(ps[1][:, 0:c1 - c0], lhsT=onesm, rhs=Mf[:, c0:c1],
                                                             start=True, stop=True), [B_M, B_on], [PS[1]])
                fw.op("dve", lambda e, c0=c0, c1=c1: e.tensor_copy(
                    out=rank.rearrange("p j e -> p (j e)")[:, c0:c1], in_=ps[0][:, 0:c1 - c0]), [PS[0]], [B_rank],
                    acc=True)
                fw.op("dve", lambda e, c0=c0, c1=c1: e.tensor_copy(
                    out=tot.rearrange("p j e -> p (j e)")[:, c0:c1], in_=ps[1][:, 0:c1 - c0]), [PS[1]], [B_tot],
                    acc=True)
            fw.op("dve", lambda e: e.memset(cum[:, 0, :], 0.0), [], [B_cum])
            for j in range(1, J):
                fw.op("dve", lambda e, j=j: e.tensor_tensor(out=cum[:, j, :], in0=cum[:, j - 1, :], in1=tot[:, j - 1, :],
                                                            op=ALU.add), [B_cum, B_tot], [B_cum])
            cnt = vec[:, 0, :]
            ntl = vec[:, 1, :]
            off = vec[:, 2, :]
            fw.op("dve", lambda e: e.tensor_tensor(out=cnt, in0=cum[:, J - 1, :], in1=tot[:, J - 1, :], op=ALU.add),
                  [B_cum, B_tot], [B_vec])
            fw.op("dve", lambda e: e.tensor_tensor(out=s16, in0=cnt.unsqueeze(2).broadcast_to([128, 16, 16]),
                                                   in1=thr16.unsqueeze(1).broadcast_to([128, 16, 16]), op=ALU.is_gt),
                  [B_vec, B_sc0], [B_s16])
            fw.op("dve", lambda e: e.tensor_reduce(out=ntl, in_=s16, axis=AX.X, op=ALU.add), [B_s16, B_vec], [B_vec])
            fw.op("dve", lambda e: e.tensor_scalar(out=ntl, in0=ntl, scalar1=512.0, scalar2=None, op0=ALU.mult),
                  [B_vec], [B_vec])
            fw.op("dve", lambda e: e.tensor_tensor(out=s16, in0=ltm, in1=ntl.unsqueeze(1).broadcast_to([128, 16, 16]),
                                                   op=ALU.mult), [B_vec, B_sc0, B_s16], [B_s16])
            fw.op("dve", lambda e: e.tensor_reduce(out=off, in_=s16, axis=AX.X, op=ALU.add), [B_s16, B_vec], [B_vec])
            fw.op("dve", lambda e: e.tensor_tensor(out=rank, in0=rank, in1=cum, op=ALU.add), [B_rank, B_cum], [B_rank])
            fw.op("dve", lambda e: e.tensor_tensor(out=rank, in0=rank, in1=off.unsqueeze(1).broadcast_to([128, J, 16]),
                                                   op=ALU.add), [B_rank, B_vec], [B_rank])
            fw.op("dve", lambda e: e.tensor_tensor(out=smt, in0=rank, in1=Mt, op=ALU.mult), [B_rank, B_M], [B_smt])
            fw.op("dve", lambda e: e.tensor_reduce(out=slots[:, 0, :], in_=smt, axis=AX.X, op=ALU.add),
                  [B_smt], [B_slots])
            fw.op("dve", lambda e: e.tensor_reduce(out=slots[:, 1, :], in_=smt, axis=AX.X, op=ALU.max),
                  [B_smt, B_slots], [B_slots])
            fw.op("dve", lambda e: e.tensor_tensor(out=slots[:, 2, :], in0=slots[:, 0, :], in1=slots[:, 1, :],
                                                   op=ALU.subtract), [B_slots], [B_slots])
            fw.op("dve", lambda e: e.tensor_tensor(out=cum, in0=smt,
                                                   in1=slots[:, 1, :].unsqueeze(2).broadcast_to([128, J, 16]),
                                                   op=ALU.is_equal), [B_smt, B_slots, B_cum], [B_cum])
            fw.op("dve", lambda e: e.tensor_tensor(out=cum, in0=cum, in1=Gall[:], op=ALU.mult), [B_cum, B_G], [B_cum])
            fw.op("dve", lambda e: e.tensor_reduce(out=slots[:, 4, :], in_=cum, axis=AX.X, op=ALU.add),
                  [B_cum, B_slots], [B_slots])
            fw.op("dve", lambda e: e.tensor_reduce(out=slots[:, 3, :], in_=Gall[:], axis=AX.X, op=ALU.add),
                  [B_G, B_slots], [B_slots])
            fw.op("dve", lambda e: e.tensor_tensor(out=slots[:, 5, :], in0=slots[:, 3, :], in1=slots[:, 4, :],
                                                   op=ALU.subtract), [B_slots], [B_slots])
            fw.op("dve", lambda e: e.tensor_copy(out=sloti, in_=slots[:, 1:3, :]), [B_slots], [B_sloti])
            fw.op("dve", lambda e: e.tensor_tensor(out=s48, in0=off.unsqueeze(1).broadcast_to([128, 48, 16]),
                                                   in1=thr48.unsqueeze(2).broadcast_to([128, 48, 16]), op=ALU.is_le),
                  [B_vec, B_sc0], [B_s48])
            fw.op("dve", lambda e: e.tensor_reduce(out=texpf, in_=s48, axis=AX.X, op=ALU.add), [B_s48], [B_texp])
            fw.op("dve", lambda e: e.tensor_scalar(out=texpf, in0=texpf, scalar1=-1.0, scalar2=0.0, op0=ALU.add,
                                                   op1=ALU.max), [B_texp], [B_texp])
            fw.op("dve", lambda e: e.tensor_scalar(out=texpf, in0=texpf, scalar1=15.0, scalar2=None, op0=ALU.min),
                  [B_texp], [B_texp])
            fw.op("dve", lambda e: e.tensor_scalar(out=texpf, in0=texpf, scalar1=128.0, scalar2=pidx[:, 0:1],
                                                   op0=ALU.mult, op1=ALU.add), [B_texp, B_sc0], [B_texp])
            fw.op("dve", lambda e: e.tensor_copy(out=texpi, in_=texpf), [B_texp], [B_texp])
            if dbg and stage == 5:
                dump("G", Gall[:], [128, J, 16], F32, [B_G])
                dump("slots", slots, [128, 6, J], F32, [B_slots])
                dump("sloti", sloti, [128, 2, J], mybir.dt.int32, [B_sloti])
                dump("texpi", texpi, [128, 48], mybir.dt.int32, [B_texp])
                dump("vec", vec, [128, 8, 16], F32, [B_vec])
        if stage >= 6:
            RB.release(mB)
            mB = RB.mark()
            B_hs = Buf("hs_s")
            B_ys = Buf("ys_s")
            h2l = [RB.alloc(f"h2l{i}", [128, D], BF16) for i in range(4)]
            for j in range(J):
                hv, B_hv = h2l[j % 4]
                fw.dma("sp", hv, h2_s[j * 128:(j + 1) * 128, :], [B_h2s], [B_hv], B_hv)
                for k in range(2):
                    fw.dma_fn("pool", lambda e, hv=hv, j=j, k=k: e.indirect_dma_start(
                        out=hs_s[:, :], out_offset=bass.IndirectOffsetOnAxis(ap=sloti[:, k, j:j + 1], axis=0),
                        in_=hv, in_offset=None), [B_hv, B_sloti], [B_hs], B_hv, acc=True)
            RB.release(mB)
            mB = RB.mark()
            mslots = list(ring_slots)
            for i in range(4):
                v, bb = RD.alloc(f"mring{i}", [128, 4096], BF16)
                mslots.append((v, bb))
            hsl = [RB.alloc(f"hsl{i}", [128, 4, D], BF16) for i in range(2)]
            hsT = [RB.alloc(f"hsT{i}", [128, DC, 512], BF16) for i in range(2)]
            gT = [RB.alloc(f"gT{i}", [128, 4, 512], BF16) for i in range(2)]
            sil = [RB.alloc(f"sil{i}", [128, 512], BF16) for i in range(2)]
            ysb = [RB.alloc(f"ysb{i}", [128, 4, D], BF16) for i in range(2)]
            identb, B_idb = RC.alloc("identb", [128, 128], BF16)
            fw.op("dve", lambda e: e.tensor_copy(out=identb, in_=ident[:]), [B_const], [B_idb])
            psb = [ps[i].bitcast(BF16) for i in range(8)]
            wsl = 0
            c5 = 0
            cy = 0
            NSTr = int(os.environ.get("DBG_NST", NST))
            def s4_fetch(s_):
                nonlocal wsl
                wv = []
                for wi, (wsrc, shape) in enumerate(((w1_s, [128, DC, EH]), (w3_s, [128, DC, EH]), (w2_s, [128, 4, D]))):
                    ap_, bf_ = mslots[wsl % 8]
                    wsl += 1
                    v = chunk_view(ap_, shape)
                    rows = wsrc.rearrange("e p a b -> (e p) (a b)")
                    fw.dma_fn("pool", lambda e, s_=s_, rows=rows, ap_=ap_: e.indirect_dma_start(
                        out=ap_, out_offset=None, in_=rows,
                        in_offset=bass.IndirectOffsetOnAxis(ap=texpi[:, s_:s_ + 1], axis=0)),
                        [B_texp] + B_w1, [bf_], bf_)
                    wv.append((v, bf_))
                hl, B_hl = hsl[s_ % 2]
                fw.dma("sp", hl, hs_s[s_ * 512:(s_ + 1) * 512, :].rearrange("(a p) d -> p a d", p=128),
                       [B_hs], [B_hl], B_hl)
                return wv

            fetched = {0: s4_fetch(0)}
            for s_ in range(NSTr):
                if s_ + 1 < NSTr:
                    fetched[s_ + 1] = s4_fetch(s_ + 1)
                (w1, B_w1c), (w3, B_w3c), (w2, B_w2c) = fetched.pop(s_)
                hl, B_hl = hsl[s_ % 2]
                hT, B_hT = hsT[s_ % 2]
                gt, B_gt = gT[s_ % 2]
                yb_, B_yb = ysb[s_ % 2]
                for dc in range(DC):
                    bank = 6 + dc % 2

                    def trb(e, dc=dc, bank=bank, hl=hl):
                        ins = None
                        for a in range(4):
                            ins = e.transpose(psb[bank][:, a * 128:(a + 1) * 128], hl[:, a, dc * 128:(dc + 1) * 128],
                                              identb)
                        return ins
                    fw.op("pe", trb, [B_hl, B_idb], [PS[bank]])
                    evac_copy(beng(bank), hT[:, dc, :], psb[bank][:, 0:512], [PS[bank]], [B_hT])
                for hc in range(4):
                    b1 = 2 * (c5 % 2)
                    b3 = b1 + 1
                    sl, B_sl = sil[c5 % 2]
                    c5 += 1
                    fw.op("pe", mm_group(ps[b1], [(w1[:, kc, hc * 128:(hc + 1) * 128], hT[:, kc, :]) for kc in range(DC)]),
                          [B_w1c, B_hT], [PS[b1]])
                    fw.op("pe", mm_group(ps[b3], [(w3[:, kc, hc * 128:(hc + 1) * 128], hT[:, kc, :]) for kc in range(DC)]),
                          [B_w3c, B_hT], [PS[b3]])
                    fw.op("act", lambda e, sl=sl, b1=b1: e.activation(out=sl, in_=ps[b1], func=AF.Silu), [PS[b1]], [B_sl])
                    fw.op("dve", lambda e, sl=sl, b3=b3, gt=gt, hc=hc: e.tensor_tensor(
                        out=gt[:, hc, :], in0=ps[b3], in1=sl, op=ALU.mult), [PS[b3], B_sl], [B_gt], acc=True)
                for a in range(4):
                    for nb_ in range(2):
                        yb = 4 + (cy % 2)
                        cy += 1
                        fw.op("pe", mm_group(ps[yb], [
                            (gt[:, hc, a * 128:(a + 1) * 128], w2[:, hc, nb_ * 512:(nb_ + 1) * 512]) for hc in range(4)]),
                            [B_gt, B_w2c], [PS[yb]])
                        evac_copy(beng(yb), yb_[:, a, nb_ * 512:(nb_ + 1) * 512], ps[yb], [PS[yb]], [B_yb])
                fw.dma("sp", ys_s[s_ * 512:(s_ + 1) * 512, :].rearrange("(a p) d -> p a d", p=128), yb_,
                       [B_yb], [B_ys], B_yb, acc=True)
            RB.release(mB)
            RD.release(mD)
            mB = RB.mark()
            ya = [RB.alloc(f"ya{i}", [128, D], BF16) for i in range(2)]
            ybb = [RB.alloc(f"yb{i}", [128, D], BF16) for i in range(2)]
            x1l = [RB.alloc(f"x1l{i}", [128, D], F32) for i in range(2)]
            mt_ = [RB.alloc(f"mt{i}", [128, D], F32) for i in range(2)]
            ot = [RB.alloc(f"ot{i}", [128, D], F32) for i in range(2)]
            fing, B_fing = RB.alloc("fing", [128, D], F32)
            g2bc, B_g2 = RB.alloc("g2bc", [128, D], F32)
            fw.dma("sp", fing, fing_d, [], [B_fing], B_fing)
            g2r = [(g2bc, B_g2)] + [RB.alloc("g2bc1", [128, D], F32)]

            def s5_fetch(j):
                s = j % 2
                if j % NT == 0:
                    gv, B_gv = g2r[(j // NT) % 2]
                    fw.dma("sp", gv, gsc_s[j // NT, 1], [B_scr["gsc"]], [B_gv], B_gv)
                yav, B_ya = ya[s]
                ybv, B_ybv = ybb[s]
                xv, B_xv = x1l[s]
                fw.dma("sp", xv, x1_s[j * 128:(j + 1) * 128, :], [B_x1s], [B_xv], B_xv)
                fw.dma_fn("pool", lambda e, yav=yav, j=j: e.indirect_dma_start(
                    out=yav, out_offset=None, in_=ys_s[:, :],
                    in_offset=bass.IndirectOffsetOnAxis(ap=sloti[:, 0, j:j + 1], axis=0)),
                    [B_ys, B_sloti], [B_ya], B_ya)
                fw.dma_fn("pool", lambda e, ybv=ybv, j=j: e.indirect_dma_start(
                    out=ybv, out_offset=None, in_=ys_s[:, :],
                    in_offset=bass.IndirectOffsetOnAxis(ap=sloti[:, 1, j:j + 1], axis=0)),
                    [B_ys, B_sloti], [B_ybv], B_ybv)

            def s5_a(j):
                b = j // NT
                s = j % 2
                gv, B_gv = g2r[b % 2]
                yav, B_ya = ya[s]
                ybv, B_ybv = ybb[s]
                xv, B_xv = x1l[s]
                mv, B_mv = mt_[s]
                o_, B_o = ot[s]
                fw.op("dve", lambda e: e.tensor_scalar(
                    out=mv, in0=yav, scalar1=slots[:, 4, j:j + 1], scalar2=None, op0=ALU.mult),
                    [B_ya, B_slots], [B_mv])
                fw.op("dve", lambda e: e.scalar_tensor_tensor(
                    out=mv, in0=ybv, scalar=slots[:, 5, j:j + 1], in1=mv, op0=ALU.mult, op1=ALU.add),
                    [B_ybv, B_slots, B_mv], [B_mv])
                fw.op("dve", lambda e: e.tensor_tensor(out=mv, in0=mv, in1=gv, op=ALU.mult),
                      [B_mv, B_gv], [B_mv])
                fw.op("pool", lambda e: e.tensor_tensor(out=xv, in0=xv, in1=mv, op=ALU.add),
                      [B_mv, B_xv], [B_xv])
                rstd, B_sm, _ = rms_stats(xv, [B_xv], o_, B_o, D)

                def s5_b():
                    fw.op("dve", lambda e: e.scalar_tensor_tensor(
                        out=o_, in0=xv, scalar=rstd, in1=fing, op0=ALU.mult, op1=ALU.mult),
                        [B_xv, B_sm, B_fing], [B_o])
                    fw.dma("sp", out_d[b, (j % NT) * 128:(j % NT + 1) * 128, :], o_, [B_o], [B_out], B_o, acc=True)
                return s5_b

            s5_fetch(0)
            if J > 1:
                s5_fetch(1)
            pend_b = s5_a(0)
            for j in range(J):
                nxt_b = s5_a(j + 1) if j + 1 < J else None
                pend_b()
                if j + 2 < J:
                    s5_fetch(j + 2)
                pend_b = nxt_b
            RB.release(mB)

        fw._waits("sp", dict(fw.dma_events))
        fw.emit()
    return nc


def host_inputs(inputs, core, nb=4):
    f = np.float32
    cs = get_consts()
    b0 = core * nb
    m = {}
    m["x"] = np.ascontiguousarray(inputs["x"][b0:b0 + nb], dtype=f)
    m["ctx"] = np.ascontiguousarray(inputs["ctx"][b0:b0 + nb], dtype=f)
    cT = np.zeros((128, DC, 8), f)
    cc = np.asarray(inputs["c"][b0:b0 + nb], dtype=f)
    cT[:, :, :nb] = cc.reshape(nb, DC, 128).transpose(2, 1, 0)
    cT[:, :, 4] = np.asarray(inputs["c_ctx"], dtype=f).reshape(DC, 128).T
    m["cT"] = cT
    m["w_mod"] = np.ascontiguousarray(inputs["w_mod"][0], dtype=f)
    bm = np.asarray(inputs["b_mod"][0], dtype=f)
    m["bmodT"] = np.ascontiguousarray(bm.reshape(48, 128).T)
    m["bmg"] = np.ascontiguousarray(np.broadcast_to(np.concatenate([bm[2048:3072], bm[5120:6144]])[None, :], (128, 2048)))
    m["n1g"] = np.ascontiguousarray(np.asarray(inputs["norm1_g"][0], dtype=f).reshape(DC, 128).T)
    m["n2g"] = np.ascontiguousarray(np.asarray(inputs["norm2_g"][0], dtype=f).reshape(DC, 128).T)
    m["fing"] = np.ascontiguousarray(np.broadcast_to(np.asarray(inputs["final_g"], dtype=f)[None, :], (128, D)))
    lamv = np.concatenate([np.asarray(inputs[k][0], dtype=f) for k in ("lam_q1", "lam_k1", "lam_q2", "lam_k2")])
    m["lamv"] = np.ascontiguousarray(np.broadcast_to(lamv[None, :], (128, 256)))
    m["subg"] = np.ascontiguousarray(np.broadcast_to(np.asarray(inputs["subln_g"][0], dtype=f)[None, :], (128, 128)))
    wr = np.concatenate([np.asarray(inputs["w_router_group"][0], dtype=f),
                         np.asarray(inputs["w_router_expert"][0], dtype=f)], axis=1)
    m["wr"] = np.ascontiguousarray(wr.reshape(DC, 128, 20).transpose(1, 0, 2))
    br = np.concatenate([np.asarray(inputs["b_router_group"][0], dtype=f),
                         np.asarray(inputs["b_router_expert"][0], dtype=f)])
    m["br"] = np.ascontiguousarray(np.broadcast_to(br[None, :], (128, 20)))
    m["w_in"] = np.ascontiguousarray(inputs["w_in"][0], dtype=f)
    m["w_ao"] = np.ascontiguousarray(inputs["w_attn_out"][0], dtype=f)
    m["w_fo"] = np.ascontiguousarray(inputs["w_four_out"][0], dtype=f)
    m["w_o"] = np.ascontiguousarray(inputs["w_out"][0], dtype=f)
    m["w1"] = np.ascontiguousarray(inputs["w_exp_gate"][0], dtype=f)
    m["w3"] = np.ascontiguousarray(inputs["w_exp_up"][0], dtype=f)
    m["w2"] = np.ascontiguousarray(inputs["w_exp_down"][0], dtype=f)
    m["bmg"] = np.ascontiguousarray(np.broadcast_to(
        np.concatenate([bm[2048:3072], bm[5120:6144], bm[3072:4096], bm[4096:5120]])[None, :], (128, 4096)))
    m["n2gbc"] = np.ascontiguousarray(np.broadcast_to(np.asarray(inputs["norm2_g"][0], dtype=f)[None, :], (128, D)))
    for k in ("ident", "rt", "cossin", "cs_c", "dft", "ustrict", "thr16", "thr48", "ltmask", "pidx"):
        m[k] = cs[k]
    return m


def kernel(**inputs):
    nb = 4
    nc = build(nb)
    in_maps = [host_inputs(inputs, c, nb) for c in range(N_CORES)]
    res = run_bass_kernel_spmd(nc, in_maps, core_ids=list(range(N_CORES)))
    return np.concatenate([np.asarray(r["out"]) for r in res.results], axis=0).astype(np.float32)
```

```python
import contextlib
import math
import os
import numpy as np
import ml_dtypes
import concourse.bass as bass
import concourse.mybir as mybir
from concourse.bass_utils import run_bass_kernel_spmd

F32 = mybir.dt.float32
BF16 = mybir.dt.bfloat16
AF = mybir.ActivationFunctionType
ALU = mybir.AluOpType
AX = mybir.AxisListType

D = 1024
DC = 8
N = 2048
NT = 16
CTX = 256
NK = N + CTX
KC = NK // 128
NH = 8
PROJ_W = 5632
Q_OFF, K_OFF, V_OFF, F_OFF, GA_OFF, GF_OFF = 0, 1024, 2048, 3072, 3584, 4608
NE = 16
EH = 512
EPS = 1e-6
LAM_INIT = 0.8 - 0.6 * math.exp(-0.3 * 0)
ATTN_SCALE = 64 ** -0.5
N_CORES = 8


class Buf:
    __slots__ = ("name", "last_w", "readers", "dsem", "dcount")

    def __init__(self, name):
        self.name = name
        self.last_w = {}
        self.readers = {}
        self.dsem = None
        self.dcount = 0


class Fw:
    ENG = ("pe", "act", "dve", "pool", "sp")

    def __init__(self, nc, stack):
        self.nc = nc
        self.stack = stack
        self.sems = []
        self.q = {e: [] for e in self.ENG}
        self.esem = {e: self.newsem("e_" + e) for e in ("pe", "act", "dve", "pool")}
        self.ecnt = {e: 0 for e in ("pe", "act", "dve", "pool")}
        self.waited = {e: {} for e in self.ENG}
        self.dma_events = {}
        self.dsem_by_name = {}

    def newsem(self, name):
        s = self.stack.enter_context(self.nc.semaphore(name))
        self.sems.append(s)
        return len(self.sems) - 1

    def _deps(self, reads, writes, acc=False):
        evs = {}

        def add(s, v):
            if evs.get(s, 0) < v:
                evs[s] = v

        for b in reads:
            for s, v in b.last_w.items():
                add(s, v)
        for b in writes:
            if not acc:
                for s, v in b.last_w.items():
                    add(s, v)
            for s, v in b.readers.items():
                add(s, v)
        return evs

    def _waits(self, eng, evs):
        w = self.waited[eng]
        own = self.esem.get(eng) if eng == "pe" else None
        for s, v in evs.items():
            if s == own:
                continue
            if w.get(s, 0) < v:
                w[s] = v
                self.q[eng].append(("w", s, v))

    def _record(self, ev, reads, writes, acc=False):
        for b in writes:
            if acc:
                if b.last_w.get(ev[0], 0) < ev[1]:
                    b.last_w[ev[0]] = ev[1]
            else:
                b.last_w = {ev[0]: ev[1]}
                b.readers = {}
        for b in reads:
            if b not in writes:
                if b.readers.get(ev[0], 0) < ev[1]:
                    b.readers[ev[0]] = ev[1]

    def op(self, eng, fn, reads=(), writes=(), acc=False):
        self._waits(eng, self._deps(reads, writes, acc))
        self.ecnt[eng] += 1
        ev = (self.esem[eng], self.ecnt[eng])
        self.q[eng].append(("i", fn, self.esem[eng]))
        self._record(ev, reads, writes, acc)
        return ev

    def dma(self, q, out_ap, in_ap, reads, writes, sembuf, acc=False):
        self._waits(q, self._deps(reads, writes, acc))
        if sembuf.name not in self.dsem_by_name:
            self.dsem_by_name[sembuf.name] = [self.newsem("d_" + sembuf.name), 0]
        ent = self.dsem_by_name[sembuf.name]
        ent[1] += 16
        sembuf.dsem, sembuf.dcount = ent[0], ent[1]
        ev = (sembuf.dsem, sembuf.dcount)
        self.dma_events[sembuf.dsem] = sembuf.dcount
        self.q[q].append(("d", out_ap, in_ap, sembuf.dsem))
        self._record(ev, reads, writes, acc)
        return ev

    def dma_fn(self, q, fn, reads, writes, sembuf, acc=False):
        self._waits(q, self._deps(reads, writes, acc))
        if sembuf.name not in self.dsem_by_name:
            self.dsem_by_name[sembuf.name] = [self.newsem("d_" + sembuf.name), 0]
        ent = self.dsem_by_name[sembuf.name]
        ent[1] += 16
        sembuf.dsem, sembuf.dcount = ent[0], ent[1]
        ev = (sembuf.dsem, sembuf.dcount)
        self.dma_events[sembuf.dsem] = sembuf.dcount
        self.q[q].append(("f", fn, sembuf.dsem))
        self._record(ev, reads, writes, acc)
        return ev

    def wait_bufs(self, eng, bufs):
        evs = {}
        for b in bufs:
            for s, v in list(b.last_w.items()) + list(b.readers.items()):
                if evs.get(s, 0) < v:
                    evs[s] = v
        self._waits(eng, evs)

    def emit(self):
        nc = self.nc
        sems = self.sems

        def run(e, items):
            for it in items:
                if it[0] == "w":
                    e.wait_ge(sems[it[1]], it[2])
                elif it[0] == "i":
                    ins = it[1](e)
                    ins.then_inc(sems[it[2]], 1)
                elif it[0] == "f":
                    ins = it[1](e)
                    ins.then_inc(sems[it[2]], 16)
                else:
                    e.dma_start(out=it[1], in_=it[2]).then_inc(sems[it[3]], 16)

        with nc.Block() as block:

            @block.tensor
            def _(t):
                run(t, self.q["pe"])

            @block.scalar
            def _(t):
                run(t, self.q["act"])

            @block.vector
            def _(t):
                run(t, self.q["dve"])

            @block.gpsimd
            def _(t):
                run(t, self.q["pool"])

            @block.sync
            def _(t):
                run(t, self.q["sp"])


def _bf(a):
    return np.ascontiguousarray(a.astype(ml_dtypes.bfloat16))


def make_consts():
    c = {}
    c["ident"] = np.eye(128, dtype=np.float32)
    R = np.zeros((128, 128), np.float32)
    for i in range(128):
        if (i % 32) < 16:
            R[i, i + 16] = -1.0
        else:
            R[i, i - 16] = 1.0
    c["rt"] = _bf(R.T)
    inv = (1.0 / (np.float32(10000.0) ** (np.arange(16, dtype=np.float32) / np.float32(16)))).astype(np.float32)
    tok = np.arange(N)
    row = (tok // 64).astype(np.float32)
    col = (tok % 64).astype(np.float32)
    cs = np.zeros((128, 2, N), np.float32)
    for p in range(128):
        hh = (p % 64) // 32
        f = p % 16
        ang = ((row if hh == 0 else col) * inv[f]).astype(np.float32)
        cs[p, 0] = np.cos(ang)
        cs[p, 1] = np.sin(ang)
    c["cossin"] = _bf(cs)
    k = np.arange(128, dtype=np.float64)
    a = 2 * np.pi * np.outer(k, k) / 128.0
    c["cs_c"] = _bf(np.concatenate([np.cos(a), np.sin(a)], axis=1) / np.sqrt(128.0))
    n = np.arange(N, dtype=np.int64)
    prod = np.outer(n, n) % N
    ang = 2 * np.pi * prod.astype(np.float64) / N
    tabs = np.stack([np.cos(ang), -np.sin(ang)], 0) / np.sqrt(float(N))
    t = tabs.reshape(2, 4, 4, 128, 4, 512)
    t = t.transpose(0, 4, 1, 3, 2, 5)
    c["dft"] = _bf(t)
    kk = np.arange(128)
    c["ustrict"] = (kk[:, None] < kk[None, :]).astype(np.float32)
    c["thr16"] = np.ascontiguousarray(np.broadcast_to((512.0 * np.arange(16, dtype=np.float32))[None, :], (128, 16)))
    c["thr48"] = np.ascontiguousarray(np.broadcast_to((512.0 * np.arange(48, dtype=np.float32))[None, :], (128, 48)))
    ee = np.arange(16)
    lt = (ee[None, :] < ee[:, None]).astype(np.float32)
    c["ltmask"] = np.ascontiguousarray(np.broadcast_to(lt[None], (128, 16, 16)))
    c["pidx"] = np.arange(128, dtype=np.float32).reshape(128, 1)
    return c


_CONSTS = None


def get_consts():
    global _CONSTS
    if _CONSTS is None:
        _CONSTS = make_consts()
    return _CONSTS


_DTSZ = {F32: 4, BF16: 2, mybir.dt.int32: 4}


class Region:
    def __init__(self, tensor, nbytes):
        self.t = tensor
        self.n = nbytes
        self.off = 0
        self.live = []
        self.hist = {}

    def newbuf(self, name):
        b = Buf(name)
        b.readers = dict(self.hist)
        self.live.append(b)
        return b

    def alloc(self, name, shape, dt):
        nbytes = int(np.prod(shape[1:])) * _DTSZ[dt]
        nbytes_al = (nbytes + 31) // 32 * 32
        assert self.off + nbytes_al <= self.n, (name, self.off, nbytes_al, self.n)
        v = self.t[:, self.off:self.off + nbytes].bitcast(dt)
        if len(shape) == 3:
            v = v.rearrange("p (a b) -> p a b", b=shape[2])
        self.off += nbytes_al
        return v, self.newbuf(name)

    def mark(self):
        return (self.off, len(self.live))

    def release(self, m):
        for b in self.live[m[1]:]:
            for ev in list(b.last_w.items()) + list(b.readers.items()):
                if self.hist.get(ev[0], 0) < ev[1]:
                    self.hist[ev[0]] = ev[1]
        del self.live[m[1]:]
        self.off = m[0]


def mm_group(out_ap, pairs):
    def fn(e):
        ins = None
        n = len(pairs)
        for i, (l, r) in enumerate(pairs):
            ins = e.matmul(out_ap, lhsT=l, rhs=r, start=(i == 0), stop=(i == n - 1))
        return ins
    return fn


def build(nb=4, stage=99, dbg=False):
    nc = bass.Bass("TRN2", target_bir_lowering=False)
    U8 = mybir.dt.uint8

    def inp(name, shape, dt=F32):
        return nc.dram_tensor(name, list(shape), dt, kind="ExternalInput").ap()

    x_d = inp("x", [nb, N, D])
    ctx_d = inp("ctx", [nb, CTX, D])
    cT_d = inp("cT", [128, DC, 8])
    wmod_d = inp("w_mod", [D, 6 * D])
    bmodT_d = inp("bmodT", [128, 48])
    bmg_d = inp("bmg", [128, 4 * D])
    n2gbc_d = inp("n2gbc", [128, D])
    ustrict_d = inp("ustrict", [128, 128])
    thr16_d = inp("thr16", [128, 16])
    thr48_d = inp("thr48", [128, 48])
    ltmask_d = inp("ltmask", [128, 16, 16])
    pidx_d = inp("pidx", [128, 1])
    n1g_d = inp("n1g", [128, DC])
    n2g_d = inp("n2g", [128, DC])
    fing_d = inp("fing", [128, D])
    lamv_d = inp("lamv", [128, 256])
    subg_d = inp("subg", [128, 128])
    wr_d = inp("wr", [128, DC, 20])
    br_d = inp("br", [128, 20])
    win_d = inp("w_in", [D, PROJ_W])
    wao_d = inp("w_ao", [D, D])
    wfo_d = inp("w_fo", [512, D])
    wo_d = inp("w_o", [D, D])
    w1_d = inp("w1", [NE, D, EH])
    w3_d = inp("w3", [NE, D, EH])
    w2_d = inp("w2", [NE, EH, D])
    ident_d = inp("ident", [128, 128])
    rt_d = inp("rt", [128, 128], BF16)
    cossin_d = inp("cossin", [128, 2, N], BF16)
    csc_d = inp("cs_c", [128, 256], BF16)
    dft_d = inp("dft", [2, 4, 4, 128, 4, 512], BF16)
    out_d = nc.dram_tensor("out", [nb, N, D], F32, kind="ExternalOutput").ap()

    def scr(name, shape, dt=BF16):
        return nc.dram_tensor(name, list(shape), dt).ap()

    wqkv_s = scr("wqkv_s", [NH, 128, DC, 384])
    wf_s = scr("wf_s", [128, DC, 512])
    wg_s = scr("wg_s", [4, 128, DC, 512])
    wao_s = scr("wao_s", [2, 128, DC, 512])
    wfo_s = scr("wfo_s", [128, 4, D])
    wo_s = scr("wo_s", [2, 128, DC, 512])
    w1_s = scr("w1_s", [NE, 128, DC, EH])
    w3_s = scr("w3_s", [NE, 128, DC, EH])
    w2_s = scr("w2_s", [NE, 128, 4, D])
    gsc_s = scr("gsc_s", [nb, 4, 128, D], F32)
    J = nb * NT
    NST = nb * 8 + 16
    x1_s = scr("x1_s", [nb * N, D], F32)
    h2_s = scr("h2_s", [nb * N, D], BF16)
    hs_s = scr("hs_s", [NST * 512, D], BF16)
    ys_s = scr("ys_s", [NST * 512, D], BF16)

    with contextlib.ExitStack() as st:
        fw = Fw(nc, st)

        def sb(name, shape, dt):
            return st.enter_context(nc.sbuf_tensor(name, list(shape), dt))

        regA_t = sb("regA", [128, DC * NK * 2], U8)
        regB_t = sb("regB", [128, 65536], U8)
        regC_t = sb("regC", [128, 32768], U8)
        regD_t = sb("regD", [128, 32768], U8)
        ring_t = sb("ring", [128, 32768], U8)
        ident = sb("ident_sb", [128, 128], F32)
        rt = sb("rt_sb", [128, 128], BF16)
        csc = sb("csc_sb", [128, 256], BF16)
        smallt = sb("small", [128, 16, 16], F32)
        modA1 = sb("modA1", [128, DC, 8], F32)
        modB1 = sb("modB1", [128, DC, 8], F32)
        modA2 = sb("modA2", [128, DC, 8], F32)
        modB2 = sb("modB2", [128, DC, 8], F32)
        subg = sb("subg_sb", [128, 128], F32)
        lamt = sb("lamt", [128, 8], F32)
        epst = sb("epst", [128, 8], F32)
        Gall = sb("Gall", [128, nb * NT, 16], F32)

        HT = regA_t[:, :].bitcast(BF16).rearrange("p (a b) -> p a b", b=NK)
        RB = Region(regB_t, 65536)
        RC = Region(regC_t, 32768)
        RD = Region(regD_t, 32768)

        psbig = st.enter_context(nc.psum_tensor("psbig", [128, 4096], F32))
        ps = [psbig[:, i * 512:(i + 1) * 512] for i in range(8)]
        PS = [Buf(f"ps{i}") for i in range(8)]

        B_const = Buf("const")
        B_HT = [Buf(f"HT{i}") for i in range(5)]
        B_mod = Buf("mod")
        B_lam = Buf("lam")
        B_scr = {k: Buf(k) for k in ("wqkv", "wf", "wg", "wao", "wfo", "wo", "gsc")}
        B_w1 = [Buf(f"w1s{e}") for e in range(NE)]
        B_out = Buf("outd")
        B_G = Buf("Gall")
        B_x1s = Buf("x1s")
        B_h2s = Buf("h2s")
        B_small = [Buf(f"small{i}") for i in range(16)]
        small_ctr = [0]

        def small_alloc():
            i = small_ctr[0] % 16
            small_ctr[0] += 1
            return smallt[:, i, :], B_small[i]

        ring_slots = [(ring_t[:, i * 8192:(i + 1) * 8192].bitcast(BF16), Buf(f"ring{i}")) for i in range(4)]

        def chunk_view(ap, shape):
            n = int(np.prod(shape[1:]))
            return ap[:, 0:n].rearrange("p (a b) -> p a b", b=shape[2])

        class Ring:
            def __init__(self, slots, la):
                self.slots = slots
                self.la = la
                self.sched = []
                self.issued = 0
                self.taken = 0
                self.views = {}

            def plan(self, src_ap, shape, srcbuf):
                self.sched.append((src_ap, shape, srcbuf))

            def _issue(self, i):
                src_ap, shape, srcbuf = self.sched[i]
                ap, buf = self.slots[i % len(self.slots)]
                v = chunk_view(ap, shape)
                fw.dma("sp", v, src_ap, [srcbuf], [buf], buf)
                return v, buf

            def take(self):
                i = self.taken
                self.taken += 1
                while self.issued < min(len(self.sched), i + 1 + self.la):
                    self.views[self.issued] = self._issue(self.issued)
                    self.issued += 1
                return self.views.pop(i)

        fw.dma("sp", ident[:], ident_d, [], [B_const], B_const)
        fw.dma("sp", rt[:], rt_d, [], [B_const], B_const)
        fw.dma("sp", csc[:], csc_d, [], [B_const], B_const)
        B_subg = Buf("subg")
        fw.dma("sp", subg[:], subg_d, [], [B_subg], B_subg)
        B_eps = Buf("eps")
        fw.op("pool", lambda e: e.memset(epst[:], EPS), [], [B_eps])

        def wview(w, c0, cw):
            return w[:, c0:c0 + cw].rearrange("(kc p) j -> p kc j", p=128)

        for h in range(NH):
            for sec, off in enumerate((Q_OFF, K_OFF, V_OFF)):
                fw.dma("pool", wqkv_s[h, :, :, sec * 128:(sec + 1) * 128],
                       wview(win_d, off + h * 128, 128), [], [B_scr["wqkv"]], B_scr["wqkv"])
        fw.dma("pool", wf_s, wview(win_d, F_OFF, 512), [], [B_scr["wf"]], B_scr["wf"])
        for i, off in enumerate((GA_OFF, GA_OFF + 512, GF_OFF, GF_OFF + 512)):
            fw.dma("pool", wg_s[i], wview(win_d, off, 512), [], [B_scr["wg"]], B_scr["wg"])
        for i in range(2):
            fw.dma("pool", wao_s[i], wview(wao_d, i * 512, 512), [], [B_scr["wao"]], B_scr["wao"])
            fw.dma("pool", wo_s[i], wview(wo_d, i * 512, 512), [], [B_scr["wo"]], B_scr["wo"])
        fw.dma("pool", wfo_s, wview(wfo_d, 0, D), [], [B_scr["wfo"]], B_scr["wfo"])
        for e in range(NE):
            fw.dma("pool", w1_s[e], wview(w1_d[e], 0, EH), [], [B_w1[e]], B_w1[e])
            fw.dma("pool", w3_s[e], wview(w3_d[e], 0, EH), [], [B_w1[e]], B_w1[e])
            fw.dma("pool", w2_s[e], wview(w2_d[e], 0, D), [], [B_w1[e]], B_w1[e])

        mD = RD.mark()
        mC = RC.mark()
        wm, B_wm = RD.alloc("wm", [128, DC, D], F32)
        rep, B_rep = RC.alloc("rep", [128, 4 * DC, 128], F32)
        sc, B_sc = RC.alloc("sc", [128, DC, 8], F32)
        modT, B_modT = RC.alloc("modT", [128, 6 * DC, 8], F32)
        bmodT, B_p0c = RC.alloc("bmodT", [128, 48, 1], F32)
        n1g, _ = RC.alloc("n1g", [128, DC, 1], F32)
        n2g, _ = RC.alloc("n2g", [128, DC, 1], F32)
        mB0 = RB.mark()
        wm2, B_wm2 = RB.alloc("wm2", [128, DC, D], F32)
        dgt = [RB.alloc(f"dg{i}", [128, 128], F32) for i in range(4)]
        gtmp0, B_gt0 = RC.alloc("gtmp0", [128, 512], F32)
        gtmp1, B_gt1 = RC.alloc("gtmp1", [128, 512], F32)
        gtmp = [gtmp0, gtmp1]
        B_gtmp = [B_gt0, B_gt1]
        ones_t, B_ones = RC.alloc("ones", [128, 128], F32)
        lamv, B_lamv = RC.alloc("lamv", [128, 256], F32)
        lamp, B_lamp = RC.alloc("lamp", [128, 2, 64], F32)

        fw.dma("sp", sc, cT_d, [], [B_sc], B_sc)
        fw.dma("sp", bmodT[:, :, 0], bmodT_d, [], [B_p0c], B_p0c)
        fw.dma("sp", n1g[:, :, 0], n1g_d, [], [B_p0c], B_p0c)
        fw.dma("sp", n2g[:, :, 0], n2g_d, [], [B_p0c], B_p0c)
        fw.dma("sp", lamv, lamv_d, [], [B_lamv], B_lamv)
        fw.op("dve", lambda e: e.tensor_tensor(out=lamp[:, 0, :], in0=lamv[:, 0:64], in1=lamv[:, 64:128],
                                               op=ALU.mult), [B_lamv], [B_lamp])
        fw.op("dve", lambda e: e.tensor_tensor(out=lamp[:, 1, :], in0=lamv[:, 128:192], in1=lamv[:, 192:256],
                                               op=ALU.mult), [B_lamv, B_lamp], [B_lamp])
        fw.op("dve", lambda e: e.tensor_reduce(out=lamt[:, 0:2], in_=lamp, axis=AX.X, op=ALU.add),
              [B_lamp], [B_lam])
        fw.op("act", lambda e: e.activation(out=lamt[:, 0:2], in_=lamt[:, 0:2], func=AF.Exp), [B_lam], [B_lam])
        fw.op("dve", lambda e: e.scalar_tensor_tensor(out=lamt[:, 2:3], in0=lamt[:, 1:2], scalar=-LAM_INIT,
                                                      in1=lamt[:, 0:1], op0=ALU.add, op1=ALU.subtract),
              [B_lam], [B_lam])
        fw.op("dve", lambda e: e.tensor_scalar(out=subg[:], in0=subg[:], scalar1=1.0 - LAM_INIT, scalar2=None,
                                               op0=ALU.mult), [B_subg], [B_subg])
        fw.op("act", lambda e: e.activation(out=sc, in_=sc, func=AF.Silu), [B_sc], [B_sc])
        fw.op("dve", lambda e: e.memset(ones_t, 1.0), [], [B_ones])
        for j in range(6):
            wmj, B_wmj = (wm, B_wm) if j % 2 == 0 else (wm2, B_wm2)
            fw.dma("sp", wmj, wmod_d[:, j * D:(j + 1) * D].rearrange("(kc p) f -> p kc f", p=128),
                   [], [B_wmj], B_wmj)

            def mm_feat(e, wmj=wmj):
                ins = None
                for fc in range(DC):
                    for kc in range(DC):
                        ins = e.matmul(ps[0][:, fc * 8:(fc + 1) * 8], lhsT=wmj[:, kc, fc * 128:(fc + 1) * 128],
                                       rhs=sc[:, kc, :], start=(kc == 0), stop=(kc == DC - 1))
                return ins
            fw.op("pe", mm_feat, [B_wmj, B_sc], [PS[0]])
            fw.op("dve", lambda e, j=j: e.tensor_tensor(
                out=modT[:, j * DC:(j + 1) * DC, :],
                in0=ps[0][:, 0:64].rearrange("p (a b) -> p a b", b=8),
                in1=bmodT[:, j * DC:(j + 1) * DC, :].broadcast_to([128, DC, 8]), op=ALU.add),
                [PS[0], B_p0c], [B_modT], acc=True)
        fw.op("dve", lambda e: e.scalar_tensor_tensor(
            out=modA1[:], in0=modT[:, 1 * DC:2 * DC, :], scalar=1.0, in1=n1g.broadcast_to([128, DC, 8]),
            op0=ALU.add, op1=ALU.mult), [B_modT, B_p0c], [B_mod], acc=True)
        fw.op("dve", lambda e: e.tensor_copy(out=modB1[:], in_=modT[:, 0:DC, :]), [B_modT], [B_mod], acc=True)
        fw.op("dve", lambda e: e.scalar_tensor_tensor(
            out=modA2[:], in0=modT[:, 4 * DC:5 * DC, :], scalar=1.0, in1=n2g.broadcast_to([128, DC, 8]),
            op0=ALU.add, op1=ALU.mult), [B_modT, B_p0c], [B_mod], acc=True)
        fw.op("dve", lambda e: e.tensor_copy(out=modB2[:], in_=modT[:, 3 * DC:4 * DC, :]), [B_modT], [B_mod], acc=True)
        row_src = (lambda fc, b: modT[:, 2 * DC + fc, b:b + 1], lambda fc, b: modT[:, 5 * DC + fc, b:b + 1],
                   lambda fc, b: modB2[:, fc, b:b + 1], lambda fc, b: modA2[:, fc, b:b + 1])
        dgc = 0
        rbk = 0
        for which in range(4):
            for b in range(nb):
                for half in range(2):
                    bank = 1 + (rbk % 4)
                    rbk += 1
                    for q in range(4):
                        fc = half * 4 + q
                        dg, B_dg = dgt[dgc % 4]
                        dgc += 1
                        fw.op("dve", lambda e, dg=dg, v=row_src[which](fc, b): e.tensor_scalar(
                            out=dg, in0=ident[:], scalar1=v, scalar2=None, op0=ALU.mult),
                            [B_const, B_modT, B_mod], [B_dg])
                        fw.op("pe", lambda e, dg=dg, bank=bank, q=q: e.matmul(
                            ps[bank][:, q * 128:(q + 1) * 128], lhsT=ones_t, rhs=dg, start=True, stop=True),
                            [B_dg, B_ones], [PS[bank]])
                    if bank % 2 == 0:
                        fw.op("dve", lambda e, half=half, bank=bank: e.tensor_copy(out=gtmp[half], in_=ps[bank]),
                              [PS[bank]], [B_gtmp[half]])
                    else:
                        fw.op("act", lambda e, half=half, bank=bank: e.activation(out=gtmp[half], in_=ps[bank],
                                                                                  func=AF.Identity),
                              [PS[bank]], [B_gtmp[half]])
                    fw.dma("sp", gsc_s[b, which, :, half * 512:(half + 1) * 512], gtmp[half],
                           [B_gtmp[half]], [B_scr["gsc"]], B_gtmp[half], acc=True)

        dbgq = []

        def dump(name, ap, shape, dt, bufs):
            d = nc.dram_tensor("dbg_" + name, list(shape), dt, kind="ExternalOutput").ap()
            bb = Buf("dbg_" + name)
            fw.dma("sp", d, ap, bufs, [], bb)
            dbgq.append(bb)

        if dbg and stage == 0:
            for nm, t in (("modA1", modA1), ("modB1", modB1), ("modA2", modA2), ("modB2", modB2)):
                dump(nm, t[:], [128, DC, 8], F32, [B_mod])
            fw.wait_bufs("sp", B_gtmp)
            dump("gsc", gsc_s, [nb, 4, 128, D], F32, [B_scr["gsc"]])
            dump("w1s", w1_s[3], [128, DC, EH], BF16, [B_w1[3]])
            dump("lam", lamt[:], [128, 8], F32, [B_lam])
        RD.release(mD)
        RC.release(mC)
        RB.release(mB0)

        def rms_stats(src_ap, src_bufs, junk, B_junk, n):
            sm, B_sm = small_alloc()
            ss = sm[:, 0:1]
            rstd = sm[:, 1:2]
            fw.op("act", lambda e: e.activation(out=junk, in_=src_ap, func=AF.Square, accum_out=ss),
                  src_bufs, [B_junk, B_sm])
            fw.op("act", lambda e: e.activation(out=rstd, in_=ss, func=AF.Ln, scale=1.0 / n, bias=epst[:, 0:1]),
                  [B_sm, B_eps], [B_sm])
            fw.op("act", lambda e: e.activation(out=rstd, in_=rstd, func=AF.Exp, scale=-0.5), [B_sm], [B_sm])
            return rstd, B_sm, sm

        def evac_affine(eng, out_ap, in_ap, scale_ap, bias_ap, reads, writes):
            if eng == "dve":
                fw.op("dve", lambda e: e.tensor_scalar(out=out_ap, in0=in_ap, scalar1=scale_ap, scalar2=bias_ap,
                                                       op0=ALU.mult, op1=ALU.add), reads, writes, acc=True)
            else:
                fw.op("act", lambda e: e.activation(out=out_ap, in_=in_ap, func=AF.Identity, scale=scale_ap,
                                                    bias=bias_ap), reads, writes, acc=True)

        def evac_copy(eng, out_ap, in_ap, reads, writes):
            if eng == "dve":
                fw.op("dve", lambda e: e.tensor_copy(out=out_ap, in_=in_ap), reads, writes, acc=True)
            else:
                fw.op("act", lambda e: e.activation(out=out_ap, in_=in_ap, func=AF.Identity), reads, writes, acc=True)

        def beng(bank):
            return "dve" if bank % 2 == 0 else "act"

        for b in range(nb if stage > 0 else 0):
            ringA = Ring(ring_slots, 2)
            ringA.plan(wf_s, [128, DC, 512], B_scr["wf"])
            for h in range(NH):
                ringA.plan(wqkv_s[h], [128, DC, 384], B_scr["wqkv"])
            for cb in range(2):
                ringA.plan(wfo_s, [128, 4, D], B_scr["wfo"])
                ringA.plan(wg_s[2 + cb], [128, DC, 512], B_scr["wg"])
                ringA.plan(wg_s[cb], [128, DC, 512], B_scr["wg"])
                ringA.plan(wao_s[cb], [128, DC, 512], B_scr["wao"])
            ringA.plan(wo_s[0], [128, DC, 512], B_scr["wo"])
            ringA.plan(wo_s[1], [128, DC, 512], B_scr["wo"])

            mC = RC.mark()
            xt = []
            xn = []
            for i in range(2):
                xt.append(RC.alloc(f"xt{i}", [128, D], F32))
                xn.append(RC.alloc(f"xn{i}", [128, D], F32))
            tcount = 0

            def norm_tile(src_ap, A, Bm, bcol, HTbuf, tok0):
                nonlocal tcount
                s = tcount % 2
                tcount += 1
                xts, B_xts = xt[s]
                xns, B_xns = xn[s]
                fw.dma("sp", xts, src_ap, [], [B_xts], B_xts)
                rstd, B_sm, _ = rms_stats(xts, [B_xts], xns, B_xns, D)
                fw.op("act", lambda e: e.activation(out=xns, in_=xts, func=AF.Identity, scale=rstd),
                      [B_xts, B_sm], [B_xns])
                return lambda: norm_tile_b(s, xns, B_xns, A, Bm, bcol, HTbuf, tok0)

            def norm_tile_b(s, xns, B_xns, A, Bm, bcol, HTbuf, tok0):
                for half in range(2):
                    bank = 4 + 2 * s + half

                    def tr(e, half=half, bank=bank):
                        ins = None
                        for j in range(4):
                            dc = half * 4 + j
                            ins = e.transpose(ps[bank][:, j * 128:(j + 1) * 128], xns[:, dc * 128:(dc + 1) * 128],
                                              ident[:])
                        return ins
                    fw.op("pe", tr, [B_xns, B_const], [PS[bank]])
                    for j in range(4):
                        dc = half * 4 + j
                        evac_affine(beng(bank), HT[:, dc, tok0:tok0 + 128], ps[bank][:, j * 128:(j + 1) * 128],
                                    A[:, dc, bcol:bcol + 1], Bm[:, dc, bcol:bcol + 1], [PS[bank], B_mod], [HTbuf])

            p1args = [(ctx_d[b, t * 128:(t + 1) * 128, :], modA1, modB1, 4, B_HT[0], t * 128) for t in range(2)]
            p1args += [(x_d[b, t * 128:(t + 1) * 128, :], modA1, modB1, b, B_HT[1 + t // 4], CTX + t * 128)
                       for t in range(NT)]
            pend = norm_tile(*p1args[0])
            for t in range(len(p1args)):
                nxt_b = norm_tile(*p1args[t + 1]) if t + 1 < len(p1args) else None
                pend()
                pend = nxt_b
            if stage == 1:
                if dbg:
                    dump("HT", HT, [128, DC, NK], BF16, B_HT)
                break

            wf, B_wf = ringA.take()
            FT = [RC.alloc(f"FT{i}", [128, 4, 512], BF16) for i in range(2)]
            mB = RB.mark()
            ZT, B_ZT = RB.alloc("ZT", [128, 4, N], BF16)
            mB2 = RB.mark()
            AB, _ = RB.alloc("AB", [128, NT, 1024], BF16)
            B_AB = [RB.newbuf(f"AB{i}") for i in range(4)]
            mD = RD.mark()
            dring = [RD.alloc(f"dft{i}", [128, 4, 512], BF16) for i in range(4)]
            cdft = 0
            for tb in range(4):
                ft, B_ft = FT[tb % 2]
                for g in range(4):
                    fw.op("pe", mm_group(ps[g][:, :], [
                        (wf[:, kc, g * 128:(g + 1) * 128], HT[:, kc, CTX + tb * 512:CTX + (tb + 1) * 512])
                        for kc in range(DC)]), [B_wf, B_HT[1 + tb]], [PS[g]])
                    evac_copy(beng(g), ft[:, g, :], ps[g][:, :], [PS[g]], [B_ft])
                for tt in range(4):
                    tile = tb * 4 + tt
                    for gp in range(2):
                        bank = 4 + (cdft % 2)
                        cdft += 1

                        def cdf(e, gp=gp, tt=tt, bank=bank, ft=ft):
                            ins = None
                            for k in range(2):
                                ins = e.matmul(ps[bank][:, k * 256:(k + 1) * 256],
                                               lhsT=ft[:, 2 * gp + k, tt * 128:(tt + 1) * 128], rhs=csc[:, :],
                                               start=True, stop=True)
                            return ins
                        fw.op("pe", cdf, [B_ft, B_const], [PS[bank]])
                        evac_copy(beng(bank), AB[:, tile, gp * 512:(gp + 1) * 512], ps[bank][:, :], [PS[bank]], [B_AB[tb]])
            for mb in range(4):
                for piece in range(8):
                    kind, nq = piece // 4, piece % 4
                    dv, B_dv = dring[(mb * 8 + piece) % 4]
                    fw.dma("sp", dv, dft_d[kind, mb, nq], [], [B_dv], B_dv)

                    def sdf(e, piece=piece, kind=kind, nq=nq, dv=dv):
                        ins = None
                        for g in range(4):
                            for n_ in range(4):
                                ins = e.matmul(ps[g][:, :],
                                               lhsT=AB[:, nq * 4 + n_, g * 256 + kind * 128:g * 256 + (kind + 1) * 128],
                                               rhs=dv[:, n_, :], start=(piece == 0 and n_ == 0),
                                               stop=(piece == 7 and n_ == 3))
                        return ins
                    fw.op("pe", sdf, [B_dv] + B_AB, PS[0:4])
                for g in range(4):
                    evac_copy(beng(g), ZT[:, g, mb * 512:(mb + 1) * 512], ps[g][:, :], [PS[g]], [B_ZT])
            RD.release(mD)
            RB.release(mB2)
            RC.release(mC)
            if stage == 2:
                if dbg:
                    dump("ZT", ZT, [128, 4, N], BF16, [B_ZT])
                break

            mB2 = RB.mark()
            mC = RC.mark()
            mD = RD.mark()
            attnT, _ = RC.alloc("attnT", [128, DC, N], BF16)
            B_attnT = [RC.newbuf(f"attnT{i}") for i in range(4)]
            qkv = []
            for i in range(2):
                QT_, B_QT_ = RD.alloc(f"QT{i}", [128, N], BF16)
                KT_, B_KT_ = RD.alloc(f"KT{i}", [128, NK], BF16)
                VA_, B_VA_ = RD.alloc(f"VA{i}", [128, KC, 132], BF16)
                qkv.append((QT_, B_QT_, KT_, B_KT_, VA_, B_VA_))
                fw.op("pool", lambda e, VA_=VA_: e.memset(VA_[:, :, 128:129], 1.0), [], [B_VA_], acc=True)
            cossin, B_cs = RB.alloc("cossin", [128, 2, N], BF16)
            fw.dma("sp", cossin, cossin_d, [], [B_cs], B_cs)
            PT = [RB.alloc(f"PT{i}", [128, 1024], BF16) for i in range(2)]
            kraw = [RB.alloc(f"kraw{i}", [128, 512], BF16) for i in range(2)]
            t1 = [RB.alloc(f"t1_{i}", [128, 512], F32) for i in range(2)]
            t2 = [RB.alloc(f"t2_{i}", [128, 512], F32) for i in range(2)]
            ocp = [RB.alloc(f"ocp{i}", [128, 9, 160], F32) for i in range(2)]
            hd0 = [RB.alloc(f"hd0_{i}", [128, 4, 128], F32) for i in range(2)]
            hd1 = [RB.alloc(f"hd1_{i}", [128, 4, 128], F32) for i in range(2)]
            rope_ctr = 0
            pj_ctr = 0

            def inproj_units(h, wq, B_wq, standalone):
                QT, B_QT, KT, B_KT, VA, B_VA = qkv[h % 2]
                units = []

                def pbank():
                    nonlocal pj_ctr
                    pj_ctr += 1
                    if not standalone:
                        return 7
                    return pj_ctr % 4

                def kctx():
                    pb = pbank()
                    fw.op("pe", mm_group(ps[pb][:, 0:CTX], [(wq[:, kc, 128:256], HT[:, kc, 0:CTX]) for kc in range(DC)]),
                          [B_wq, B_HT[0]], [PS[pb]])
                    fw.op("dve", lambda e: e.tensor_copy(out=KT[:, 0:CTX], in_=ps[pb][:, 0:CTX]), [PS[pb]], [B_KT],
                          acc=True)
                units.append(kctx)

                def rope_pair(c0, dst_ap, dst_buf, tb):
                    st_ = {}

                    def ua():
                        nonlocal rope_ctr
                        r = rope_ctr % 2
                        rope_ctr += 1
                        st_["r"] = r
                        kr, B_kr = kraw[r]
                        pb = pbank()
                        fw.op("pe", mm_group(ps[pb], [
                            (wq[:, kc, c0:c0 + 128], HT[:, kc, CTX + tb * 512:CTX + (tb + 1) * 512])
                            for kc in range(DC)]), [B_wq, B_HT[1 + tb]], [PS[pb]])
                        fw.op("dve", lambda e: e.tensor_copy(out=kr, in_=ps[pb]), [PS[pb]], [B_kr])

                    def ub():
                        r = st_["r"]
                        kr, B_kr = kraw[r]
                        a1, B_a1 = t1[r]
                        a2, B_a2 = t2[r]
                        rb = pbank()
                        fw.op("pe", lambda e: e.matmul(ps[rb], lhsT=rt[:], rhs=kr, start=True, stop=True),
                              [B_kr, B_const], [PS[rb]])
                        fw.op("pool", lambda e: e.tensor_tensor(out=a1, in0=kr, in1=cossin[:, 0, tb * 512:(tb + 1) * 512],
                                                                op=ALU.mult), [B_kr, B_cs], [B_a1])
                        fw.op("dve", lambda e: e.tensor_tensor(out=a2, in0=ps[rb],
                                                               in1=cossin[:, 1, tb * 512:(tb + 1) * 512], op=ALU.mult),
                              [PS[rb], B_cs], [B_a2])
                        fw.op("pool", lambda e: e.tensor_tensor(out=dst_ap, in0=a1, in1=a2, op=ALU.add),
                              [B_a1, B_a2], [dst_buf], acc=True)
                    units.append(ua)
                    units.append(ub)

                for tb in range(4):
                    rope_pair(128, KT[:, CTX + tb * 512:CTX + (tb + 1) * 512], B_KT, tb)
                for tb in range(4):
                    rope_pair(0, QT[:, tb * 512:(tb + 1) * 512], B_QT, tb)

                def vunit(kc):
                    def u():
                        hb = B_HT[0] if kc < 2 else B_HT[1 + (kc - 2) // 4]
                        pb = pbank()
                        fw.op("pe", mm_group(ps[pb][:, 0:128], [
                            (HT[:, dc, kc * 128:(kc + 1) * 128], wq[:, dc, 256:384]) for dc in range(DC)]),
                            [B_wq, hb], [PS[pb]])
                        fw.op("dve", lambda e: e.tensor_copy(out=VA[:, kc, 0:128], in_=ps[pb][:, 0:128]),
                              [PS[pb]], [B_VA], acc=True)
                    return u
                for kc in range(KC):
                    units.append(vunit(kc))
                return units

            def ogrp(g):
                return 4 + g // 3, (g % 3) * 160

            NHR = int(os.environ.get('DBG_NH', NH))
            pending_fin = []
            wq0, B_wq0 = ringA.take()
            for u in inproj_units(0, wq0, B_wq0, True):
                u()
            for h in range(NHR):
                QT, B_QT, KT, B_KT, VA, B_VA = qkv[h % 2]
                nxt = []
                if h + 1 < NHR:
                    wqn, B_wqn = ringA.take()
                    nxt = inproj_units(h + 1, wqn, B_wqn, False)
                step = 0
                for qb in range(4):
                    q0 = qb * 512

                    def s_op(kc, q0=q0, QT=QT, KT=KT, B_QT=B_QT, B_KT=B_KT):
                        sb0 = 2 * (kc % 2)

                        def fn(e):
                            e.matmul(ps[sb0], lhsT=KT[0:64, kc * 128:(kc + 1) * 128],
                                     rhs=QT[0:64, q0:q0 + 512], start=True, stop=True)
                            return e.matmul(ps[sb0 + 1], lhsT=KT[64:128, kc * 128:(kc + 1) * 128],
                                            rhs=QT[64:128, q0:q0 + 512], start=True, stop=True)
                        fw.op("pe", fn, [B_KT, B_QT], [PS[sb0], PS[sb0 + 1]])

                    def av_op(kc, VA=VA, B_VA=B_VA):
                        pt, B_pt = PT[kc % 2]

                        def av(e):
                            ins = None
                            for g in range(8):
                                bank, c0 = ogrp(g)
                                i, qt = g // 4, g % 4
                                ins = e.matmul(ps[bank][:, c0:c0 + 129],
                                               lhsT=pt[:, i * 512 + qt * 128:i * 512 + (qt + 1) * 128],
                                               rhs=VA[:, kc, 0:129], start=(kc == 0 and g % 3 == 0),
                                               stop=(kc == KC - 1), skip_group_check=True)
                            return ins
                        fw.op("pe", av, [B_pt, B_VA], PS[4:7])

                    s_op(0)
                    for kc in range(KC):
                        if kc + 1 < KC:
                            s_op(kc + 1)
                        sb0 = 2 * (kc % 2)
                        pt, B_pt = PT[kc % 2]
                        fw.op("act", lambda e, pt=pt, sb0=sb0: e.activation(
                            out=pt, in_=psbig[:, sb0 * 512:sb0 * 512 + 1024], func=AF.Exp, scale=ATTN_SCALE),
                            [PS[sb0], PS[sb0 + 1]], [B_pt])
                        if nxt and (step % 2 == 1):
                            nxt.pop(0)()
                        step += 1
                        av_op(kc)
                        if kc == 8 and pending_fin:
                            if nxt and getattr(nxt[0], "second_half", False):
                                nxt.pop(0)()
                            pending_fin.pop(0)()
                    def make_fin(qb=qb, q0=q0, h=h):
                        oc, B_oc = ocp[qb % 2]
                        h0, B_h0 = hd0[qb % 2]
                        h1, B_h1 = hd1[qb % 2]
                        sm, B_sm = small_alloc()

                        def fa():
                            for k in range(3):
                                ng = 3 if k < 2 else 2
                                fw.op("dve", lambda e, k=k, ng=ng: e.tensor_copy(
                                    out=oc[:, 3 * k:3 * k + ng, :],
                                    in_=ps[4 + k][:, 0:ng * 160].rearrange("p (a b) -> p a b", b=160)),
                                    [PS[4 + k]], [B_oc], acc=True)
                            fw.op("dve", lambda e: e.reciprocal(out=sm[:, 0:8].unsqueeze(2), in_=oc[:, 0:8, 128:129]),
                                  [B_oc], [B_sm])
                            fw.op("dve", lambda e: e.tensor_scalar(out=sm[:, 8:12], in0=sm[:, 4:8], scalar1=lamt[:, 2:3],
                                                                   scalar2=None, op0=ALU.mult), [B_sm, B_lam], [B_sm])
                            fw.op("dve", lambda e: e.tensor_tensor(
                                out=h0, in0=oc[:, 0:4, 0:128], in1=sm[:, 0:4].unsqueeze(2).broadcast_to([128, 4, 128]),
                                op=ALU.mult), [B_oc, B_sm], [B_h0])
                            fw.op("dve", lambda e: e.tensor_tensor(
                                out=h1, in0=oc[:, 4:8, 0:128], in1=sm[:, 8:12].unsqueeze(2).broadcast_to([128, 4, 128]),
                                op=ALU.mult), [B_oc, B_sm], [B_h1])
                            fw.op("pool", lambda e: e.tensor_tensor(out=h1, in0=h0, in1=h1, op=ALU.add),
                                  [B_h0, B_h1], [B_h1])
                            fw.op("dve", lambda e: e.tensor_tensor(out=h0, in0=h1, in1=h1, op=ALU.mult),
                                  [B_h1], [B_h0])
                            fw.op("dve", lambda e: e.tensor_reduce(out=sm[:, 12:16], in_=h0, axis=AX.X, op=ALU.add),
                                  [B_h0, B_sm], [B_sm])

                        def fbc():
                            fw.op("act", lambda e: e.activation(out=sm[:, 12:16], in_=sm[:, 12:16], func=AF.Ln,
                                                                scale=1.0 / 128, bias=epst[:, 0:1]),
                                  [B_sm, B_eps], [B_sm])
                            fw.op("act", lambda e: e.activation(out=sm[:, 12:16], in_=sm[:, 12:16], func=AF.Exp,
                                                                scale=-0.5), [B_sm], [B_sm])
                            fw.op("dve", lambda e: e.tensor_tensor(
                                out=h0, in0=h1, in1=sm[:, 12:16].unsqueeze(2).broadcast_to([128, 4, 128]), op=ALU.mult),
                                [B_h1, B_sm], [B_h0])
                            fw.op("pool", lambda e: e.tensor_tensor(
                                out=h1, in0=h0, in1=subg[:].unsqueeze(1).broadcast_to([128, 4, 128]), op=ALU.mult),
                                [B_h0, B_subg], [B_h1])

                            def trf(e):
                                ins = None
                                for qt in range(4):
                                    ins = e.transpose(ps[7][:, qt * 128:(qt + 1) * 128], h1[:, qt, :], ident[:])
                                return ins
                            fw.op("pe", trf, [B_h1, B_const], [PS[7]])
                            fw.op("dve", lambda e: e.tensor_copy(out=attnT[:, h, q0:q0 + 512], in_=ps[7]),
                                  [PS[7]], [B_attnT[qb]])
                        return fa, fbc
                    fa_, fbc_ = make_fin()
                    fa_()
                    pending_fin.append(fbc_)
                while nxt:
                    nxt.pop(0)()
            while pending_fin:
                pending_fin.pop(0)()
            RD.release(mD)
            RB.release(mB2)
            if stage == 3:
                if dbg:
                    dump("attnT", attnT, [128, DC, N], BF16, B_attnT)
                break

            mB2 = RB.mark()
            mD = RD.mark()
            YT, _ = RD.alloc("YT", [128, DC, N], BF16)
            B_YT = [RD.newbuf(f"YT{i}") for i in range(4)]
            sgt = [RB.alloc(f"sg{i}", [128, 512], BF16) for i in range(2)]
            tt_ = [RB.alloc(f"tt{i}", [128, 512], BF16) for i in range(2)]
            cnt4 = 0
            for cb in range(2):
                wfo, B_wfo = ringA.take()
                wgf, B_wgf = ringA.take()
                for dcl in range(4):
                    dc = cb * 4 + dcl
                    for tb in range(4):
                        bg = 2 * (cnt4 % 2)
                        bf_ = bg + 1
                        sg, B_sg = sgt[cnt4 % 2]
                        cnt4 += 1
                        fw.op("pe", mm_group(ps[bg][:, :], [
                            (wgf[:, kc, dcl * 128:(dcl + 1) * 128], HT[:, kc, CTX + tb * 512:CTX + (tb + 1) * 512])
                            for kc in range(DC)]), [B_wgf, B_HT[1 + tb]], [PS[bg]])
                        fw.op("pe", mm_group(ps[bf_][:, :], [
                            (wfo[:, g, dc * 128:(dc + 1) * 128], ZT[:, g, tb * 512:(tb + 1) * 512])
                            for g in range(4)]), [B_wfo, B_ZT], [PS[bf_]])
                        fw.op("act", lambda e, sg=sg, bg=bg: e.activation(out=sg, in_=ps[bg][:, :], func=AF.Sigmoid),
                              [PS[bg]], [B_sg])
                        fw.op("dve", lambda e, sg=sg, bf_=bf_, dc=dc, tb=tb: e.tensor_tensor(
                            out=YT[:, dc, tb * 512:(tb + 1) * 512], in0=ps[bf_][:, :], in1=sg, op=ALU.mult),
                            [PS[bf_], B_sg], [B_YT[tb]], acc=True)
                wga, B_wga = ringA.take()
                wao, B_wao = ringA.take()
                for dcl in range(4):
                    dc = cb * 4 + dcl
                    for tb in range(4):
                        bg = 4 + 2 * (cnt4 % 2)
                        ba = bg + 1
                        sg, B_sg = sgt[cnt4 % 2]
                        tq, B_tq = tt_[cnt4 % 2]
                        cnt4 += 1
                        fw.op("pe", mm_group(ps[bg][:, :], [
                            (wga[:, kc, dcl * 128:(dcl + 1) * 128], HT[:, kc, CTX + tb * 512:CTX + (tb + 1) * 512])
                            for kc in range(DC)]), [B_wga, B_HT[1 + tb]], [PS[bg]])
                        fw.op("pe", mm_group(ps[ba][:, :], [
                            (wao[:, kc, dcl * 128:(dcl + 1) * 128], attnT[:, kc, tb * 512:(tb + 1) * 512])
                            for kc in range(DC)]), [B_wao, B_attnT[tb]], [PS[ba]])
                        fw.op("act", lambda e, sg=sg, bg=bg: e.activation(out=sg, in_=ps[bg][:, :], func=AF.Sigmoid),
                              [PS[bg]], [B_sg])
                        fw.op("dve", lambda e, sg=sg, ba=ba, tq=tq: e.tensor_tensor(
                            out=tq, in0=ps[ba][:, :], in1=sg, op=ALU.mult), [PS[ba], B_sg], [B_tq])
                        fw.op("pool", lambda e, tq=tq, dc=dc, tb=tb: e.tensor_tensor(
                            out=YT[:, dc, tb * 512:(tb + 1) * 512], in0=YT[:, dc, tb * 512:(tb + 1) * 512], in1=tq,
                            op=ALU.add), [B_tq, B_YT[tb]], [B_YT[tb]])
            RB.release(mB2)
            RB.release(mB)
            RC.release(mC)
            if stage == 4:
                if dbg:
                    dump("YT", YT, [128, DC, N], BF16, B_YT)
                break

            mB = RB.mark()
            mC = RC.mark()
            x1r = [RB.alloc(f"x1r{i}", [128, D], F32) for i in range(2)]
            h2r = [RB.alloc(f"h2r{i}", [128, D], BF16) for i in range(2)]
            h2tmp, B_h2tmp = RB.alloc("h2tmp", [128, D], F32)
            A2bc, B_a2bc = RB.alloc("A2bc", [128, D], F32)
            B2bc, _ = RB.alloc("B2bc", [128, D], F32)
            fw.dma("sp", B2bc, gsc_s[b, 2], [B_scr["gsc"]], [B_a2bc], B_a2bc)
            fw.dma("sp", A2bc, gsc_s[b, 3], [B_scr["gsc"]], [B_a2bc], B_a2bc)
            wo0, B_wo0 = ringA.take()
            wo1, B_wo1 = ringA.take()
            wo = [wo0, wo1]
            mC1 = RC.mark()
            g1bc, B_g1 = RC.alloc("g1bc", [128, D], F32)
            xt2 = [RC.alloc(f"xt2_{i}", [128, D], F32) for i in range(2)]
            tmp2, B_tmp2 = RC.alloc("tmp2", [128, D], F32)
            xn2 = [RC.alloc(f"xn2_{i}", [128, D], F32) for i in range(2)]
            h2f, B_h2f = RC.alloc("h2f", [128, DC, 128], F32)
            wr, B_wr = RC.alloc("wr", [128, DC, 20], F32)
            brt, _ = RC.alloc("br", [128, 20], F32)
            LG, B_LG = RB.alloc("LG", [128, NT, 20], F32)
            RV, B_RW = RB.alloc("RV", [128, 7, NT], F32)
            RW3, _ = RB.alloc("RW3", [128, 7 * NT, 4], F32)
            RW3 = RW3.rearrange("p (i t) e -> p i t e", t=NT)
            RW4, _ = RB.alloc("RW4", [128, NT * 4, 4], F32)
            RW4 = RW4.rearrange("p (t g) e -> p t g e", g=4)
            fw.dma("sp", g1bc, gsc_s[b, 0], [B_scr["gsc"]], [B_g1], B_g1)
            fw.dma("sp", wr, wr_d, [], [B_wr], B_wr)
            fw.dma("sp", brt, br_d, [], [B_wr], B_wr)
            xt2.append(RB.alloc("xt2_2", [128, D], F32))

            def xload(tt, b=b):
                xv_, B_xv_ = xt2[tt % 3]
                fw.dma("sp", xv_, x_d[b, tt * 128:(tt + 1) * 128, :], [], [B_xv_], B_xv_)

            def stageA(tt, b=b):
                s = tt % 2
                xts, B_xts = xt2[tt % 3]
                xns, B_xns = xn2[s]
                if tt + 1 < NT:
                    xload(tt + 1)
                for nb_ in range(2):
                    bank = 2 * s + nb_
                    fw.op("pe", mm_group(ps[bank][:, :], [
                        (YT[:, kc, tt * 128:(tt + 1) * 128], wo[nb_][:, kc, :]) for kc in range(DC)]),
                        [B_YT[tt // 4], B_wo0, B_wo1], [PS[bank]])
                    fw.op("dve", lambda e, bank=bank, nb_=nb_: e.tensor_tensor(
                        out=tmp2[:, nb_ * 512:(nb_ + 1) * 512], in0=ps[bank][:, :],
                        in1=g1bc[:, nb_ * 512:(nb_ + 1) * 512], op=ALU.mult), [PS[bank], B_g1], [B_tmp2], acc=True)
                x1t, B_x1t = x1r[s]
                h2t, B_h2t = h2r[s]
                fw.op("pool", lambda e, x1t=x1t, xts=xts: e.tensor_tensor(out=x1t, in0=tmp2, in1=xts, op=ALU.add),
                      [B_tmp2, B_xts], [B_x1t])
                fw.dma("sp", x1_s[b * N + tt * 128:b * N + (tt + 1) * 128, :], x1t, [B_x1t], [B_x1s], B_x1t, acc=True)
                rstd, B_sm, _ = rms_stats(x1t, [B_x1t], xns, B_xns, D)
                fw.op("act", lambda e, x1t=x1t, xns=xns, rstd=rstd: e.activation(
                    out=xns, in_=x1t, func=AF.Identity, scale=rstd), [B_x1t, B_sm], [B_xns])
                fw.op("pool", lambda e, xns=xns: e.tensor_tensor(out=h2tmp, in0=xns, in1=A2bc, op=ALU.mult),
                      [B_xns, B_a2bc], [B_h2tmp])
                fw.op("pool", lambda e, h2t=h2t: e.tensor_tensor(out=h2t, in0=h2tmp, in1=B2bc, op=ALU.add),
                      [B_h2tmp, B_a2bc], [B_h2t])
                fw.dma("sp", h2_s[b * N + tt * 128:b * N + (tt + 1) * 128, :], h2t, [B_h2t], [B_h2s], B_h2t, acc=True)

            def stageB(tt, b=b):
                s = tt % 2
                xns, B_xns = xn2[s]
                for half in range(2):
                    bank = 4 + half

                    def tr(e, half=half, bank=bank, xns=xns):
                        ins = None
                        for j in range(4):
                            dc = half * 4 + j
                            ins = e.transpose(ps[bank][:, j * 128:(j + 1) * 128], xns[:, dc * 128:(dc + 1) * 128],
                                              ident[:])
                        return ins
                    fw.op("pe", tr, [B_xns, B_const], [PS[bank]])
                    for j in range(4):
                        dc = half * 4 + j
                        fw.op("dve", lambda e, dc=dc, j=j, bank=bank, b=b: e.tensor_scalar(
                            out=h2f[:, dc, :], in0=ps[bank][:, j * 128:(j + 1) * 128],
                            scalar1=modA2[:, dc, b:b + 1], scalar2=modB2[:, dc, b:b + 1],
                            op0=ALU.mult, op1=ALU.add), [PS[bank], B_mod], [B_h2f], acc=True)
                fw.op("pe", mm_group(ps[6][:, 0:20], [(h2f[:, kc, :], wr[:, kc, :]) for kc in range(DC)]),
                      [B_h2f, B_wr], [PS[6]])
                fw.op("dve", lambda e, tt=tt: e.tensor_tensor(out=LG[:, tt, :], in0=ps[6][:, 0:20], in1=brt,
                                                              op=ALU.add), [PS[6], B_wr], [B_LG], acc=True)

            xload(0)
            stageA(0)
            for tt in range(NT):
                if tt + 1 < NT:
                    stageA(tt + 1)
                stageB(tt)
            lg = LG[:, :, 0:4]
            le4 = LG[:, :, 4:20].rearrange("p t (g e) -> p t g e", e=4)
            T3 = [128, NT, 4]
            T4 = [128, NT, 4, 4]

            def RR(fn):
                fw.op("dve", fn, [B_LG, B_RW], [B_RW])

            def bc3(v):
                return v.unsqueeze(2).broadcast_to(T3)
            gmax, wgrp, m1, m2, dd, p1, p2 = (RV[:, i, :] for i in range(7))
            ohg, dlg, leg, oh1, leg2, oh2, gi = (RW3[:, i, :, :] for i in range(7))
            RR(lambda e: e.tensor_reduce(out=gmax, in_=lg, axis=AX.X, op=ALU.max))
            RR(lambda e: e.tensor_tensor(out=ohg, in0=lg, in1=bc3(gmax), op=ALU.is_equal))
            RR(lambda e: e.tensor_tensor(out=dlg, in0=lg, in1=bc3(gmax), op=ALU.subtract))
            fw.op("act", lambda e: e.activation(out=dlg, in_=dlg, func=AF.Exp), [B_RW], [B_RW])
            RR(lambda e: e.tensor_reduce(out=wgrp, in_=dlg, axis=AX.X, op=ALU.add))
            RR(lambda e: e.reciprocal(out=wgrp, in_=wgrp))
            RR(lambda e: e.tensor_tensor(out=RW4, in0=le4, in1=ohg.unsqueeze(3).broadcast_to(T4), op=ALU.mult))
            RR(lambda e: e.tensor_reduce(out=leg, in_=RW4.rearrange("p t g e -> p t e g"), axis=AX.X, op=ALU.add))
            RR(lambda e: e.tensor_reduce(out=m1, in_=leg, axis=AX.X, op=ALU.max))
            RR(lambda e: e.tensor_tensor(out=oh1, in0=leg, in1=bc3(m1), op=ALU.is_equal))
            RR(lambda e: e.scalar_tensor_tensor(out=leg2, in0=oh1, scalar=-1e30, in1=leg, op0=ALU.mult, op1=ALU.add))
            RR(lambda e: e.tensor_reduce(out=m2, in_=leg2, axis=AX.X, op=ALU.max))
            RR(lambda e: e.tensor_tensor(out=oh2, in0=leg2, in1=bc3(m2), op=ALU.is_equal))
            RR(lambda e: e.tensor_tensor(out=dd, in0=m2, in1=m1, op=ALU.subtract))
            fw.op("act", lambda e: e.activation(out=dd, in_=dd, func=AF.Exp), [B_RW], [B_RW])
            RR(lambda e: e.tensor_scalar(out=p1, in0=dd, scalar1=1.0, scalar2=None, op0=ALU.add))
            RR(lambda e: e.reciprocal(out=p1, in_=p1))
            RR(lambda e: e.tensor_tensor(out=p2, in0=dd, in1=p1, op=ALU.mult))
            RR(lambda e: e.tensor_tensor(out=p1, in0=p1, in1=wgrp, op=ALU.mult))
            RR(lambda e: e.tensor_tensor(out=p2, in0=p2, in1=wgrp, op=ALU.mult))
            RR(lambda e: e.tensor_tensor(out=gi, in0=oh1, in1=bc3(p1), op=ALU.mult))
            RR(lambda e: e.tensor_tensor(out=oh2, in0=oh2, in1=bc3(p2), op=ALU.mult))
            RR(lambda e: e.tensor_tensor(out=gi, in0=gi, in1=oh2, op=ALU.add))
            fw.op("dve", lambda e, b=b: e.tensor_tensor(
                out=Gall[:, b * NT:(b + 1) * NT, :].rearrange("p t (g e) -> p t g e", e=4),
                in0=gi.unsqueeze(2).broadcast_to(T4), in1=ohg.unsqueeze(3).broadcast_to(T4), op=ALU.mult),
                [B_RW], [B_G], acc=True)
            RD.release(mD)
            RC.release(mC1)
            RC.release(mC)
            RB.release(mB)

        if stage >= 5:
            JJ = J * 16
            mC = RC.mark()
            mB = RB.mark()
            mD = RD.mark()
            ustr, B_sc0 = RC.alloc("ustr", [128, 128], F32)
            onesm, _ = RC.alloc("onesm", [128, 128], F32)
            thr16, _ = RC.alloc("thr16", [128, 16], F32)
            thr48, _ = RC.alloc("thr48", [128, 48], F32)
            ltm, _ = RC.alloc("ltm", [128, 16, 16], F32)
            fw.dma("sp", ustr, ustrict_d, [], [B_sc0], B_sc0)
            fw.dma("sp", thr16, thr16_d, [], [B_sc0], B_sc0)
            fw.dma("sp", thr48, thr48_d, [], [B_sc0], B_sc0)
            fw.dma("sp", ltm, ltmask_d, [], [B_sc0], B_sc0)
            B_on = Buf("onesm")
            fw.op("dve", lambda e: e.memset(onesm, 1.0), [], [B_on])
            Mt, B_M = RB.alloc("Mt", [128, J, 16], F32)
            rank, B_rank = RB.alloc("rank", [128, J, 16], F32)
            tot, B_tot = RB.alloc("tot", [128, J, 16], F32)
            cum, B_cum = RB.alloc("cum", [128, J, 16], F32)
            smt, B_smt = RB.alloc("smt", [128, J, 16], F32)
            s48, B_s48 = RB.alloc("s48", [128, 48, 16], F32)
            s16, B_s16 = RB.alloc("s16", [128, 16, 16], F32)
            vec, B_vec = RC.alloc("vec", [128, 8, 16], F32)
            slots, B_slots = RC.alloc("slots", [128, 6, J], F32)
            sloti, B_sloti = RC.alloc("sloti", [128, 2, J], mybir.dt.int32)
            texpf, B_texp = RC.alloc("texpf", [128, 48], F32)
            texpi, _ = RC.alloc("texpi", [128, 48], mybir.dt.int32)
            pidx, _ = RC.alloc("pidx", [128, 1], F32)
            fw.dma("sp", pidx, pidx_d, [], [B_sc0], B_sc0)
            Mf = Mt.rearrange("p j e -> p (j e)")
            fw.op("dve", lambda e: e.tensor_single_scalar(out=Mt, in_=Gall[:], scalar=0.0, op=ALU.is_gt),
                  [B_G], [B_M])
            for cbk in range((JJ + 511) // 512):
                c0, c1 = cbk * 512, min(JJ, (cbk + 1) * 512)
                fw.op("pe", lambda e, c0=c0, c1=c1: e.matmul(ps[0][:, 0:c1 - c0], lhsT=ustr, rhs=Mf[:, c0:c1],
                                                             start=True, stop=True), [B_M, B_sc0], [PS[0]])
                fw.op("pe", lambda e, c0=c0, c1=c1: e.matmul(ps[1][:, 0:c1 - c0], lhsT=onesm, rhs=Mf[:, c0:c1],
                                                             start=True, stop=True), [B_M, B_on], [PS[1]])
                fw.op("dve", lambda e, c0=c0, c1=c1: e.tensor_copy(
                    out=rank.rearrange("p j e -> p (j e)")[:, c0:c1], in_=ps[0][:, 0:c1 - c0]), [PS[0]], [B_rank],
                    acc=True)
                fw.op("dve", lambda e, c0=c0, c1=c1: e.tensor_copy(
                    out=tot.rearrange("p j e -> p (j e)")[:, c0:c1], in_=ps[1][:, 0:c1 - c0]), [PS[1]], [B_tot],
                    acc=True)
            fw.op("dve", lambda e: e.memset(cum[:, 0, :], 0.0), [], [B_cum])
            for j in range(1, J):
                fw.op("dve", lambda e, j=j: e.tensor_tensor(out=cum[:, j, :], in0=cum[:, j - 1, :], in1=tot[:, j - 1, :],
                                                            op=ALU.add), [B_cum, B_tot], [B_cum])
            cnt = vec[:, 0, :]
            ntl = vec[:, 1, :]
            off = vec[:, 2, :]
            fw.op("dve", lambda e: e.tensor_tensor(out=cnt, in0=cum[:, J - 1, :], in1=tot[:, J - 1, :], op=ALU.add),
                  [B_cum, B_tot], [B_vec])
            fw.op("dve", lambda e: e.tensor_tensor(out=s16, in0=cnt.unsqueeze(2).broadcast_to([128, 16, 16]),
                                                   in1=thr16.unsqueeze(1).broadcast_to([128, 16, 16]), op=ALU.is_gt),
                  [B_vec, B_sc0], [B_s16])
            fw.op("dve", lambda e: e.tensor_reduce(out=ntl, in_=s16, axis=AX.X, op=ALU.add), [B_s16, B_vec], [B_vec])
            fw.op("dve", lambda e: e.tensor_scalar(out=ntl, in0=ntl, scalar1=512.0, scalar2=None, op0=ALU.mult),
                  [B_vec], [B_vec])
            fw.op("dve", lambda e: e.tensor_tensor(out=s16, in0=ltm, in1=ntl.unsqueeze(1).broadcast_to([128, 16, 16]),
                                                   op=ALU.mult), [B_vec, B_sc0, B_s16], [B_s16])
            fw.op("dve", lambda e: e.tensor_reduce(out=off, in_=s16, axis=AX.X, op=ALU.add), [B_s16, B_vec], [B_vec])
            fw.op("dve", lambda e: e.tensor_tensor(out=rank, in0=rank, in1=cum, op=ALU.add), [B_rank, B_cum], [B_rank])
            fw.op("dve", lambda e: e.tensor_tensor(out=rank, in0=rank, in1=off.unsqueeze(1).broadcast_to([128, J, 16]),
                                                   op=ALU.add), [B_rank, B_vec], [B_rank])
            fw.op("dve", lambda e: e.tensor_tensor(out=smt, in0=rank, in1=Mt, op=ALU.mult), [B_rank, B_M], [B_smt])
            fw.op("dve", lambda e: e.tensor_reduce(out=slots[:, 0, :], in_=smt, axis=AX.X, op=ALU.add),
                  [B_smt], [B_slots])
            fw.op("dve", lambda e: e.tensor_reduce(out=slots[:, 1, :], in_=smt, axis=AX.X, op=ALU.max),
                  [B_smt, B_slots], [B_slots])
            fw.op("dve", lambda e: e.tensor_tensor(out=slots[:, 2, :], in0=slots[:, 0, :], in1=slots[:, 1, :],
                                                   op=ALU.subtract), [B_slots], [B_slots])
            fw.op("dve", lambda e: e.tensor_tensor(out=cum, in0=smt,
                                                   in1=slots[:, 1, :].unsqueeze(2).broadcast_to([128, J, 16]),
                                                   op=ALU.is_equal), [B_smt, B_slots, B_cum], [B_cum])
            fw.op("dve", lambda e: e.tensor_tensor(out=cum, in0=cum, in1=Gall[:], op=ALU.mult), [B_cum, B_G], [B_cum])
            fw.op("dve", lambda e: e.tensor_reduce(out=slots[:, 4, :], in_=cum, axis=AX.X, op=ALU.add),
                  [B_cum, B_slots], [B_slots])
            fw.op("dve", lambda e: e.tensor_reduce(out=slots[:, 3, :], in_=Gall[:], axis=AX.X, op=ALU.add),
                  [B_G, B_slots], [B_slots])
            fw.op("dve", lambda e: e.tensor_tensor(out=slots[:, 5, :], in0=slots[:, 3, :], in1=slots[:, 4, :],
                                                   op=ALU.subtract), [B_slots], [B_slots])
            fw.op("dve", lambda e: e.tensor_copy(out=sloti, in_=slots[:, 1:3, :]), [B_slots], [B_sloti])
            fw.op("dve", lambda e: e.tensor_tensor(out=s48, in0=off.unsqueeze(1).broadcast_to([128, 48, 16]),
                                                   in1=thr48.unsqueeze(2).broadcast_to([128, 48, 16]), op=ALU.is_le),
                  [B_vec, B_sc0], [B_s48])
            fw.op("dve", lambda e: e.tensor_reduce(out=texpf, in_=s48, axis=AX.X, op=ALU.add), [B_s48], [B_texp])
            fw.op("dve", lambda e: e.tensor_scalar(out=texpf, in0=texpf, scalar1=-1.0, scalar2=0.0, op0=ALU.add,
                                                   op1=ALU.max), [B_texp], [B_texp])
            fw.op("dve", lambda e: e.tensor_scalar(out=texpf, in0=texpf, scalar1=15.0, scalar2=None, op0=ALU.min),
                  [B_texp], [B_texp])
            fw.op("dve", lambda e: e.tensor_scalar(out=texpf, in0=texpf, scalar1=128.0, scalar2=pidx[:, 0:1],
                                                   op0=ALU.mult, op1=ALU.add), [B_texp, B_sc0], [B_texp])
            fw.op("dve", lambda e: e.tensor_copy(out=texpi, in_=texpf), [B_texp], [B_texp])
            if dbg and stage == 5:
                dump("G", Gall[:], [128, J, 16], F32, [B_G])
                dump("slots", slots, [128, 6, J], F32, [B_slots])
                dump("sloti", sloti, [128, 2, J], mybir.dt.int32, [B_sloti])
                dump("texpi", texpi, [128, 48], mybir.dt.int32, [B_texp])
                dump("vec", vec, [128, 8, 16], F32, [B_vec])
        if stage >= 6:
            RB.release(mB)
            mB = RB.mark()
            B_hs = Buf("hs_s")
            B_ys = Buf("ys_s")
            h2l = [RB.alloc(f"h2l{i}", [128, D], BF16) for i in range(4)]
            for j in range(J):
                hv, B_hv = h2l[j % 4]
                fw.dma("sp", hv, h2_s[j * 128:(j + 1) * 128, :], [B_h2s], [B_hv], B_hv)
                for k in range(2):
                    fw.dma_fn("pool", lambda e, hv=hv, j=j, k=k: e.indirect_dma_start(
                        out=hs_s[:, :], out_offset=bass.IndirectOffsetOnAxis(ap=sloti[:, k, j:j + 1], axis=0),
                        in_=hv, in_offset=None), [B_hv, B_sloti], [B_hs], B_hv, acc=True)
            RB.release(mB)
            mB = RB.mark()
            mslots = list(ring_slots)
            for i in range(4):
                v, bb = RD.alloc(f"mring{i}", [128, 4096], BF16)
                mslots.append((v, bb))
            hsl = [RB.alloc(f"hsl{i}", [128, 4, D], BF16) for i in range(2)]
            hsT = [RB.alloc(f"hsT{i}", [128, DC, 512], BF16) for i in range(2)]
            gT = [RB.alloc(f"gT{i}", [128, 4, 512], BF16) for i in range(2)]
            sil = [RB.alloc(f"sil{i}", [128, 512], BF16) for i in range(2)]
            ysb = [RB.alloc(f"ysb{i}", [128, 4, D], BF16) for i in range(2)]
            identb, B_idb = RC.alloc("identb", [128, 128], BF16)
            fw.op("dve", lambda e: e.tensor_copy(out=identb, in_=ident[:]), [B_const], [B_idb])
            psb = [ps[i].bitcast(BF16) for i in range(8)]
            wsl = 0
            c5 = 0
            cy = 0
            NSTr = int(os.environ.get("DBG_NST", NST))
            def s4_fetch(s_):
                nonlocal wsl
                wv = []
                for wi, (wsrc, shape) in enumerate(((w1_s, [128, DC, EH]), (w3_s, [128, DC, EH]), (w2_s, [128, 4, D]))):
                    ap_, bf_ = mslots[wsl % 8]
                    wsl += 1
                    v = chunk_view(ap_, shape)
                    rows = wsrc.rearrange("e p a b -> (e p) (a b)")
                    fw.dma_fn("pool", lambda e, s_=s_, rows=rows, ap_=ap_: e.indirect_dma_start(
                        out=ap_, out_offset=None, in_=rows,
                        in_offset=bass.IndirectOffsetOnAxis(ap=texpi[:, s_:s_ + 1], axis=0)),
                        [B_texp] + B_w1, [bf_], bf_)
                    wv.append((v, bf_))
                hl, B_hl = hsl[s_ % 2]
                fw.dma("sp", hl, hs_s[s_ * 512:(s_ + 1) * 512, :].rearrange("(a p) d -> p a d", p=128),
                       [B_hs], [B_hl], B_hl)
                return wv

            fetched = {0: s4_fetch(0)}
            for s_ in range(NSTr):
                if s_ + 1 < NSTr:
                    fetched[s_ + 1] = s4_fetch(s_ + 1)
                (w1, B_w1c), (w3, B_w3c), (w2, B_w2c) = fetched.pop(s_)
                hl, B_hl = hsl[s_ % 2]
                hT, B_hT = hsT[s_ % 2]
                gt, B_gt = gT[s_ % 2]
                yb_, B_yb = ysb[s_ % 2]
                for dc in range(DC):
                    bank = 6 + dc % 2

                    def trb(e, dc=dc, bank=bank, hl=hl):
                        ins = None
                        for a in range(4):
                            ins = e.transpose(psb[bank][:, a * 128:(a + 1) * 128], hl[:, a, dc * 128:(dc + 1) * 128],
                                              identb)
                        return ins
                    fw.op("pe", trb, [B_hl, B_idb], [PS[bank]])
                    evac_copy(beng(bank), hT[:, dc, :], psb[bank][:, 0:512], [PS[bank]], [B_hT])
                for hc in range(4):
                    b1 = 2 * (c5 % 2)
                    b3 = b1 + 1
                    sl, B_sl = sil[c5 % 2]
                    c5 += 1
                    fw.op("pe", mm_group(ps[b1], [(w1[:, kc, hc * 128:(hc + 1) * 128], hT[:, kc, :]) for kc in range(DC)]),
                          [B_w1c, B_hT], [PS[b1]])
                    fw.op("pe", mm_group(ps[b3], [(w3[:, kc, hc * 128:(hc + 1) * 128], hT[:, kc, :]) for kc in range(DC)]),
                          [B_w3c, B_hT], [PS[b3]])
                    fw.op("act", lambda e, sl=sl, b1=b1: e.activation(out=sl, in_=ps[b1], func=AF.Silu), [PS[b1]], [B_sl])
                    fw.op("dve", lambda e, sl=sl, b3=b3, gt=gt, hc=hc: e.tensor_tensor(
                        out=gt[:, hc, :], in0=ps[b3], in1=sl, op=ALU.mult), [PS[b3], B_sl], [B_gt], acc=True)
                for a in range(4):
                    for nb_ in range(2):
                        yb = 4 + (cy % 2)
                        cy += 1
                        fw.op("pe", mm_group(ps[yb], [
                            (gt[:, hc, a * 128:(a + 1) * 128], w2[:, hc, nb_ * 512:(nb_ + 1) * 512]) for hc in range(4)]),
                            [B_gt, B_w2c], [PS[yb]])
                        evac_copy(beng(yb), yb_[:, a, nb_ * 512:(nb_ + 1) * 512], ps[yb], [PS[yb]], [B_yb])
                fw.dma("sp", ys_s[s_ * 512:(s_ + 1) * 512, :].rearrange("(a p) d -> p a d", p=128), yb_,
                       [B_yb], [B_ys], B_yb, acc=True)
            RB.release(mB)
            RD.release(mD)
            mB = RB.mark()
            ya = [RB.alloc(f"ya{i}", [128, D], BF16) for i in range(2)]
            ybb = [RB.alloc(f"yb{i}", [128, D], BF16) for i in range(2)]
            x1l = [RB.alloc(f"x1l{i}", [128, D], F32) for i in range(2)]
            mt_ = [RB.alloc(f"mt{i}", [128, D], F32) for i in range(2)]
            ot = [RB.alloc(f"ot{i}", [128, D], F32) for i in range(2)]
            fing, B_fing = RB.alloc("fing", [128, D], F32)
            g2bc, B_g2 = RB.alloc("g2bc", [128, D], F32)
            fw.dma("sp", fing, fing_d, [], [B_fing], B_fing)
            g2r = [(g2bc, B_g2)] + [RB.alloc("g2bc1", [128, D], F32)]

            def s5_fetch(j):
                s = j % 2
                if j % NT == 0:
                    gv, B_gv = g2r[(j // NT) % 2]
                    fw.dma("sp", gv, gsc_s[j // NT, 1], [B_scr["gsc"]], [B_gv], B_gv)
                yav, B_ya = ya[s]
                ybv, B_ybv = ybb[s]
                xv, B_xv = x1l[s]
                fw.dma("sp", xv, x1_s[j * 128:(j + 1) * 128, :], [B_x1s], [B_xv], B_xv)
                fw.dma_fn("pool", lambda e, yav=yav, j=j: e.indirect_dma_start(
                    out=yav, out_offset=None, in_=ys_s[:, :],
                    in_offset=bass.IndirectOffsetOnAxis(ap=sloti[:, 0, j:j + 1], axis=0)),
                    [B_ys, B_sloti], [B_ya], B_ya)
                fw.dma_fn("pool", lambda e, ybv=ybv, j=j: e.indirect_dma_start(
                    out=ybv, out_offset=None, in_=ys_s[:, :],
                    in_offset=bass.IndirectOffsetOnAxis(ap=sloti[:, 1, j:j + 1], axis=0)),
                    [B_ys, B_sloti], [B_ybv], B_ybv)

            s5_fetch(0)
            for j in range(J):
                b = j // NT
                s = j % 2
                if j + 1 < J:
                    s5_fetch(j + 1)
                gv, B_gv = g2r[b % 2]
                yav, B_ya = ya[s]
                ybv, B_ybv = ybb[s]
                xv, B_xv = x1l[s]
                mv, B_mv = mt_[s]
                o_, B_o = ot[s]
                fw.op("dve", lambda e, mv=mv, yav=yav, j=j: e.tensor_scalar(
                    out=mv, in0=yav, scalar1=slots[:, 4, j:j + 1], scalar2=None, op0=ALU.mult),
                    [B_ya, B_slots], [B_mv])
                fw.op("dve", lambda e, mv=mv, ybv=ybv, j=j: e.scalar_tensor_tensor(
                    out=mv, in0=ybv, scalar=slots[:, 5, j:j + 1], in1=mv, op0=ALU.mult, op1=ALU.add),
                    [B_ybv, B_slots, B_mv], [B_mv])
                fw.op("dve", lambda e, mv=mv, gv=gv: e.tensor_tensor(out=mv, in0=mv, in1=gv, op=ALU.mult),
                      [B_mv, B_gv], [B_mv])
                fw.op("pool", lambda e, mv=mv, xv=xv: e.tensor_tensor(out=xv, in0=xv, in1=mv, op=ALU.add),
                      [B_mv, B_xv], [B_xv])
                rstd, B_sm, _ = rms_stats(xv, [B_xv], o_, B_o, D)
                fw.op("dve", lambda e, o_=o_, xv=xv, rstd=rstd: e.scalar_tensor_tensor(
                    out=o_, in0=xv, scalar=rstd, in1=fing, op0=ALU.mult, op1=ALU.mult),
                    [B_xv, B_sm, B_fing], [B_o])
                fw.dma("sp", out_d[b, (j % NT) * 128:(j % NT + 1) * 128, :], o_, [B_o], [B_out], B_o, acc=True)
            RB.release(mB)

        fw._waits("sp", dict(fw.dma_events))
        fw.emit()
    return nc


def host_inputs(inputs, core, nb=4):
    f = np.float32
    cs = get_consts()
    b0 = core * nb
    m = {}
    m["x"] = np.ascontiguousarray(inputs["x"][b0:b0 + nb], dtype=f)
    m["ctx"] = np.ascontiguousarray(inputs["ctx"][b0:b0 + nb], dtype=f)
    cT = np.zeros((128, DC, 8), f)
    cc = np.asarray(inputs["c"][b0:b0 + nb], dtype=f)
    cT[:, :, :nb] = cc.reshape(nb, DC, 128).transpose(2, 1, 0)
    cT[:, :, 4] = np.asarray(inputs["c_ctx"], dtype=f).reshape(DC, 128).T
    m["cT"] = cT
    m["w_mod"] = np.ascontiguousarray(inputs["w_mod"][0], dtype=f)
    bm = np.asarray(inputs["b_mod"][0], dtype=f)
    m["bmodT"] = np.ascontiguousarray(bm.reshape(48, 128).T)
    m["bmg"] = np.ascontiguousarray(np.broadcast_to(np.concatenate([bm[2048:3072], bm[5120:6144]])[None, :], (128, 2048)))
    m["n1g"] = np.ascontiguousarray(np.asarray(inputs["norm1_g"][0], dtype=f).reshape(DC, 128).T)
    m["n2g"] = np.ascontiguousarray(np.asarray(inputs["norm2_g"][0], dtype=f).reshape(DC, 128).T)
    m["fing"] = np.ascontiguousarray(np.broadcast_to(np.asarray(inputs["final_g"], dtype=f)[None, :], (128, D)))
    lamv = np.concatenate([np.asarray(inputs[k][0], dtype=f) for k in ("lam_q1", "lam_k1", "lam_q2", "lam_k2")])
    m["lamv"] = np.ascontiguousarray(np.broadcast_to(lamv[None, :], (128, 256)))
    m["subg"] = np.ascontiguousarray(np.broadcast_to(np.asarray(inputs["subln_g"][0], dtype=f)[None, :], (128, 128)))
    wr = np.concatenate([np.asarray(inputs["w_router_group"][0], dtype=f),
                         np.asarray(inputs["w_router_expert"][0], dtype=f)], axis=1)
    m["wr"] = np.ascontiguousarray(wr.reshape(DC, 128, 20).transpose(1, 0, 2))
    br = np.concatenate([np.asarray(inputs["b_router_group"][0], dtype=f),
                         np.asarray(inputs["b_router_expert"][0], dtype=f)])
    m["br"] = np.ascontiguousarray(np.broadcast_to(br[None, :], (128, 20)))
    m["w_in"] = np.ascontiguousarray(inputs["w_in"][0], dtype=f)
    m["w_ao"] = np.ascontiguousarray(inputs["w_attn_out"][0], dtype=f)
    m["w_fo"] = np.ascontiguousarray(inputs["w_four_out"][0], dtype=f)
    m["w_o"] = np.ascontiguousarray(inputs["w_out"][0], dtype=f)
    m["w1"] = np.ascontiguousarray(inputs["w_exp_gate"][0], dtype=f)
    m["w3"] = np.ascontiguousarray(inputs["w_exp_up"][0], dtype=f)
    m["w2"] = np.ascontiguousarray(inputs["w_exp_down"][0], dtype=f)
    m["bmg"] = np.ascontiguousarray(np.broadcast_to(
        np.concatenate([bm[2048:3072], bm[5120:6144], bm[3072:4096], bm[4096:5120]])[None, :], (128, 4096)))
    m["n2gbc"] = np.ascontiguousarray(np.broadcast_to(np.asarray(inputs["norm2_g"][0], dtype=f)[None, :], (128, D)))
    for k in ("ident", "rt", "cossin", "cs_c", "dft", "ustrict", "thr16", "thr48", "ltmask", "pidx"):
        m[k] = cs[k]
    return m


def kernel(**inputs):
    nb = 4
    nc = build(nb)
    in_maps = [host_inputs(inputs, c, nb) for c in range(N_CORES)]
    res = run_bass_kernel_spmd(nc, in_maps, core_ids=list(range(N_CORES)))
    return np.concatenate([np.asarray(r["out"]) for r in res.results], axis=0).astype(np.float32)
```

```python
import contextlib
import math
import os
import numpy as np
import ml_dtypes
import concourse.bass as bass
import concourse.mybir as mybir
from concourse.bass_utils import run_bass_kernel_spmd

F32 = mybir.dt.float32
BF16 = mybir.dt.bfloat16
AF = mybir.ActivationFunctionType
ALU = mybir.AluOpType
AX = mybir.AxisListType

D = 1024
DC = 8
N = 2048
NT = 16
CTX = 256
NK = N + CTX
KC = NK // 128
NH = 8
PROJ_W = 5632
Q_OFF, K_OFF, V_OFF, F_OFF, GA_OFF, GF_OFF = 0, 1024, 2048, 3072, 3584, 4608
NE = 16
EH = 512
EPS = 1e-6
LAM_INIT = 0.8 - 0.6 * math.exp(-0.3 * 0)
ATTN_SCALE = 64 ** -0.5
N_CORES = 8


class Buf:
    __slots__ = ("name", "last_w", "readers", "dsem", "dcount")

    def __init__(self, name):
        self.name = name
        self.last_w = {}
        self.readers = {}
        self.dsem = None
        self.dcount = 0


class Fw:
    ENG = ("pe", "act", "dve", "pool", "sp")

    def __init__(self, nc, stack):
        self.nc = nc
        self.stack = stack
        self.sems = []
        self.q = {e: [] for e in self.ENG}
        self.esem = {e: self.newsem("e_" + e) for e in ("pe", "act", "dve", "pool")}
        self.ecnt = {e: 0 for e in ("pe", "act", "dve", "pool")}
        self.waited = {e: {} for e in self.ENG}
        self.dma_events = {}
        self.dsem_by_name = {}

    def newsem(self, name):
        s = self.stack.enter_context(self.nc.semaphore(name))
        self.sems.append(s)
        return len(self.sems) - 1

    def _deps(self, reads, writes, acc=False):
        evs = {}

        def add(s, v):
            if evs.get(s, 0) < v:
                evs[s] = v

        for b in reads:
            for s, v in b.last_w.items():
                add(s, v)
        for b in writes:
            if not acc:
                for s, v in b.last_w.items():
                    add(s, v)
            for s, v in b.readers.items():
                add(s, v)
        return evs

    def _waits(self, eng, evs):
        w = self.waited[eng]
        own = self.esem.get(eng) if eng == "pe" else None
        for s, v in evs.items():
            if s == own:
                continue
            if w.get(s, 0) < v:
                w[s] = v
                self.q[eng].append(("w", s, v))

    def _record(self, ev, reads, writes, acc=False):
        for b in writes:
            if acc:
                if b.last_w.get(ev[0], 0) < ev[1]:
                    b.last_w[ev[0]] = ev[1]
            else:
                b.last_w = {ev[0]: ev[1]}
                b.readers = {}
        for b in reads:
            if b not in writes:
                if b.readers.get(ev[0], 0) < ev[1]:
                    b.readers[ev[0]] = ev[1]

    def op(self, eng, fn, reads=(), writes=(), acc=False):
        self._waits(eng, self._deps(reads, writes, acc))
        self.ecnt[eng] += 1
        ev = (self.esem[eng], self.ecnt[eng])
        self.q[eng].append(("i", fn, self.esem[eng]))
        self._record(ev, reads, writes, acc)
        return ev

    def dma(self, q, out_ap, in_ap, reads, writes, sembuf, acc=False):
        self._waits(q, self._deps(reads, writes, acc))
        if sembuf.name not in self.dsem_by_name:
            self.dsem_by_name[sembuf.name] = [self.newsem("d_" + sembuf.name), 0]
        ent = self.dsem_by_name[sembuf.name]
        ent[1] += 16
        sembuf.dsem, sembuf.dcount = ent[0], ent[1]
        ev = (sembuf.dsem, sembuf.dcount)
        self.dma_events[sembuf.dsem] = sembuf.dcount
        self.q[q].append(("d", out_ap, in_ap, sembuf.dsem))
        self._record(ev, reads, writes, acc)
        return ev

    def dma_fn(self, q, fn, reads, writes, sembuf, acc=False):
        self._waits(q, self._deps(reads, writes, acc))
        if sembuf.name not in self.dsem_by_name:
            self.dsem_by_name[sembuf.name] = [self.newsem("d_" + sembuf.name), 0]
        ent = self.dsem_by_name[sembuf.name]
        ent[1] += 16
        sembuf.dsem, sembuf.dcount = ent[0], ent[1]
        ev = (sembuf.dsem, sembuf.dcount)
        self.dma_events[sembuf.dsem] = sembuf.dcount
        self.q[q].append(("f", fn, sembuf.dsem))
        self._record(ev, reads, writes, acc)
        return ev

    def wait_bufs(self, eng, bufs):
        evs = {}
        for b in bufs:
            for s, v in list(b.last_w.items()) + list(b.readers.items()):
                if evs.get(s, 0) < v:
                    evs[s] = v
        self._waits(eng, evs)

    def emit(self):
        nc = self.nc
        sems = self.sems

        def run(e, items):
            for it in items:
                if it[0] == "w":
                    e.wait_ge(sems[it[1]], it[2])
                elif it[0] == "i":
                    ins = it[1](e)
                    ins.then_inc(sems[it[2]], 1)
                elif it[0] == "f":
                    ins = it[1](e)
                    ins.then_inc(sems[it[2]], 16)
                else:
                    e.dma_start(out=it[1], in_=it[2]).then_inc(sems[it[3]], 16)

        with nc.Block() as block:

            @block.tensor
            def _(t):
                run(t, self.q["pe"])

            @block.scalar
            def _(t):
                run(t, self.q["act"])

            @block.vector
            def _(t):
                run(t, self.q["dve"])

            @block.gpsimd
            def _(t):
                run(t, self.q["pool"])

            @block.sync
            def _(t):
                run(t, self.q["sp"])


def _bf(a):
    return np.ascontiguousarray(a.astype(ml_dtypes.bfloat16))


def make_consts():
    c = {}
    c["ident"] = np.eye(128, dtype=np.float32)
    R = np.zeros((128, 128), np.float32)
    for i in range(128):
        if (i % 32) < 16:
            R[i, i + 16] = -1.0
        else:
            R[i, i - 16] = 1.0
    c["rt"] = _bf(R.T)
    inv = (1.0 / (np.float32(10000.0) ** (np.arange(16, dtype=np.float32) / np.float32(16)))).astype(np.float32)
    tok = np.arange(N)
    row = (tok // 64).astype(np.float32)
    col = (tok % 64).astype(np.float32)
    cs = np.zeros((128, 2, N), np.float32)
    for p in range(128):
        hh = (p % 64) // 32
        f = p % 16
        ang = ((row if hh == 0 else col) * inv[f]).astype(np.float32)
        cs[p, 0] = np.cos(ang)
        cs[p, 1] = np.sin(ang)
    c["cossin"] = _bf(cs)
    k = np.arange(128, dtype=np.float64)
    a = 2 * np.pi * np.outer(k, k) / 128.0
    c["cs_c"] = _bf(np.concatenate([np.cos(a), np.sin(a)], axis=1) / np.sqrt(128.0))
    n = np.arange(N, dtype=np.int64)
    prod = np.outer(n, n) % N
    ang = 2 * np.pi * prod.astype(np.float64) / N
    tabs = np.stack([np.cos(ang), -np.sin(ang)], 0) / np.sqrt(float(N))
    t = tabs.reshape(2, 4, 4, 128, 4, 512)
    t = t.transpose(0, 4, 1, 3, 2, 5)
    c["dft"] = _bf(t)
    kk = np.arange(128)
    c["ustrict"] = (kk[:, None] < kk[None, :]).astype(np.float32)
    c["thr16"] = np.ascontiguousarray(np.broadcast_to((512.0 * np.arange(16, dtype=np.float32))[None, :], (128, 16)))
    c["thr48"] = np.ascontiguousarray(np.broadcast_to((512.0 * np.arange(48, dtype=np.float32))[None, :], (128, 48)))
    ee = np.arange(16)
    lt = (ee[None, :] < ee[:, None]).astype(np.float32)
    c["ltmask"] = np.ascontiguousarray(np.broadcast_to(lt[None], (128, 16, 16)))
    c["pidx"] = np.arange(128, dtype=np.float32).reshape(128, 1)
    return c


_CONSTS = None


def get_consts():
    global _CONSTS
    if _CONSTS is None:
        _CONSTS = make_consts()
    return _CONSTS


_DTSZ = {F32: 4, BF16: 2, mybir.dt.int32: 4}


class Region:
    def __init__(self, tensor, nbytes):
        self.t = tensor
        self.n = nbytes
        self.off = 0
        self.live = []
        self.hist = {}

    def newbuf(self, name):
        b = Buf(name)
        b.readers = dict(self.hist)
        self.live.append(b)
        return b

    def alloc(self, name, shape, dt):
        nbytes = int(np.prod(shape[1:])) * _DTSZ[dt]
        nbytes_al = (nbytes + 31) // 32 * 32
        assert self.off + nbytes_al <= self.n, (name, self.off, nbytes_al, self.n)
        v = self.t[:, self.off:self.off + nbytes].bitcast(dt)
        if len(shape) == 3:
            v = v.rearrange("p (a b) -> p a b", b=shape[2])
        self.off += nbytes_al
        return v, self.newbuf(name)

    def mark(self):
        return (self.off, len(self.live))

    def release(self, m):
        for b in self.live[m[1]:]:
            for ev in list(b.last_w.items()) + list(b.readers.items()):
                if self.hist.get(ev[0], 0) < ev[1]:
                    self.hist[ev[0]] = ev[1]
        del self.live[m[1]:]
        self.off = m[0]


def mm_group(out_ap, pairs):
    def fn(e):
        ins = None
        n = len(pairs)
        for i, (l, r) in enumerate(pairs):
            ins = e.matmul(out_ap, lhsT=l, rhs=r, start=(i == 0), stop=(i == n - 1))
        return ins
    return fn


def build(nb=4, stage=99, dbg=False):
    nc = bass.Bass("TRN2", target_bir_lowering=False)
    U8 = mybir.dt.uint8

    def inp(name, shape, dt=F32):
        return nc.dram_tensor(name, list(shape), dt, kind="ExternalInput").ap()

    x_d = inp("x", [nb, N, D])
    ctx_d = inp("ctx", [nb, CTX, D])
    cT_d = inp("cT", [128, DC, 8])
    wmod_d = inp("w_mod", [D, 6 * D])
    bmodT_d = inp("bmodT", [128, 48])
    bmg_d = inp("bmg", [128, 4 * D])
    n2gbc_d = inp("n2gbc", [128, D])
    ustrict_d = inp("ustrict", [128, 128])
    thr16_d = inp("thr16", [128, 16])
    thr48_d = inp("thr48", [128, 48])
    ltmask_d = inp("ltmask", [128, 16, 16])
    pidx_d = inp("pidx", [128, 1])
    n1g_d = inp("n1g", [128, DC])
    n2g_d = inp("n2g", [128, DC])
    fing_d = inp("fing", [128, D])
    lamv_d = inp("lamv", [128, 256])
    subg_d = inp("subg", [128, 128])
    wr_d = inp("wr", [128, DC, 20])
    br_d = inp("br", [128, 20])
    win_d = inp("w_in", [D, PROJ_W])
    wao_d = inp("w_ao", [D, D])
    wfo_d = inp("w_fo", [512, D])
    wo_d = inp("w_o", [D, D])
    w1_d = inp("w1", [NE, D, EH])
    w3_d = inp("w3", [NE, D, EH])
    w2_d = inp("w2", [NE, EH, D])
    ident_d = inp("ident", [128, 128])
    rt_d = inp("rt", [128, 128], BF16)
    cossin_d = inp("cossin", [128, 2, N], BF16)
    csc_d = inp("cs_c", [128, 256], BF16)
    dft_d = inp("dft", [2, 4, 4, 128, 4, 512], BF16)
    out_d = nc.dram_tensor("out", [nb, N, D], F32, kind="ExternalOutput").ap()

    def scr(name, shape, dt=BF16):
        return nc.dram_tensor(name, list(shape), dt).ap()

    wqkv_s = scr("wqkv_s", [NH, 128, DC, 384])
    wf_s = scr("wf_s", [128, DC, 512])
    wg_s = scr("wg_s", [4, 128, DC, 512])
    wao_s = scr("wao_s", [2, 128, DC, 512])
    wfo_s = scr("wfo_s", [128, 4, D])
    wo_s = scr("wo_s", [2, 128, DC, 512])
    w1_s = scr("w1_s", [NE, 128, DC, EH])
    w3_s = scr("w3_s", [NE, 128, DC, EH])
    w2_s = scr("w2_s", [NE, 128, 4, D])
    gsc_s = scr("gsc_s", [nb, 4, 128, D], F32)
    J = nb * NT
    NST = nb * 8 + 16
    x1_s = scr("x1_s", [nb * N, D], F32)
    h2_s = scr("h2_s", [nb * N, D], BF16)
    hs_s = scr("hs_s", [NST * 512, D], BF16)
    ys_s = scr("ys_s", [NST * 512, D], BF16)

    with contextlib.ExitStack() as st:
        fw = Fw(nc, st)

        def sb(name, shape, dt):
            return st.enter_context(nc.sbuf_tensor(name, list(shape), dt))

        regA_t = sb("regA", [128, DC * NK * 2], U8)
        regB_t = sb("regB", [128, 65536], U8)
        regC_t = sb("regC", [128, 32768], U8)
        regD_t = sb("regD", [128, 32768], U8)
        ring_t = sb("ring", [128, 32768], U8)
        ident = sb("ident_sb", [128, 128], F32)
        rt = sb("rt_sb", [128, 128], BF16)
        csc = sb("csc_sb", [128, 256], BF16)
        smallt = sb("small", [128, 16, 16], F32)
        modA1 = sb("modA1", [128, DC, 8], F32)
        modB1 = sb("modB1", [128, DC, 8], F32)
        modA2 = sb("modA2", [128, DC, 8], F32)
        modB2 = sb("modB2", [128, DC, 8], F32)
        subg = sb("subg_sb", [128, 128], F32)
        lamt = sb("lamt", [128, 8], F32)
        epst = sb("epst", [128, 8], F32)
        Gall = sb("Gall", [128, nb * NT, 16], F32)

        HT = regA_t[:, :].bitcast(BF16).rearrange("p (a b) -> p a b", b=NK)
        RB = Region(regB_t, 65536)
        RC = Region(regC_t, 32768)
        RD = Region(regD_t, 32768)

        psbig = st.enter_context(nc.psum_tensor("psbig", [128, 4096], F32))
        ps = [psbig[:, i * 512:(i + 1) * 512] for i in range(8)]
        PS = [Buf(f"ps{i}") for i in range(8)]

        B_const = Buf("const")
        B_HT = [Buf(f"HT{i}") for i in range(5)]
        B_mod = Buf("mod")
        B_lam = Buf("lam")
        B_scr = {k: Buf(k) for k in ("wqkv", "wf", "wg", "wao", "wfo", "wo", "gsc")}
        B_w1 = [Buf(f"w1s{e}") for e in range(NE)]
        B_out = Buf("outd")
        B_G = Buf("Gall")
        B_x1s = Buf("x1s")
        B_h2s = Buf("h2s")
        B_small = [Buf(f"small{i}") for i in range(16)]
        small_ctr = [0]

        def small_alloc():
            i = small_ctr[0] % 16
            small_ctr[0] += 1
            return smallt[:, i, :], B_small[i]

        ring_slots = [(ring_t[:, i * 8192:(i + 1) * 8192].bitcast(BF16), Buf(f"ring{i}")) for i in range(4)]

        def chunk_view(ap, shape):
            n = int(np.prod(shape[1:]))
            return ap[:, 0:n].rearrange("p (a b) -> p a b", b=shape[2])

        class Ring:
            def __init__(self, slots, la):
                self.slots = slots
                self.la = la
                self.sched = []
                self.issued = 0
                self.taken = 0
                self.views = {}

            def plan(self, src_ap, shape, srcbuf):
                self.sched.append((src_ap, shape, srcbuf))

            def _issue(self, i):
                src_ap, shape, srcbuf = self.sched[i]
                ap, buf = self.slots[i % len(self.slots)]
                v = chunk_view(ap, shape)
                fw.dma("sp", v, src_ap, [srcbuf], [buf], buf)
                return v, buf

            def take(self):
                i = self.taken
                self.taken += 1
                while self.issued < min(len(self.sched), i + 1 + self.la):
                    self.views[self.issued] = self._issue(self.issued)
                    self.issued += 1
                return self.views.pop(i)

        fw.dma("sp", ident[:], ident_d, [], [B_const], B_const)
        fw.dma("sp", rt[:], rt_d, [], [B_const], B_const)
        fw.dma("sp", csc[:], csc_d, [], [B_const], B_const)
        B_subg = Buf("subg")
        fw.dma("sp", subg[:], subg_d, [], [B_subg], B_subg)
        B_eps = Buf("eps")
        fw.op("pool", lambda e: e.memset(epst[:], EPS), [], [B_eps])

        def wview(w, c0, cw):
            return w[:, c0:c0 + cw].rearrange("(kc p) j -> p kc j", p=128)

        for h in range(NH):
            for sec, off in enumerate((Q_OFF, K_OFF, V_OFF)):
                fw.dma("pool", wqkv_s[h, :, :, sec * 128:(sec + 1) * 128],
                       wview(win_d, off + h * 128, 128), [], [B_scr["wqkv"]], B_scr["wqkv"])
        fw.dma("pool", wf_s, wview(win_d, F_OFF, 512), [], [B_scr["wf"]], B_scr["wf"])
        for i, off in enumerate((GA_OFF, GA_OFF + 512, GF_OFF, GF_OFF + 512)):
            fw.dma("pool", wg_s[i], wview(win_d, off, 512), [], [B_scr["wg"]], B_scr["wg"])
        for i in range(2):
            fw.dma("pool", wao_s[i], wview(wao_d, i * 512, 512), [], [B_scr["wao"]], B_scr["wao"])
            fw.dma("pool", wo_s[i], wview(wo_d, i * 512, 512), [], [B_scr["wo"]], B_scr["wo"])
        fw.dma("pool", wfo_s, wview(wfo_d, 0, D), [], [B_scr["wfo"]], B_scr["wfo"])
        for e in range(NE):
            fw.dma("pool", w1_s[e], wview(w1_d[e], 0, EH), [], [B_w1[e]], B_w1[e])
            fw.dma("pool", w3_s[e], wview(w3_d[e], 0, EH), [], [B_w1[e]], B_w1[e])
            fw.dma("pool", w2_s[e], wview(w2_d[e], 0, D), [], [B_w1[e]], B_w1[e])

        mD = RD.mark()
        mC = RC.mark()
        wm, B_wm = RD.alloc("wm", [128, DC, D], F32)
        rep, B_rep = RC.alloc("rep", [128, 4 * DC, 128], F32)
        sc, B_sc = RC.alloc("sc", [128, DC, 8], F32)
        modT, B_modT = RC.alloc("modT", [128, 6 * DC, 8], F32)
        bmodT, B_p0c = RC.alloc("bmodT", [128, 48, 1], F32)
        n1g, _ = RC.alloc("n1g", [128, DC, 1], F32)
        n2g, _ = RC.alloc("n2g", [128, DC, 1], F32)
        mB0 = RB.mark()
        wm2, B_wm2 = RB.alloc("wm2", [128, DC, D], F32)
        dgt = [RB.alloc(f"dg{i}", [128, 128], F32) for i in range(4)]
        gtmp0, B_gt0 = RC.alloc("gtmp0", [128, 512], F32)
        gtmp1, B_gt1 = RC.alloc("gtmp1", [128, 512], F32)
        gtmp = [gtmp0, gtmp1]
        B_gtmp = [B_gt0, B_gt1]
        ones_t, B_ones = RC.alloc("ones", [128, 128], F32)
        lamv, B_lamv = RC.alloc("lamv", [128, 256], F32)
        lamp, B_lamp = RC.alloc("lamp", [128, 2, 64], F32)

        fw.dma("sp", sc, cT_d, [], [B_sc], B_sc)
        fw.dma("sp", bmodT[:, :, 0], bmodT_d, [], [B_p0c], B_p0c)
        fw.dma("sp", n1g[:, :, 0], n1g_d, [], [B_p0c], B_p0c)
        fw.dma("sp", n2g[:, :, 0], n2g_d, [], [B_p0c], B_p0c)
        fw.dma("sp", lamv, lamv_d, [], [B_lamv], B_lamv)
        fw.op("dve", lambda e: e.tensor_tensor(out=lamp[:, 0, :], in0=lamv[:, 0:64], in1=lamv[:, 64:128],
                                               op=ALU.mult), [B_lamv], [B_lamp])
        fw.op("dve", lambda e: e.tensor_tensor(out=lamp[:, 1, :], in0=lamv[:, 128:192], in1=lamv[:, 192:256],
                                               op=ALU.mult), [B_lamv, B_lamp], [B_lamp])
        fw.op("dve", lambda e: e.tensor_reduce(out=lamt[:, 0:2], in_=lamp, axis=AX.X, op=ALU.add),
              [B_lamp], [B_lam])
        fw.op("act", lambda e: e.activation(out=lamt[:, 0:2], in_=lamt[:, 0:2], func=AF.Exp), [B_lam], [B_lam])
        fw.op("dve", lambda e: e.scalar_tensor_tensor(out=lamt[:, 2:3], in0=lamt[:, 1:2], scalar=-LAM_INIT,
                                                      in1=lamt[:, 0:1], op0=ALU.add, op1=ALU.subtract),
              [B_lam], [B_lam])
        fw.op("dve", lambda e: e.tensor_scalar(out=subg[:], in0=subg[:], scalar1=1.0 - LAM_INIT, scalar2=None,
                                               op0=ALU.mult), [B_subg], [B_subg])
        fw.op("act", lambda e: e.activation(out=sc, in_=sc, func=AF.Silu), [B_sc], [B_sc])
        fw.op("dve", lambda e: e.memset(ones_t, 1.0), [], [B_ones])
        for j in range(6):
            wmj, B_wmj = (wm, B_wm) if j % 2 == 0 else (wm2, B_wm2)
            fw.dma("sp", wmj, wmod_d[:, j * D:(j + 1) * D].rearrange("(kc p) f -> p kc f", p=128),
                   [], [B_wmj], B_wmj)

            def mm_feat(e, wmj=wmj):
                ins = None
                for fc in range(DC):
                    for kc in range(DC):
                        ins = e.matmul(ps[0][:, fc * 8:(fc + 1) * 8], lhsT=wmj[:, kc, fc * 128:(fc + 1) * 128],
                                       rhs=sc[:, kc, :], start=(kc == 0), stop=(kc == DC - 1))
                return ins
            fw.op("pe", mm_feat, [B_wmj, B_sc], [PS[0]])
            fw.op("dve", lambda e, j=j: e.tensor_tensor(
                out=modT[:, j * DC:(j + 1) * DC, :],
                in0=ps[0][:, 0:64].rearrange("p (a b) -> p a b", b=8),
                in1=bmodT[:, j * DC:(j + 1) * DC, :].broadcast_to([128, DC, 8]), op=ALU.add),
                [PS[0], B_p0c], [B_modT], acc=True)
        fw.op("dve", lambda e: e.scalar_tensor_tensor(
            out=modA1[:], in0=modT[:, 1 * DC:2 * DC, :], scalar=1.0, in1=n1g.broadcast_to([128, DC, 8]),
            op0=ALU.add, op1=ALU.mult), [B_modT, B_p0c], [B_mod], acc=True)
        fw.op("dve", lambda e: e.tensor_copy(out=modB1[:], in_=modT[:, 0:DC, :]), [B_modT], [B_mod], acc=True)
        fw.op("dve", lambda e: e.scalar_tensor_tensor(
            out=modA2[:], in0=modT[:, 4 * DC:5 * DC, :], scalar=1.0, in1=n2g.broadcast_to([128, DC, 8]),
            op0=ALU.add, op1=ALU.mult), [B_modT, B_p0c], [B_mod], acc=True)
        fw.op("dve", lambda e: e.tensor_copy(out=modB2[:], in_=modT[:, 3 * DC:4 * DC, :]), [B_modT], [B_mod], acc=True)
        row_src = (lambda fc, b: modT[:, 2 * DC + fc, b:b + 1], lambda fc, b: modT[:, 5 * DC + fc, b:b + 1],
                   lambda fc, b: modB2[:, fc, b:b + 1], lambda fc, b: modA2[:, fc, b:b + 1])
        dgc = 0
        rbk = 0
        for which in range(4):
            for b in range(nb):
                for half in range(2):
                    bank = 1 + (rbk % 4)
                    rbk += 1
                    for q in range(4):
                        fc = half * 4 + q
                        dg, B_dg = dgt[dgc % 4]
                        dgc += 1
                        fw.op("dve", lambda e, dg=dg, v=row_src[which](fc, b): e.tensor_scalar(
                            out=dg, in0=ident[:], scalar1=v, scalar2=None, op0=ALU.mult),
                            [B_const, B_modT, B_mod], [B_dg])
                        fw.op("pe", lambda e, dg=dg, bank=bank, q=q: e.matmul(
                            ps[bank][:, q * 128:(q + 1) * 128], lhsT=ones_t, rhs=dg, start=True, stop=True),
                            [B_dg, B_ones], [PS[bank]])
                    if bank % 2 == 0:
                        fw.op("dve", lambda e, half=half, bank=bank: e.tensor_copy(out=gtmp[half], in_=ps[bank]),
                              [PS[bank]], [B_gtmp[half]])
                    else:
                        fw.op("act", lambda e, half=half, bank=bank: e.activation(out=gtmp[half], in_=ps[bank],
                                                                                  func=AF.Identity),
                              [PS[bank]], [B_gtmp[half]])
                    fw.dma("sp", gsc_s[b, which, :, half * 512:(half + 1) * 512], gtmp[half],
                           [B_gtmp[half]], [B_scr["gsc"]], B_gtmp[half], acc=True)

        dbgq = []

        def dump(name, ap, shape, dt, bufs):
            d = nc.dram_tensor("dbg_" + name, list(shape), dt, kind="ExternalOutput").ap()
            bb = Buf("dbg_" + name)
            fw.dma("sp", d, ap, bufs, [], bb)
            dbgq.append(bb)

        if dbg and stage == 0:
            for nm, t in (("modA1", modA1), ("modB1", modB1), ("modA2", modA2), ("modB2", modB2)):
                dump(nm, t[:], [128, DC, 8], F32, [B_mod])
            fw.wait_bufs("sp", B_gtmp)
            dump("gsc", gsc_s, [nb, 4, 128, D], F32, [B_scr["gsc"]])
            dump("w1s", w1_s[3], [128, DC, EH], BF16, [B_w1[3]])
            dump("lam", lamt[:], [128, 8], F32, [B_lam])
        RD.release(mD)
        RC.release(mC)
        RB.release(mB0)

        def rms_stats(src_ap, src_bufs, junk, B_junk, n):
            sm, B_sm = small_alloc()
            ss = sm[:, 0:1]
            rstd = sm[:, 1:2]
            fw.op("act", lambda e: e.activation(out=junk, in_=src_ap, func=AF.Square, accum_out=ss),
                  src_bufs, [B_junk, B_sm])
            fw.op("act", lambda e: e.activation(out=rstd, in_=ss, func=AF.Ln, scale=1.0 / n, bias=epst[:, 0:1]),
                  [B_sm, B_eps], [B_sm])
            fw.op("act", lambda e: e.activation(out=rstd, in_=rstd, func=AF.Exp, scale=-0.5), [B_sm], [B_sm])
            return rstd, B_sm, sm

        def evac_affine(eng, out_ap, in_ap, scale_ap, bias_ap, reads, writes):
            if eng == "dve":
                fw.op("dve", lambda e: e.tensor_scalar(out=out_ap, in0=in_ap, scalar1=scale_ap, scalar2=bias_ap,
                                                       op0=ALU.mult, op1=ALU.add), reads, writes, acc=True)
            else:
                fw.op("act", lambda e: e.activation(out=out_ap, in_=in_ap, func=AF.Identity, scale=scale_ap,
                                                    bias=bias_ap), reads, writes, acc=True)

        def evac_copy(eng, out_ap, in_ap, reads, writes):
            if eng == "dve":
                fw.op("dve", lambda e: e.tensor_copy(out=out_ap, in_=in_ap), reads, writes, acc=True)
            else:
                fw.op("act", lambda e: e.activation(out=out_ap, in_=in_ap, func=AF.Identity), reads, writes, acc=True)

        def beng(bank):
            return "dve" if bank % 2 == 0 else "act"

        for b in range(nb if stage > 0 else 0):
            ringA = Ring(ring_slots, 2)
            ringA.plan(wf_s, [128, DC, 512], B_scr["wf"])
            for h in range(NH):
                ringA.plan(wqkv_s[h], [128, DC, 384], B_scr["wqkv"])
            for cb in range(2):
                ringA.plan(wfo_s, [128, 4, D], B_scr["wfo"])
                ringA.plan(wg_s[2 + cb], [128, DC, 512], B_scr["wg"])
                ringA.plan(wg_s[cb], [128, DC, 512], B_scr["wg"])
                ringA.plan(wao_s[cb], [128, DC, 512], B_scr["wao"])
            ringA.plan(wo_s[0], [128, DC, 512], B_scr["wo"])
            ringA.plan(wo_s[1], [128, DC, 512], B_scr["wo"])

            mC = RC.mark()
            xt = []
            xn = []
            for i in range(2):
                xt.append(RC.alloc(f"xt{i}", [128, D], F32))
                xn.append(RC.alloc(f"xn{i}", [128, D], F32))
            tcount = 0

            def norm_tile(src_ap, A, Bm, bcol, HTbuf, tok0):
                nonlocal tcount
                s = tcount % 2
                tcount += 1
                xts, B_xts = xt[s]
                xns, B_xns = xn[s]
                fw.dma("sp", xts, src_ap, [], [B_xts], B_xts)
                rstd, B_sm, _ = rms_stats(xts, [B_xts], xns, B_xns, D)
                fw.op("act", lambda e: e.activation(out=xns, in_=xts, func=AF.Identity, scale=rstd),
                      [B_xts, B_sm], [B_xns])
                return lambda: norm_tile_b(s, xns, B_xns, A, Bm, bcol, HTbuf, tok0)

            def norm_tile_b(s, xns, B_xns, A, Bm, bcol, HTbuf, tok0):
                for half in range(2):
                    bank = 4 + 2 * s + half

                    def tr(e, half=half, bank=bank):
                        ins = None
                        for j in range(4):
                            dc = half * 4 + j
                            ins = e.transpose(ps[bank][:, j * 128:(j + 1) * 128], xns[:, dc * 128:(dc + 1) * 128],
                                              ident[:])
                        return ins
                    fw.op("pe", tr, [B_xns, B_const], [PS[bank]])
                    for j in range(4):
                        dc = half * 4 + j
                        evac_affine(beng(bank), HT[:, dc, tok0:tok0 + 128], ps[bank][:, j * 128:(j + 1) * 128],
                                    A[:, dc, bcol:bcol + 1], Bm[:, dc, bcol:bcol + 1], [PS[bank], B_mod], [HTbuf])

            p1args = [(ctx_d[b, t * 128:(t + 1) * 128, :], modA1, modB1, 4, B_HT[0], t * 128) for t in range(2)]
            p1args += [(x_d[b, t * 128:(t + 1) * 128, :], modA1, modB1, b, B_HT[1 + t // 4], CTX + t * 128)
                       for t in range(NT)]
            pend = norm_tile(*p1args[0])
            for t in range(len(p1args)):
                nxt_b = norm_tile(*p1args[t + 1]) if t + 1 < len(p1args) else None
                pend()
                pend = nxt_b
            if stage == 1:
                if dbg:
                    dump("HT", HT, [128, DC, NK], BF16, B_HT)
                break

            wf, B_wf = ringA.take()
            FT = [RC.alloc(f"FT{i}", [128, 4, 512], BF16) for i in range(2)]
            mB = RB.mark()
            ZT, B_ZT = RB.alloc("ZT", [128, 4, N], BF16)
            mB2 = RB.mark()
            AB, _ = RB.alloc("AB", [128, NT, 1024], BF16)
            B_AB = [RB.newbuf(f"AB{i}") for i in range(4)]
            mD = RD.mark()
            dring = [RD.alloc(f"dft{i}", [128, 4, 512], BF16) for i in range(4)]
            cdft = 0
            for tb in range(4):
                ft, B_ft = FT[tb % 2]
                for g in range(4):
                    fw.op("pe", mm_group(ps[g][:, :], [
                        (wf[:, kc, g * 128:(g + 1) * 128], HT[:, kc, CTX + tb * 512:CTX + (tb + 1) * 512])
                        for kc in range(DC)]), [B_wf, B_HT[1 + tb]], [PS[g]])
                    evac_copy(beng(g), ft[:, g, :], ps[g][:, :], [PS[g]], [B_ft])
                for tt in range(4):
                    tile = tb * 4 + tt
                    for gp in range(2):
                        bank = 4 + (cdft % 2)
                        cdft += 1

                        def cdf(e, gp=gp, tt=tt, bank=bank, ft=ft):
                            ins = None
                            for k in range(2):
                                ins = e.matmul(ps[bank][:, k * 256:(k + 1) * 256],
                                               lhsT=ft[:, 2 * gp + k, tt * 128:(tt + 1) * 128], rhs=csc[:, :],
                                               start=True, stop=True)
                            return ins
                        fw.op("pe", cdf, [B_ft, B_const], [PS[bank]])
                        evac_copy(beng(bank), AB[:, tile, gp * 512:(gp + 1) * 512], ps[bank][:, :], [PS[bank]], [B_AB[tb]])
            for mb in range(4):
                for piece in range(8):
                    kind, nq = piece // 4, piece % 4
                    dv, B_dv = dring[(mb * 8 + piece) % 4]
                    fw.dma("sp", dv, dft_d[kind, mb, nq], [], [B_dv], B_dv)

                    def sdf(e, piece=piece, kind=kind, nq=nq, dv=dv):
                        ins = None
                        for g in range(4):
                            for n_ in range(4):
                                ins = e.matmul(ps[g][:, :],
                                               lhsT=AB[:, nq * 4 + n_, g * 256 + kind * 128:g * 256 + (kind + 1) * 128],
                                               rhs=dv[:, n_, :], start=(piece == 0 and n_ == 0),
                                               stop=(piece == 7 and n_ == 3))
                        return ins
                    fw.op("pe", sdf, [B_dv] + B_AB, PS[0:4])
                for g in range(4):
                    evac_copy(beng(g), ZT[:, g, mb * 512:(mb + 1) * 512], ps[g][:, :], [PS[g]], [B_ZT])
            RD.release(mD)
            RB.release(mB2)
            RC.release(mC)
            if stage == 2:
                if dbg:
                    dump("ZT", ZT, [128, 4, N], BF16, [B_ZT])
                break

            mB2 = RB.mark()
            mC = RC.mark()
            mD = RD.mark()
            attnT, _ = RC.alloc("attnT", [128, DC, N], BF16)
            B_attnT = [RC.newbuf(f"attnT{i}") for i in range(4)]
            qkv = []
            for i in range(2):
                QT_, B_QT_ = RD.alloc(f"QT{i}", [128, N], BF16)
                KT_, B_KT_ = RD.alloc(f"KT{i}", [128, NK], BF16)
                VA_, B_VA_ = RD.alloc(f"VA{i}", [128, KC, 132], BF16)
                qkv.append((QT_, B_QT_, KT_, B_KT_, VA_, B_VA_))
                fw.op("pool", lambda e, VA_=VA_: e.memset(VA_[:, :, 128:129], 1.0), [], [B_VA_], acc=True)
            cossin, B_cs = RB.alloc("cossin", [128, 2, N], BF16)
            fw.dma("sp", cossin, cossin_d, [], [B_cs], B_cs)
            PT = [RB.alloc(f"PT{i}", [128, 1024], BF16) for i in range(3)]
            kraw = [RB.alloc(f"kraw{i}", [128, 512], BF16) for i in range(2)]
            t1 = [RB.alloc(f"t1_{i}", [128, 512], F32) for i in range(2)]
            t2 = [RB.alloc(f"t2_{i}", [128, 512], F32) for i in range(2)]
            ocp = [RB.alloc(f"ocp{i}", [128, 9, 160], F32) for i in range(2)]
            hd0 = [RB.alloc(f"hd0_{i}", [128, 4, 128], F32) for i in range(2)]
            hd1 = [RB.alloc(f"hd1_{i}", [128, 4, 128], F32) for i in range(2)]
            rope_ctr = 0
            pj_ctr = 0

            def inproj_units(h, wq, B_wq, standalone):
                QT, B_QT, KT, B_KT, VA, B_VA = qkv[h % 2]
                units = []

                def pbank():
                    nonlocal pj_ctr
                    pj_ctr += 1
                    if not standalone:
                        return 7
                    return pj_ctr % 4

                def kctx():
                    pb = pbank()
                    fw.op("pe", mm_group(ps[pb][:, 0:CTX], [(wq[:, kc, 128:256], HT[:, kc, 0:CTX]) for kc in range(DC)]),
                          [B_wq, B_HT[0]], [PS[pb]])
                    fw.op("dve", lambda e: e.tensor_copy(out=KT[:, 0:CTX], in_=ps[pb][:, 0:CTX]), [PS[pb]], [B_KT],
                          acc=True)
                units.append(kctx)

                def rope_pair(c0, dst_ap, dst_buf, tb):
                    st_ = {}

                    def ua():
                        nonlocal rope_ctr
                        r = rope_ctr % 2
                        rope_ctr += 1
                        st_["r"] = r
                        kr, B_kr = kraw[r]
                        pb = pbank()
                        fw.op("pe", mm_group(ps[pb], [
                            (wq[:, kc, c0:c0 + 128], HT[:, kc, CTX + tb * 512:CTX + (tb + 1) * 512])
                            for kc in range(DC)]), [B_wq, B_HT[1 + tb]], [PS[pb]])
                        fw.op("dve", lambda e: e.tensor_copy(out=kr, in_=ps[pb]), [PS[pb]], [B_kr])

                    def ub():
                        r = st_["r"]
                        kr, B_kr = kraw[r]
                        a1, B_a1 = t1[r]
                        a2, B_a2 = t2[r]
                        rb = pbank()
                        fw.op("pe", lambda e: e.matmul(ps[rb], lhsT=rt[:], rhs=kr, start=True, stop=True),
                              [B_kr, B_const], [PS[rb]])
                        fw.op("pool", lambda e: e.tensor_tensor(out=a1, in0=kr, in1=cossin[:, 0, tb * 512:(tb + 1) * 512],
                                                                op=ALU.mult), [B_kr, B_cs], [B_a1])
                        fw.op("dve", lambda e: e.tensor_tensor(out=a2, in0=ps[rb],
                                                               in1=cossin[:, 1, tb * 512:(tb + 1) * 512], op=ALU.mult),
                              [PS[rb], B_cs], [B_a2])
                        fw.op("pool", lambda e: e.tensor_tensor(out=dst_ap, in0=a1, in1=a2, op=ALU.add),
                              [B_a1, B_a2], [dst_buf], acc=True)
                    units.append(ua)
                    units.append(ub)

                for tb in range(4):
                    rope_pair(128, KT[:, CTX + tb * 512:CTX + (tb + 1) * 512], B_KT, tb)
                for tb in range(4):
                    rope_pair(0, QT[:, tb * 512:(tb + 1) * 512], B_QT, tb)

                def vunit(kc):
                    def u():
                        hb = B_HT[0] if kc < 2 else B_HT[1 + (kc - 2) // 4]
                        pb = pbank()
                        fw.op("pe", mm_group(ps[pb][:, 0:128], [
                            (HT[:, dc, kc * 128:(kc + 1) * 128], wq[:, dc, 256:384]) for dc in range(DC)]),
                            [B_wq, hb], [PS[pb]])
                        fw.op("dve", lambda e: e.tensor_copy(out=VA[:, kc, 0:128], in_=ps[pb][:, 0:128]),
                              [PS[pb]], [B_VA], acc=True)
                    return u
                for kc in range(KC):
                    units.append(vunit(kc))
                return units

            def ogrp(g):
                return 4 + g // 3, (g % 3) * 160

            NHR = int(os.environ.get('DBG_NH', NH))
            pending_fin = []
            wq0, B_wq0 = ringA.take()
            for u in inproj_units(0, wq0, B_wq0, True):
                u()
            for h in range(NHR):
                QT, B_QT, KT, B_KT, VA, B_VA = qkv[h % 2]
                nxt = []
                if h + 1 < NHR:
                    wqn, B_wqn = ringA.take()
                    nxt = inproj_units(h + 1, wqn, B_wqn, False)
                step = 0
                for qb in range(4):
                    q0 = qb * 512

                    def s_op(kc, q0=q0, QT=QT, KT=KT, B_QT=B_QT, B_KT=B_KT):
                        sb0 = 2 * (kc % 2)

                        def fn(e):
                            e.matmul(ps[sb0], lhsT=KT[0:64, kc * 128:(kc + 1) * 128],
                                     rhs=QT[0:64, q0:q0 + 512], start=True, stop=True)
                            return e.matmul(ps[sb0 + 1], lhsT=KT[64:128, kc * 128:(kc + 1) * 128],
                                            rhs=QT[64:128, q0:q0 + 512], start=True, stop=True)
                        fw.op("pe", fn, [B_KT, B_QT], [PS[sb0], PS[sb0 + 1]])

                    def av_op(kc, VA=VA, B_VA=B_VA):
                        pt, B_pt = PT[kc % 3]

                        def av(e):
                            ins = None
                            for g in range(8):
                                bank, c0 = ogrp(g)
                                i, qt = g // 4, g % 4
                                ins = e.matmul(ps[bank][:, c0:c0 + 129],
                                               lhsT=pt[:, i * 512 + qt * 128:i * 512 + (qt + 1) * 128],
                                               rhs=VA[:, kc, 0:129], start=(kc == 0 and g % 3 == 0),
                                               stop=(kc == KC - 1), skip_group_check=True)
                            return ins
                        fw.op("pe", av, [B_pt, B_VA], PS[4:7])

                    s_op(0)
                    for kc in range(KC):
                        if kc + 1 < KC:
                            s_op(kc + 1)
                        sb0 = 2 * (kc % 2)
                        pt, B_pt = PT[kc % 3]
                        fw.op("act", lambda e, pt=pt, sb0=sb0: e.activation(
                            out=pt, in_=psbig[:, sb0 * 512:sb0 * 512 + 1024], func=AF.Exp, scale=ATTN_SCALE),
                            [PS[sb0], PS[sb0 + 1]], [B_pt])
                        if nxt and (step % 2 == 1):
                            nxt.pop(0)()
                        step += 1
                        if kc >= 1:
                            av_op(kc - 1)
                        if kc == 8 and pending_fin:
                            if nxt and getattr(nxt[0], "second_half", False):
                                nxt.pop(0)()
                            pending_fin.pop(0)()
                    av_op(KC - 1)
                    def make_fin(qb=qb, q0=q0, h=h):
                        oc, B_oc = ocp[qb % 2]
                        h0, B_h0 = hd0[qb % 2]
                        h1, B_h1 = hd1[qb % 2]
                        sm, B_sm = small_alloc()

                        def fa():
                            for k in range(3):
                                ng = 3 if k < 2 else 2
                                fw.op("dve", lambda e, k=k, ng=ng: e.tensor_copy(
                                    out=oc[:, 3 * k:3 * k + ng, :],
                                    in_=ps[4 + k][:, 0:ng * 160].rearrange("p (a b) -> p a b", b=160)),
                                    [PS[4 + k]], [B_oc], acc=True)
                            fw.op("dve", lambda e: e.reciprocal(out=sm[:, 0:8].unsqueeze(2), in_=oc[:, 0:8, 128:129]),
                                  [B_oc], [B_sm])
                            fw.op("dve", lambda e: e.tensor_scalar(out=sm[:, 8:12], in0=sm[:, 4:8], scalar1=lamt[:, 2:3],
                                                                   scalar2=None, op0=ALU.mult), [B_sm, B_lam], [B_sm])
                            fw.op("dve", lambda e: e.tensor_tensor(
                                out=h0, in0=oc[:, 0:4, 0:128], in1=sm[:, 0:4].unsqueeze(2).broadcast_to([128, 4, 128]),
                                op=ALU.mult), [B_oc, B_sm], [B_h0])
                            fw.op("dve", lambda e: e.tensor_tensor(
                                out=h1, in0=oc[:, 4:8, 0:128], in1=sm[:, 8:12].unsqueeze(2).broadcast_to([128, 4, 128]),
                                op=ALU.mult), [B_oc, B_sm], [B_h1])
                            fw.op("pool", lambda e: e.tensor_tensor(out=h1, in0=h0, in1=h1, op=ALU.add),
                                  [B_h0, B_h1], [B_h1])
                            fw.op("dve", lambda e: e.tensor_tensor(out=h0, in0=h1, in1=h1, op=ALU.mult),
                                  [B_h1], [B_h0])
                            fw.op("dve", lambda e: e.tensor_reduce(out=sm[:, 12:16], in_=h0, axis=AX.X, op=ALU.add),
                                  [B_h0, B_sm], [B_sm])

                        def fbc():
                            fw.op("act", lambda e: e.activation(out=sm[:, 12:16], in_=sm[:, 12:16], func=AF.Ln,
                                                                scale=1.0 / 128, bias=epst[:, 0:1]),
                                  [B_sm, B_eps], [B_sm])
                            fw.op("act", lambda e: e.activation(out=sm[:, 12:16], in_=sm[:, 12:16], func=AF.Exp,
                                                                scale=-0.5), [B_sm], [B_sm])
                            fw.op("dve", lambda e: e.tensor_tensor(
                                out=h0, in0=h1, in1=sm[:, 12:16].unsqueeze(2).broadcast_to([128, 4, 128]), op=ALU.mult),
                                [B_h1, B_sm], [B_h0])
                            fw.op("pool", lambda e: e.tensor_tensor(
                                out=h1, in0=h0, in1=subg[:].unsqueeze(1).broadcast_to([128, 4, 128]), op=ALU.mult),
                                [B_h0, B_subg], [B_h1])

                            def trf(e):
                                ins = None
                                for qt in range(4):
                                    ins = e.transpose(ps[7][:, qt * 128:(qt + 1) * 128], h1[:, qt, :], ident[:])
                                return ins
                            fw.op("pe", trf, [B_h1, B_const], [PS[7]])
                            fw.op("dve", lambda e: e.tensor_copy(out=attnT[:, h, q0:q0 + 512], in_=ps[7]),
                                  [PS[7]], [B_attnT[qb]])
                        return fa, fbc
                    fa_, fbc_ = make_fin()
                    fa_()
                    pending_fin.append(fbc_)
                while nxt:
                    nxt.pop(0)()
            while pending_fin:
                pending_fin.pop(0)()
            RD.release(mD)
            RB.release(mB2)
            if stage == 3:
                if dbg:
                    dump("attnT", attnT, [128, DC, N], BF16, B_attnT)
                break

            mB2 = RB.mark()
            mD = RD.mark()
            YT, _ = RD.alloc("YT", [128, DC, N], BF16)
            B_YT = [RD.newbuf(f"YT{i}") for i in range(4)]
            sgt = [RB.alloc(f"sg{i}", [128, 512], BF16) for i in range(2)]
            tt_ = [RB.alloc(f"tt{i}", [128, 512], BF16) for i in range(2)]
            cnt4 = 0
            for cb in range(2):
                wfo, B_wfo = ringA.take()
                wgf, B_wgf = ringA.take()
                for dcl in range(4):
                    dc = cb * 4 + dcl
                    for tb in range(4):
                        bg = 2 * (cnt4 % 2)
                        bf_ = bg + 1
                        sg, B_sg = sgt[cnt4 % 2]
                        cnt4 += 1
                        fw.op("pe", mm_group(ps[bg][:, :], [
                            (wgf[:, kc, dcl * 128:(dcl + 1) * 128], HT[:, kc, CTX + tb * 512:CTX + (tb + 1) * 512])
                            for kc in range(DC)]), [B_wgf, B_HT[1 + tb]], [PS[bg]])
                        fw.op("pe", mm_group(ps[bf_][:, :], [
                            (wfo[:, g, dc * 128:(dc + 1) * 128], ZT[:, g, tb * 512:(tb + 1) * 512])
                            for g in range(4)]), [B_wfo, B_ZT], [PS[bf_]])
                        fw.op("act", lambda e, sg=sg, bg=bg: e.activation(out=sg, in_=ps[bg][:, :], func=AF.Sigmoid),
                              [PS[bg]], [B_sg])
                        fw.op("dve", lambda e, sg=sg, bf_=bf_, dc=dc, tb=tb: e.tensor_tensor(
                            out=YT[:, dc, tb * 512:(tb + 1) * 512], in0=ps[bf_][:, :], in1=sg, op=ALU.mult),
                            [PS[bf_], B_sg], [B_YT[tb]], acc=True)
                wga, B_wga = ringA.take()
                wao, B_wao = ringA.take()
                for dcl in range(4):
                    dc = cb * 4 + dcl
                    for tb in range(4):
                        bg = 4 + 2 * (cnt4 % 2)
                        ba = bg + 1
                        sg, B_sg = sgt[cnt4 % 2]
                        tq, B_tq = tt_[cnt4 % 2]
                        cnt4 += 1
                        fw.op("pe", mm_group(ps[bg][:, :], [
                            (wga[:, kc, dcl * 128:(dcl + 1) * 128], HT[:, kc, CTX + tb * 512:CTX + (tb + 1) * 512])
                            for kc in range(DC)]), [B_wga, B_HT[1 + tb]], [PS[bg]])
                        fw.op("pe", mm_group(ps[ba][:, :], [
                            (wao[:, kc, dcl * 128:(dcl + 1) * 128], attnT[:, kc, tb * 512:(tb + 1) * 512])
                            for kc in range(DC)]), [B_wao, B_attnT[tb]], [PS[ba]])
                        fw.op("act", lambda e, sg=sg, bg=bg: e.activation(out=sg, in_=ps[bg][:, :], func=AF.Sigmoid),
                              [PS[bg]], [B_sg])
                        fw.op("dve", lambda e, sg=sg, ba=ba, tq=tq: e.tensor_tensor(
                            out=tq, in0=ps[ba][:, :], in1=sg, op=ALU.mult), [PS[ba], B_sg], [B_tq])
                        fw.op("pool", lambda e, tq=tq, dc=dc, tb=tb: e.tensor_tensor(
                            out=YT[:, dc, tb * 512:(tb + 1) * 512], in0=YT[:, dc, tb * 512:(tb + 1) * 512], in1=tq,
                            op=ALU.add), [B_tq, B_YT[tb]], [B_YT[tb]])
            RB.release(mB2)
            RB.release(mB)
            RC.release(mC)
            if stage == 4:
                if dbg:
                    dump("YT", YT, [128, DC, N], BF16, B_YT)
                break

            mB = RB.mark()
            mC = RC.mark()
            x1r = [RB.alloc(f"x1r{i}", [128, D], F32) for i in range(2)]
            h2r = [RB.alloc(f"h2r{i}", [128, D], BF16) for i in range(2)]
            h2tmp, B_h2tmp = RB.alloc("h2tmp", [128, D], F32)
            A2bc, B_a2bc = RB.alloc("A2bc", [128, D], F32)
            B2bc, _ = RB.alloc("B2bc", [128, D], F32)
            fw.dma("sp", B2bc, gsc_s[b, 2], [B_scr["gsc"]], [B_a2bc], B_a2bc)
            fw.dma("sp", A2bc, gsc_s[b, 3], [B_scr["gsc"]], [B_a2bc], B_a2bc)
            wo0, B_wo0 = ringA.take()
            wo1, B_wo1 = ringA.take()
            wo = [wo0, wo1]
            mC1 = RC.mark()
            g1bc, B_g1 = RC.alloc("g1bc", [128, D], F32)
            xt2 = [RC.alloc(f"xt2_{i}", [128, D], F32) for i in range(2)]
            tmp2, B_tmp2 = RC.alloc("tmp2", [128, D], F32)
            xn2 = [RC.alloc(f"xn2_{i}", [128, D], F32) for i in range(2)]
            h2f, B_h2f = RC.alloc("h2f", [128, DC, 128], F32)
            wr, B_wr = RC.alloc("wr", [128, DC, 20], F32)
            brt, _ = RC.alloc("br", [128, 20], F32)
            LG, B_LG = RB.alloc("LG", [128, NT, 20], F32)
            RV, B_RW = RB.alloc("RV", [128, 7, NT], F32)
            RW3, _ = RB.alloc("RW3", [128, 7 * NT, 4], F32)
            RW3 = RW3.rearrange("p (i t) e -> p i t e", t=NT)
            RW4, _ = RB.alloc("RW4", [128, NT * 4, 4], F32)
            RW4 = RW4.rearrange("p (t g) e -> p t g e", g=4)
            fw.dma("sp", g1bc, gsc_s[b, 0], [B_scr["gsc"]], [B_g1], B_g1)
            fw.dma("sp", wr, wr_d, [], [B_wr], B_wr)
            fw.dma("sp", brt, br_d, [], [B_wr], B_wr)
            xt2.append(RB.alloc("xt2_2", [128, D], F32))

            def xload(tt, b=b):
                xv_, B_xv_ = xt2[tt % 3]
                fw.dma("sp", xv_, x_d[b, tt * 128:(tt + 1) * 128, :], [], [B_xv_], B_xv_)

            def stageA(tt, b=b):
                s = tt % 2
                xts, B_xts = xt2[tt % 3]
                xns, B_xns = xn2[s]
                if tt + 1 < NT:
                    xload(tt + 1)
                for nb_ in range(2):
                    bank = 2 * s + nb_
                    fw.op("pe", mm_group(ps[bank][:, :], [
                        (YT[:, kc, tt * 128:(tt + 1) * 128], wo[nb_][:, kc, :]) for kc in range(DC)]),
                        [B_YT[tt // 4], B_wo0, B_wo1], [PS[bank]])
                    fw.op("dve", lambda e, bank=bank, nb_=nb_: e.tensor_tensor(
                        out=tmp2[:, nb_ * 512:(nb_ + 1) * 512], in0=ps[bank][:, :],
                        in1=g1bc[:, nb_ * 512:(nb_ + 1) * 512], op=ALU.mult), [PS[bank], B_g1], [B_tmp2], acc=True)
                x1t, B_x1t = x1r[s]
                h2t, B_h2t = h2r[s]
                fw.op("pool", lambda e, x1t=x1t, xts=xts: e.tensor_tensor(out=x1t, in0=tmp2, in1=xts, op=ALU.add),
                      [B_tmp2, B_xts], [B_x1t])
                fw.dma("sp", x1_s[b * N + tt * 128:b * N + (tt + 1) * 128, :], x1t, [B_x1t], [B_x1s], B_x1t, acc=True)
                rstd, B_sm, _ = rms_stats(x1t, [B_x1t], xns, B_xns, D)
                fw.op("act", lambda e, x1t=x1t, xns=xns, rstd=rstd: e.activation(
                    out=xns, in_=x1t, func=AF.Identity, scale=rstd), [B_x1t, B_sm], [B_xns])
                fw.op("pool", lambda e, xns=xns: e.tensor_tensor(out=h2tmp, in0=xns, in1=A2bc, op=ALU.mult),
                      [B_xns, B_a2bc], [B_h2tmp])
                fw.op("pool", lambda e, h2t=h2t: e.tensor_tensor(out=h2t, in0=h2tmp, in1=B2bc, op=ALU.add),
                      [B_h2tmp, B_a2bc], [B_h2t])
                fw.dma("sp", h2_s[b * N + tt * 128:b * N + (tt + 1) * 128, :], h2t, [B_h2t], [B_h2s], B_h2t, acc=True)

            def stageB(tt, b=b):
                s = tt % 2
                xns, B_xns = xn2[s]
                for half in range(2):
                    bank = 4 + half

                    def tr(e, half=half, bank=bank, xns=xns):
                        ins = None
                        for j in range(4):
                            dc = half * 4 + j
                            ins = e.transpose(ps[bank][:, j * 128:(j + 1) * 128], xns[:, dc * 128:(dc + 1) * 128],
                                              ident[:])
                        return ins
                    fw.op("pe", tr, [B_xns, B_const], [PS[bank]])
                    for j in range(4):
                        dc = half * 4 + j
                        fw.op("dve", lambda e, dc=dc, j=j, bank=bank, b=b: e.tensor_scalar(
                            out=h2f[:, dc, :], in0=ps[bank][:, j * 128:(j + 1) * 128],
                            scalar1=modA2[:, dc, b:b + 1], scalar2=modB2[:, dc, b:b + 1],
                            op0=ALU.mult, op1=ALU.add), [PS[bank], B_mod], [B_h2f], acc=True)
                fw.op("pe", mm_group(ps[6][:, 0:20], [(h2f[:, kc, :], wr[:, kc, :]) for kc in range(DC)]),
                      [B_h2f, B_wr], [PS[6]])
                fw.op("dve", lambda e, tt=tt: e.tensor_tensor(out=LG[:, tt, :], in0=ps[6][:, 0:20], in1=brt,
                                                              op=ALU.add), [PS[6], B_wr], [B_LG], acc=True)

            xload(0)
            stageA(0)
            for tt in range(NT):
                if tt + 1 < NT:
                    stageA(tt + 1)
                stageB(tt)
            lg = LG[:, :, 0:4]
            le4 = LG[:, :, 4:20].rearrange("p t (g e) -> p t g e", e=4)
            T3 = [128, NT, 4]
            T4 = [128, NT, 4, 4]

            def RR(fn):
                fw.op("dve", fn, [B_LG, B_RW], [B_RW])

            def bc3(v):
                return v.unsqueeze(2).broadcast_to(T3)
            gmax, wgrp, m1, m2, dd, p1, p2 = (RV[:, i, :] for i in range(7))
            ohg, dlg, leg, oh1, leg2, oh2, gi = (RW3[:, i, :, :] for i in range(7))
            RR(lambda e: e.tensor_reduce(out=gmax, in_=lg, axis=AX.X, op=ALU.max))
            RR(lambda e: e.tensor_tensor(out=ohg, in0=lg, in1=bc3(gmax), op=ALU.is_equal))
            RR(lambda e: e.tensor_tensor(out=dlg, in0=lg, in1=bc3(gmax), op=ALU.subtract))
            fw.op("act", lambda e: e.activation(out=dlg, in_=dlg, func=AF.Exp), [B_RW], [B_RW])
            RR(lambda e: e.tensor_reduce(out=wgrp, in_=dlg, axis=AX.X, op=ALU.add))
            RR(lambda e: e.reciprocal(out=wgrp, in_=wgrp))
            RR(lambda e: e.tensor_tensor(out=RW4, in0=le4, in1=ohg.unsqueeze(3).broadcast_to(T4), op=ALU.mult))
            RR(lambda e: e.tensor_reduce(out=leg, in_=RW4.rearrange("p t g e -> p t e g"), axis=AX.X, op=ALU.add))
            RR(lambda e: e.tensor_reduce(out=m1, in_=leg, axis=AX.X, op=ALU.max))
            RR(lambda e: e.tensor_tensor(out=oh1, in0=leg, in1=bc3(m1), op=ALU.is_equal))
            RR(lambda e: e.scalar_tensor_tensor(out=leg2, in0=oh1, scalar=-1e30, in1=leg, op0=ALU.mult, op1=ALU.add))
            RR(lambda e: e.tensor_reduce(out=m2, in_=leg2, axis=AX.X, op=ALU.max))
            RR(lambda e: e.tensor_tensor(out=oh2, in0=leg2, in1=bc3(m2), op=ALU.is_equal))
            RR(lambda e: e.tensor_tensor(out=dd, in0=m2, in1=m1, op=ALU.subtract))
            fw.op("act", lambda e: e.activation(out=dd, in_=dd, func=AF.Exp), [B_RW], [B_RW])
            RR(lambda e: e.tensor_scalar(out=p1, in0=dd, scalar1=1.0, scalar2=None, op0=ALU.add))
            RR(lambda e: e.reciprocal(out=p1, in_=p1))
            RR(lambda e: e.tensor_tensor(out=p2, in0=dd, in1=p1, op=ALU.mult))
            RR(lambda e: e.tensor_tensor(out=p1, in0=p1, in1=wgrp, op=ALU.mult))
            RR(lambda e: e.tensor_tensor(out=p2, in0=p2, in1=wgrp, op=ALU.mult))
            RR(lambda e: e.tensor_tensor(out=gi, in0=oh1, in1=bc3(p1), op=ALU.mult))
            RR(lambda e: e.tensor_tensor(out=oh2, in0=oh2, in1=bc3(p2), op=ALU.mult))
            RR(lambda e: e.tensor_tensor(out=gi, in0=gi, in1=oh2, op=ALU.add))
            fw.op("dve", lambda e, b=b: e.tensor_tensor(
                out=Gall[:, b * NT:(b + 1) * NT, :].rearrange("p t (g e) -> p t g e", e=4),
                in0=gi.unsqueeze(2).broadcast_to(T4), in1=ohg.unsqueeze(3).broadcast_to(T4), op=ALU.mult),
                [B_RW], [B_G], acc=True)
            RD.release(mD)
            RC.release(mC1)
            RC.release(mC)
            RB.release(mB)

        if stage >= 5:
            JJ = J * 16
            mC = RC.mark()
            mB = RB.mark()
            mD = RD.mark()
            ustr, B_sc0 = RC.alloc("ustr", [128, 128], F32)
            onesm, _ = RC.alloc("onesm", [128, 128], F32)
            thr16, _ = RC.alloc("thr16", [128, 16], F32)
            thr48, _ = RC.alloc("thr48", [128, 48], F32)
            ltm, _ = RC.alloc("ltm", [128, 16, 16], F32)
            fw.dma("sp", ustr, ustrict_d, [], [B_sc0], B_sc0)
            fw.dma("sp", thr16, thr16_d, [], [B_sc0], B_sc0)
            fw.dma("sp", thr48, thr48_d, [], [B_sc0], B_sc0)
            fw.dma("sp", ltm, ltmask_d, [], [B_sc0], B_sc0)
            B_on = Buf("onesm")
            fw.op("dve", lambda e: e.memset(onesm, 1.0), [], [B_on])
            Mt, B_M = RB.alloc("Mt", [128, J, 16], F32)
            rank, B_rank = RB.alloc("rank", [128, J, 16], F32)
            tot, B_tot = RB.alloc("tot", [128, J, 16], F32)
            cum, B_cum = RB.alloc("cum", [128, J, 16], F32)
            smt, B_smt = RB.alloc("smt", [128, J, 16], F32)
            s48, B_s48 = RB.alloc("s48", [128, 48, 16], F32)
            s16, B_s16 = RB.alloc("s16", [128, 16, 16], F32)
            vec, B_vec = RC.alloc("vec", [128, 8, 16], F32)
            slots, B_slots = RC.alloc("slots", [128, 6, J], F32)
            sloti, B_sloti = RC.alloc("sloti", [128, 2, J], mybir.dt.int32)
            texpf, B_texp = RC.alloc("texpf", [128, 48], F32)
            texpi, _ = RC.alloc("texpi", [128, 48], mybir.dt.int32)
            pidx, _ = RC.alloc("pidx", [128, 1], F32)
            fw.dma("sp", pidx, pidx_d, [], [B_sc0], B_sc0)
            Mf = Mt.rearrange("p j e -> p (j e)")
            fw.op("dve", lambda e: e.tensor_single_scalar(out=Mt, in_=Gall[:], scalar=0.0, op=ALU.is_gt),
                  [B_G], [B_M])
            for cbk in range((JJ + 511) // 512):
                c0, c1 = cbk * 512, min(JJ, (cbk + 1) * 512)
                fw.op("pe", lambda e, c0=c0, c1=c1: e.matmul(ps[0][:, 0:c1 - c0], lhsT=ustr, rhs=Mf[:, c0:c1],
                                                             start=True, stop=True), [B_M, B_sc0], [PS[0]])
                fw.op("pe", lambda e, c0=c0, c1=c1: e.matmul(ps[1][:, 0:c1 - c0], lhsT=onesm, rhs=Mf[:, c0:c1],
                                                             start=True, stop=True), [B_M, B_on], [PS[1]])
                fw.op("dve", lambda e, c0=c0, c1=c1: e.tensor_copy(
                    out=rank.rearrange("p j e -> p (j e)")[:, c0:c1], in_=ps[0][:, 0:c1 - c0]), [PS[0]], [B_rank],
                    acc=True)
                fw.op("dve", lambda e, c0=c0, c1=c1: e.tensor_copy(
                    out=tot.rearrange("p j e -> p (j e)")[:, c0:c1], in_=ps[1][:, 0:c1 - c0]), [PS[1]], [B_tot],
                    acc=True)
            fw.op("dve", lambda e: e.memset(cum[:, 0, :], 0.0), [], [B_cum])
            for j in range(1, J):
                fw.op("dve", lambda e, j=j: e.tensor_tensor(out=cum[:, j, :], in0=cum[:, j - 1, :], in1=tot[:, j - 1, :],
                                                            op=ALU.add), [B_cum, B_tot], [B_cum])
            cnt = vec[:, 0, :]
            ntl = vec[:, 1, :]
            off = vec[:, 2, :]
            fw.op("dve", lambda e: e.tensor_tensor(out=cnt, in0=cum[:, J - 1, :], in1=tot[:, J - 1, :], op=ALU.add),
                  [B_cum, B_tot], [B_vec])
            fw.op("dve", lambda e: e.tensor_tensor(out=s16, in0=cnt.unsqueeze(2).broadcast_to([128, 16, 16]),
                                                   in1=thr16.unsqueeze(1).broadcast_to([128, 16, 16]), op=ALU.is_gt),
                  [B_vec, B_sc0], [B_s16])
            fw.op("dve", lambda e: e.tensor_reduce(out=ntl, in_=s16, axis=AX.X, op=ALU.add), [B_s16, B_vec], [B_vec])
            fw.op("dve", lambda e: e.tensor_scalar(out=ntl, in0=ntl, scalar1=512.0, scalar2=None, op0=ALU.mult),
                  [B_vec], [B_vec])
            fw.op("dve", lambda e: e.tensor_tensor(out=s16, in0=ltm, in1=ntl.unsqueeze(1).broadcast_to([128, 16, 16]),
                                                   op=ALU.mult), [B_vec, B_sc0, B_s16], [B_s16])
            fw.op("dve", lambda e: e.tensor_reduce(out=off, in_=s16, axis=AX.X, op=ALU.add), [B_s16, B_vec], [B_vec])
            fw.op("dve", lambda e: e.tensor_tensor(out=rank, in0=rank, in1=cum, op=ALU.add), [B_rank, B_cum], [B_rank])
            fw.op("dve", lambda e: e.tensor_tensor(out=rank, in0=rank, in1=off.unsqueeze(1).broadcast_to([128, J, 16]),
                                                   op=ALU.add), [B_rank, B_vec], [B_rank])
            fw.op("dve", lambda e: e.tensor_tensor(out=smt, in0=rank, in1=Mt, op=ALU.mult), [B_rank, B_M], [B_smt])
            fw.op("dve", lambda e: e.tensor_reduce(out=slots[:, 0, :], in_=smt, axis=AX.X, op=ALU.add),
                  [B_smt], [B_slots])
            fw.op("dve", lambda e: e.tensor_reduce(out=slots[:, 1, :], in_=smt, axis=AX.X, op=ALU.max),
                  [B_smt, B_slots], [B_slots])
            fw.op("dve", lambda e: e.tensor_tensor(out=slots[:, 2, :], in0=slots[:, 0, :], in1=slots[:, 1, :],
                                                   op=ALU.subtract), [B_slots], [B_slots])
            fw.op("dve", lambda e: e.tensor_tensor(out=cum, in0=smt,
                                                   in1=slots[:, 1, :].unsqueeze(2).broadcast_to([128, J, 16]),
                                                   op=ALU.is_equal), [B_smt, B_slots, B_cum], [B_cum])
            fw.op("dve", lambda e: e.tensor_tensor(out=cum, in0=cum, in1=Gall[:], op=ALU.mult), [B_cum, B_G], [B_cum])
            fw.op("dve", lambda e: e.tensor_reduce(out=slots[:, 4, :], in_=cum, axis=AX.X, op=ALU.add),
                  [B_cum, B_slots], [B_slots])
            fw.op("dve", lambda e: e.tensor_reduce(out=slots[:, 3, :], in_=Gall[:], axis=AX.X, op=ALU.add),
                  [B_G, B_slots], [B_slots])
            fw.op("dve", lambda e: e.tensor_tensor(out=slots[:, 5, :], in0=slots[:, 3, :], in1=slots[:, 4, :],
                                                   op=ALU.subtract), [B_slots], [B_slots])
            fw.op("dve", lambda e: e.tensor_copy(out=sloti, in_=slots[:, 1:3, :]), [B_slots], [B_sloti])
            fw.op("dve", lambda e: e.tensor_tensor(out=s48, in0=off.unsqueeze(1).broadcast_to([128, 48, 16]),
                                                   in1=thr48.unsqueeze(2).broadcast_to([128, 48, 16]), op=ALU.is_le),
                  [B_vec, B_sc0], [B_s48])
            fw.op("dve", lambda e: e.tensor_reduce(out=texpf, in_=s48, axis=AX.X, op=ALU.add), [B_s48], [B_texp])
            fw.op("dve", lambda e: e.tensor_scalar(out=texpf, in0=texpf, scalar1=-1.0, scalar2=0.0, op0=ALU.add,
                                                   op1=ALU.max), [B_texp], [B_texp])
            fw.op("dve", lambda e: e.tensor_scalar(out=texpf, in0=texpf, scalar1=15.0, scalar2=None, op0=ALU.min),
                  [B_texp], [B_texp])
            fw.op("dve", lambda e: e.tensor_scalar(out=texpf, in0=texpf, scalar1=128.0, scalar2=pidx[:, 0:1],
                                                   op0=ALU.mult, op1=ALU.add), [B_texp, B_sc0], [B_texp])
            fw.op("dve", lambda e: e.tensor_copy(out=texpi, in_=texpf), [B_texp], [B_texp])
            if dbg and stage == 5:
                dump("G", Gall[:], [128, J, 16], F32, [B_G])
                dump("slots", slots, [128, 6, J], F32, [B_slots])
                dump("sloti", sloti, [128, 2, J], mybir.dt.int32, [B_sloti])
                dump("texpi", texpi, [128, 48], mybir.dt.int32, [B_texp])
                dump("vec", vec, [128, 8, 16], F32, [B_vec])
        if stage >= 6:
            RB.release(mB)
            mB = RB.mark()
            B_hs = Buf("hs_s")
            B_ys = Buf("ys_s")
            h2l = [RB.alloc(f"h2l{i}", [128, D], BF16) for i in range(4)]
            for j in range(J):
                hv, B_hv = h2l[j % 4]
                fw.dma("sp", hv, h2_s[j * 128:(j + 1) * 128, :], [B_h2s], [B_hv], B_hv)
                for k in range(2):
                    fw.dma_fn("pool", lambda e, hv=hv, j=j, k=k: e.indirect_dma_start(
                        out=hs_s[:, :], out_offset=bass.IndirectOffsetOnAxis(ap=sloti[:, k, j:j + 1], axis=0),
                        in_=hv, in_offset=None), [B_hv, B_sloti], [B_hs], B_hv, acc=True)
            RB.release(mB)
            mB = RB.mark()
            mslots = list(ring_slots)
            for i in range(4):
                v, bb = RD.alloc(f"mring{i}", [128, 4096], BF16)
                mslots.append((v, bb))
            hsl = [RB.alloc(f"hsl{i}", [128, 4, D], BF16) for i in range(2)]
            hsT = [RB.alloc(f"hsT{i}", [128, DC, 512], BF16) for i in range(2)]
            gT = [RB.alloc(f"gT{i}", [128, 4, 512], BF16) for i in range(2)]
            sil = [RB.alloc(f"sil{i}", [128, 512], BF16) for i in range(2)]
            ysb = [RB.alloc(f"ysb{i}", [128, 4, D], BF16) for i in range(2)]
            identb, B_idb = RC.alloc("identb", [128, 128], BF16)
            fw.op("dve", lambda e: e.tensor_copy(out=identb, in_=ident[:]), [B_const], [B_idb])
            psb = [ps[i].bitcast(BF16) for i in range(8)]
            wsl = 0
            c5 = 0
            cy = 0
            NSTr = int(os.environ.get("DBG_NST", NST))
            def s4_fetch(s_):
                nonlocal wsl
                wv = []
                for wi, (wsrc, shape) in enumerate(((w1_s, [128, DC, EH]), (w3_s, [128, DC, EH]), (w2_s, [128, 4, D]))):
                    ap_, bf_ = mslots[wsl % 8]
                    wsl += 1
                    v = chunk_view(ap_, shape)
                    rows = wsrc.rearrange("e p a b -> (e p) (a b)")
                    fw.dma_fn("pool", lambda e, s_=s_, rows=rows, ap_=ap_: e.indirect_dma_start(
                        out=ap_, out_offset=None, in_=rows,
                        in_offset=bass.IndirectOffsetOnAxis(ap=texpi[:, s_:s_ + 1], axis=0)),
                        [B_texp] + B_w1, [bf_], bf_)
                    wv.append((v, bf_))
                hl, B_hl = hsl[s_ % 2]
                fw.dma("sp", hl, hs_s[s_ * 512:(s_ + 1) * 512, :].rearrange("(a p) d -> p a d", p=128),
                       [B_hs], [B_hl], B_hl)
                return wv

            fetched = {0: s4_fetch(0)}
            for s_ in range(NSTr):
                if s_ + 1 < NSTr:
                    fetched[s_ + 1] = s4_fetch(s_ + 1)
                (w1, B_w1c), (w3, B_w3c), (w2, B_w2c) = fetched.pop(s_)
                hl, B_hl = hsl[s_ % 2]
                hT, B_hT = hsT[s_ % 2]
                gt, B_gt = gT[s_ % 2]
                yb_, B_yb = ysb[s_ % 2]
                for dc in range(DC):
                    bank = 6 + dc % 2

                    def trb(e, dc=dc, bank=bank, hl=hl):
                        ins = None
                        for a in range(4):
                            ins = e.transpose(psb[bank][:, a * 128:(a + 1) * 128], hl[:, a, dc * 128:(dc + 1) * 128],
                                              identb)
                        return ins
                    fw.op("pe", trb, [B_hl, B_idb], [PS[bank]])
                    evac_copy(beng(bank), hT[:, dc, :], psb[bank][:, 0:512], [PS[bank]], [B_hT])
                for hc in range(4):
                    b1 = 2 * (c5 % 2)
                    b3 = b1 + 1
                    sl, B_sl = sil[c5 % 2]
                    c5 += 1
                    fw.op("pe", mm_group(ps[b1], [(w1[:, kc, hc * 128:(hc + 1) * 128], hT[:, kc, :]) for kc in range(DC)]),
                          [B_w1c, B_hT], [PS[b1]])
                    fw.op("pe", mm_group(ps[b3], [(w3[:, kc, hc * 128:(hc + 1) * 128], hT[:, kc, :]) for kc in range(DC)]),
                          [B_w3c, B_hT], [PS[b3]])
                    fw.op("act", lambda e, sl=sl, b1=b1: e.activation(out=sl, in_=ps[b1], func=AF.Silu), [PS[b1]], [B_sl])
                    fw.op("dve", lambda e, sl=sl, b3=b3, gt=gt, hc=hc: e.tensor_tensor(
                        out=gt[:, hc, :], in0=ps[b3], in1=sl, op=ALU.mult), [PS[b3], B_sl], [B_gt], acc=True)
                for a in range(4):
                    for nb_ in range(2):
                        yb = 4 + (cy % 2)
                        cy += 1
                        fw.op("pe", mm_group(ps[yb], [
                            (gt[:, hc, a * 128:(a + 1) * 128], w2[:, hc, nb_ * 512:(nb_ + 1) * 512]) for hc in range(4)]),
                            [B_gt, B_w2c], [PS[yb]])
                        evac_copy(beng(yb), yb_[:, a, nb_ * 512:(nb_ + 1) * 512], ps[yb], [PS[yb]], [B_yb])
                fw.dma("sp", ys_s[s_ * 512:(s_ + 1) * 512, :].rearrange("(a p) d -> p a d", p=128), yb_,
                       [B_yb], [B_ys], B_yb, acc=True)
            RB.release(mB)
            RD.release(mD)
            mB = RB.mark()
            ya = [RB.alloc(f"ya{i}", [128, D], BF16) for i in range(2)]
            ybb = [RB.alloc(f"yb{i}", [128, D], BF16) for i in range(2)]
            x1l = [RB.alloc(f"x1l{i}", [128, D], F32) for i in range(2)]
            mt_ = [RB.alloc(f"mt{i}", [128, D], F32) for i in range(2)]
            ot = [RB.alloc(f"ot{i}", [128, D], F32) for i in range(2)]
            fing, B_fing = RB.alloc("fing", [128, D], F32)
            g2bc, B_g2 = RB.alloc("g2bc", [128, D], F32)
            fw.dma("sp", fing, fing_d, [], [B_fing], B_fing)
            g2r = [(g2bc, B_g2)] + [RB.alloc("g2bc1", [128, D], F32)]

            def s5_fetch(j):
                s = j % 2
                if j % NT == 0:
                    gv, B_gv = g2r[(j // NT) % 2]
                    fw.dma("sp", gv, gsc_s[j // NT, 1], [B_scr["gsc"]], [B_gv], B_gv)
                yav, B_ya = ya[s]
                ybv, B_ybv = ybb[s]
                xv, B_xv = x1l[s]
                fw.dma("sp", xv, x1_s[j * 128:(j + 1) * 128, :], [B_x1s], [B_xv], B_xv)
                fw.dma_fn("pool", lambda e, yav=yav, j=j: e.indirect_dma_start(
                    out=yav, out_offset=None, in_=ys_s[:, :],
                    in_offset=bass.IndirectOffsetOnAxis(ap=sloti[:, 0, j:j + 1], axis=0)),
                    [B_ys, B_sloti], [B_ya], B_ya)
                fw.dma_fn("pool", lambda e, ybv=ybv, j=j: e.indirect_dma_start(
                    out=ybv, out_offset=None, in_=ys_s[:, :],
                    in_offset=bass.IndirectOffsetOnAxis(ap=sloti[:, 1, j:j + 1], axis=0)),
                    [B_ys, B_sloti], [B_ybv], B_ybv)

            s5_fetch(0)
            for j in range(J):
                b = j // NT
                s = j % 2
                if j + 1 < J:
                    s5_fetch(j + 1)
                gv, B_gv = g2r[b % 2]
                yav, B_ya = ya[s]
                ybv, B_ybv = ybb[s]
                xv, B_xv = x1l[s]
                mv, B_mv = mt_[s]
                o_, B_o = ot[s]
                fw.op("dve", lambda e, mv=mv, yav=yav, j=j: e.tensor_scalar(
                    out=mv, in0=yav, scalar1=slots[:, 4, j:j + 1], scalar2=None, op0=ALU.mult),
                    [B_ya, B_slots], [B_mv])
                fw.op("dve", lambda e, mv=mv, ybv=ybv, j=j: e.scalar_tensor_tensor(
                    out=mv, in0=ybv, scalar=slots[:, 5, j:j + 1], in1=mv, op0=ALU.mult, op1=ALU.add),
                    [B_ybv, B_slots, B_mv], [B_mv])
                fw.op("dve", lambda e, mv=mv, gv=gv: e.tensor_tensor(out=mv, in0=mv, in1=gv, op=ALU.mult),
                      [B_mv, B_gv], [B_mv])
                fw.op("pool", lambda e, mv=mv, xv=xv: e.tensor_tensor(out=xv, in0=xv, in1=mv, op=ALU.add),
                      [B_mv, B_xv], [B_xv])
                rstd, B_sm, _ = rms_stats(xv, [B_xv], o_, B_o, D)
                fw.op("dve", lambda e, o_=o_, xv=xv, rstd=rstd: e.scalar_tensor_tensor(
                    out=o_, in0=xv, scalar=rstd, in1=fing, op0=ALU.mult, op1=ALU.mult),
                    [B_xv, B_sm, B_fing], [B_o])
                fw.dma("sp", out_d[b, (j % NT) * 128:(j % NT + 1) * 128, :], o_, [B_o], [B_out], B_o, acc=True)
            RB.release(mB)

        fw._waits("sp", dict(fw.dma_events))
        fw.emit()
    return nc


def host_inputs(inputs, core, nb=4):
    f = np.float32
    cs = get_consts()
    b0 = core * nb
    m = {}
    m["x"] = np.ascontiguousarray(inputs["x"][b0:b0 + nb], dtype=f)
    m["ctx"] = np.ascontiguousarray(inputs["ctx"][b0:b0 + nb], dtype=f)
    cT = np.zeros((128, DC, 8), f)
    cc = np.asarray(inputs["c"][b0:b0 + nb], dtype=f)
    cT[:, :, :nb] = cc.reshape(nb, DC, 128).transpose(2, 1, 0)
    cT[:, :, 4] = np.asarray(inputs["c_ctx"], dtype=f).reshape(DC, 128).T
    m["cT"] = cT
    m["w_mod"] = np.ascontiguousarray(inputs["w_mod"][0], dtype=f)
    bm = np.asarray(inputs["b_mod"][0], dtype=f)
    m["bmodT"] = np.ascontiguousarray(bm.reshape(48, 128).T)
    m["bmg"] = np.ascontiguousarray(np.broadcast_to(np.concatenate([bm[2048:3072], bm[5120:6144]])[None, :], (128, 2048)))
    m["n1g"] = np.ascontiguousarray(np.asarray(inputs["norm1_g"][0], dtype=f).reshape(DC, 128).T)
    m["n2g"] = np.ascontiguousarray(np.asarray(inputs["norm2_g"][0], dtype=f).reshape(DC, 128).T)
    m["fing"] = np.ascontiguousarray(np.broadcast_to(np.asarray(inputs["final_g"], dtype=f)[None, :], (128, D)))
    lamv = np.concatenate([np.asarray(inputs[k][0], dtype=f) for k in ("lam_q1", "lam_k1", "lam_q2", "lam_k2")])
    m["lamv"] = np.ascontiguousarray(np.broadcast_to(lamv[None, :], (128, 256)))
    m["subg"] = np.ascontiguousarray(np.broadcast_to(np.asarray(inputs["subln_g"][0], dtype=f)[None, :], (128, 128)))
    wr = np.concatenate([np.asarray(inputs["w_router_group"][0], dtype=f),
                         np.asarray(inputs["w_router_expert"][0], dtype=f)], axis=1)
    m["wr"] = np.ascontiguousarray(wr.reshape(DC, 128, 20).transpose(1, 0, 2))
    br = np.concatenate([np.asarray(inputs["b_router_group"][0], dtype=f),
                         np.asarray(inputs["b_router_expert"][0], dtype=f)])
    m["br"] = np.ascontiguousarray(np.broadcast_to(br[None, :], (128, 20)))
    m["w_in"] = np.ascontiguousarray(inputs["w_in"][0], dtype=f)
    m["w_ao"] = np.ascontiguousarray(inputs["w_attn_out"][0], dtype=f)
    m["w_fo"] = np.ascontiguousarray(inputs["w_four_out"][0], dtype=f)
    m["w_o"] = np.ascontiguousarray(inputs["w_out"][0], dtype=f)
    m["w1"] = np.ascontiguousarray(inputs["w_exp_gate"][0], dtype=f)
    m["w3"] = np.ascontiguousarray(inputs["w_exp_up"][0], dtype=f)
    m["w2"] = np.ascontiguousarray(inputs["w_exp_down"][0], dtype=f)
    m["bmg"] = np.ascontiguousarray(np.broadcast_to(
        np.concatenate([bm[2048:3072], bm[5120:6144], bm[3072:4096], bm[4096:5120]])[None, :], (128, 4096)))
    m["n2gbc"] = np.ascontiguousarray(np.broadcast_to(np.asarray(inputs["norm2_g"][0], dtype=f)[None, :], (128, D)))
    for k in ("ident", "rt", "cossin", "cs_c", "dft", "ustrict", "thr16", "thr48", "ltmask", "pidx"):
        m[k] = cs[k]
    return m


def kernel(**inputs):
    nb = 4
    nc = build(nb)
    in_maps = [host_inputs(inputs, c, nb) for c in range(N_CORES)]
    res = run_bass_kernel_spmd(nc, in_maps, core_ids=list(range(N_CORES)))
    return np.concatenate([np.asarray(r["out"]) for r in res.results], axis=0).astype(np.float32)
```
